# Optimizing a Trainium2 kernel written in Bass

```python
import jax
import jax.numpy as jnp
from jax import lax
import numpy as np

D_MODEL = 2048
BATCH = 1
SEQ = 16384
DEPTH = 2

GRID_W = 64
CTX_LEN = 256
NORM_EPS = 1e-6
CHUNK = 32

MIX_HALF = D_MODEL // 2

HEAD_DIM = 128
ATT_HEADS = MIX_HALF // HEAD_DIM
ATT_KV_HEADS = 2
WINDOW = 128
ATT_BLOCK = 128
ROPE_THETA = 10000.0

HGRN_HEADS = 8
HGRN_DK = 128
HGRN_DV = MIX_HALF // HGRN_HEADS
N_HGRN_LAYERS = (DEPTH + 1) // 2

GLA_HEADS = 4
GLA_DK = MIX_HALF // 2 // GLA_HEADS
GLA_DV = MIX_HALF // GLA_HEADS
GLA_GATE_RANK = 16
GLA_GATE_NORMALIZER = 16.0

RWKV_N = 64
RWKV_HEADS = MIX_HALF // RWKV_N
RWKV_DECAY_RANK = 96
RWKV_A_RANK = 96
RWKV_GATE_RANK = 256
RWKV_LN_EPS = 64e-5

D_FF = 5632
N_EXPERTS = 8
TOP_K = 2
D_FF_EXPERT = 7168

ATT_Q = ATT_HEADS * HEAD_DIM
ATT_KV = ATT_KV_HEADS * HEAD_DIM
HGRN_K = HGRN_HEADS * HGRN_DK
HGRN_V = HGRN_HEADS * HGRN_DV
EVEN_COLS = (ATT_Q, ATT_KV, ATT_KV, HGRN_K, HGRN_V, HGRN_K, HGRN_K, HGRN_V)
EVEN_IN = sum(EVEN_COLS)
EVEN_OUT = ATT_Q + HGRN_V
GLA_K = GLA_HEADS * GLA_DK
GLA_V = GLA_HEADS * GLA_DV
GLA_COLS = (GLA_K, GLA_K, GLA_V, GLA_GATE_RANK, GLA_GATE_RANK, GLA_V)
GLA_IN = sum(GLA_COLS)
RWKV_C = RWKV_HEADS * RWKV_N
RWKV_COLS = (RWKV_C, RWKV_C, RWKV_C, RWKV_DECAY_RANK, RWKV_DECAY_RANK, RWKV_A_RANK, RWKV_GATE_RANK)
RWKV_IN = sum(RWKV_COLS)
ODD_IN = GLA_IN + RWKV_IN
ODD_OUT = GLA_V + RWKV_C

kernel_name = 'hybrid_diffusion_trunk'


def rmsnorm(x, gain):
    xf = x.astype(jnp.float32)
    y = xf * lax.rsqrt(jnp.mean(xf * xf, axis=-1, keepdims=True) + NORM_EPS)
    return (y * gain.astype(jnp.float32)).astype(x.dtype)


def split_last(t, sizes):
    return jnp.split(t, np.cumsum(sizes)[:-1].tolist(), axis=-1)


def to_heads(t, n_heads):
    return t.reshape(t.shape[:-1] + (n_heads, -1))


def adaln(cvec, w, b):
    return jnp.split(jax.nn.silu(cvec) @ w + b, 6, axis=-1)


def axial_rope_tables(rows):
    n_freq = HEAD_DIM // 4
    t = jnp.arange(rows * GRID_W)
    row = (t // GRID_W).astype(jnp.float32)
    col = (t % GRID_W).astype(jnp.float32)
    inv = ROPE_THETA ** (-jnp.arange(n_freq, dtype=jnp.float32) / n_freq)
    ang = jnp.stack([row[:, None] * inv, col[:, None] * inv], axis=1)
    return jnp.cos(ang), jnp.sin(ang)


def apply_axial_rope(x, cos, sin):
    n_freq = HEAD_DIM // 4
    xr = x.reshape(x.shape[:-1] + (2, 2, n_freq))
    x1, x2 = xr[..., 0, :], xr[..., 1, :]
    cb, sb = cos[None, :, None], sin[None, :, None]
    out = jnp.stack([x1 * cb - x2 * sb, x1 * sb + x2 * cb], axis=-2)
    return out.reshape(x.shape)


def sink_softmax(scores, sink):
    m = sink
    for s in scores:
        m = jnp.maximum(m, jnp.max(s, axis=-1, keepdims=True))
    probs = [jnp.exp(s - m) for s in scores]
    denom = jnp.exp(sink - m)
    for p in probs:
        denom = denom + jnp.sum(p, axis=-1, keepdims=True)
    return [p / denom for p in probs]


def window_sink_attention(q, k, v, kc, vc, sink):
    B, S, H, Dh = q.shape
    G = k.shape[2]
    R = H // G
    nb = S // ATT_BLOCK
    qb = q.reshape(B, nb, ATT_BLOCK, G, R, Dh) * (Dh ** -0.5)

    def band(t):
        pad = jnp.zeros((B, ATT_BLOCK) + t.shape[2:], t.dtype)
        tb = jnp.concatenate([pad, t, pad], axis=1).reshape((B, nb + 2, ATT_BLOCK) + t.shape[2:])
        return jnp.concatenate([tb[:, :-2], tb[:, 1:-1], tb[:, 2:]], axis=2)

    kw, vw = band(k), band(v)
    s_win = jnp.einsum('bnqgrd,bnkgd->bngrqk', qb, kw)
    s_ctx = jnp.einsum('bnqgrd,bcgd->bngrqc', qb, kc)
    qi = jnp.arange(ATT_BLOCK)[:, None]
    kj = jnp.arange(3 * ATT_BLOCK)[None, :]
    kpos = jnp.arange(nb)[:, None, None] * ATT_BLOCK + (kj - ATT_BLOCK)[None]
    valid = (jnp.abs(kj - ATT_BLOCK - qi) <= WINDOW)[None] & (kpos >= 0) & (kpos < S)
    s_win = jnp.where(valid[None, :, None, None], s_win, -jnp.inf)
    p_win, p_ctx = sink_softmax([s_win, s_ctx], sink.reshape(G, R)[None, None, :, :, None, None])
    o = jnp.einsum('bngrqk,bnkgd->bnqgrd', p_win, vw) + jnp.einsum('bngrqc,bcgd->bnqgrd', p_ctx, vc)
    return o.reshape(B, S, H * Dh)


def context_sink_attention(qc, kc, vc, sink):
    B, L, H, Dh = qc.shape
    G = kc.shape[2]
    R = H // G
    qg = qc.reshape(B, L, G, R, Dh) * (Dh ** -0.5)
    s = jnp.einsum('blgrd,bcgd->bgrlc', qg, kc)
    (p,) = sink_softmax([s], sink.reshape(G, R)[None, :, :, None, None])
    return jnp.einsum('bgrlc,bcgd->blgrd', p, vc).reshape(B, L, H * Dh)


def chunk_gated_recurrence(q, k, v, log_decay, state0, reverse):
    if reverse:
        q, k, v, log_decay = [jnp.flip(t, axis=1) for t in (q, k, v, log_decay)]
    B, T, H, K = q.shape
    V = v.shape[-1]
    n = T // CHUNK
    qc, kc, gc = [t.reshape(B, n, CHUNK, H, K) for t in (q, k, log_decay)]
    vc = v.reshape(B, n, CHUNK, H, V)
    G = jnp.cumsum(gc, axis=2)
    G_last = G[:, :, -1:]
    q_dec = qc * jnp.exp(G)
    k_inv = kc * jnp.exp(-G)
    k_tail = kc * jnp.exp(G_last - G)
    causal = jnp.tril(jnp.ones((CHUNK, CHUNK), dtype=bool))
    A = jnp.where(causal, jnp.einsum('bnihk,bnjhk->bnhij', q_dec, k_inv), 0.0)
    o = jnp.einsum('bnhij,bnjhv->bnihv', A, vc)
    kv = jnp.einsum('bnjhk,bnjhv->nbhkv', k_tail, vc)
    decay = jnp.moveaxis(jnp.exp(G_last[:, :, 0]), 1, 0)

    def step(S, inp):
        d, kv_n = inp
        return d[..., None] * S + kv_n, S

    S_final, S_start = lax.scan(step, state0, (decay, kv))
    o = o + jnp.einsum('bnihk,nbhkv->bnihv', q_dec, S_start)
    o = o.reshape(B, T, H, V)
    if reverse:
        o = jnp.flip(o, axis=1)
    return o, S_final


def rwkv7_scan(r, w, k, v, a, b, state0, reverse):
    def step(S, inp):
        r_t, w_t, k_t, v_t, a_t, b_t = inp
        sa = jnp.einsum('bhvk,bhk->bhv', S, a_t)
        S = S * w_t[:, :, None, :] + sa[..., None] * b_t[:, :, None, :] + v_t[..., None] * k_t[:, :, None, :]
        return S, jnp.einsum('bhvk,bhk->bhv', S, r_t)

    xs = tuple(jnp.moveaxis(t, 1, 0) for t in (r, w, k, v, a, b))
    S_final, o = lax.scan(step, state0, xs, reverse=reverse)
    return jnp.moveaxis(o, 0, 1), S_final


def bidirectional_prefix_scan(scan_fn, ctx_fwd, ctx_bwd, lat_fwd, lat_bwd, state0):
    oc_f, s_f = scan_fn(*ctx_fwd, state0, False)
    oc_b, s_b = scan_fn(*ctx_bwd, state0, True)
    ol_f, _ = scan_fn(*lat_fwd, s_f, False)
    ol_b, _ = scan_fn(*lat_bwd, s_b, True)
    return oc_f + oc_b, ol_f + ol_b


def centred_token_shift(p, mu_prev, mu_next):
    zero = jnp.zeros_like(p[:, :1])
    prev = jnp.concatenate([zero, p[:, :-1]], axis=1)
    nxt = jnp.concatenate([p[:, 1:], zero], axis=1)
    return p + mu_prev * (prev - p) + mu_next * (nxt - p)


def even_mixer(h, hc, w_in, w_out, attn_sink, hgrn_norm, hgrn_lb, rope_cos, rope_sin, need_ctx):
    B, S, _ = h.shape
    L = hc.shape[1]
    T = L + S
    ctx_sl, lat_sl = slice(0, L), slice(L, T)
    proj = (jnp.concatenate([hc, h], axis=1) @ w_in).astype(jnp.float32)
    qa, ka, va, qh, ih, f_fw, f_bw, gh = split_last(proj, EVEN_COLS)
    qa = to_heads(qa, ATT_HEADS)
    ka = to_heads(ka, ATT_KV_HEADS)
    va = to_heads(va, ATT_KV_HEADS)
    att = window_sink_attention(apply_axial_rope(qa[:, lat_sl], rope_cos, rope_sin),
                                apply_axial_rope(ka[:, lat_sl], rope_cos, rope_sin),
                                va[:, lat_sl], ka[:, ctx_sl], va[:, ctx_sl], attn_sink)
    qh = to_heads(jax.nn.silu(qh), HGRN_HEADS)
    ih = to_heads(ih, HGRN_HEADS)
    f_fw = to_heads(hgrn_lb + (1.0 - hgrn_lb) * jax.nn.sigmoid(f_fw), HGRN_HEADS)
    f_bw = to_heads(hgrn_lb + (1.0 - hgrn_lb) * jax.nn.sigmoid(f_bw), HGRN_HEADS)

    def hg_in(f, sl):
        return (qh[:, sl], 1.0 - f[:, sl], ih[:, sl], jnp.log(f[:, sl]))

    state0 = jnp.zeros((B, HGRN_HEADS, HGRN_DK, HGRN_DV), jnp.float32)
    o_ctx, o_lat = bidirectional_prefix_scan(chunk_gated_recurrence, hg_in(f_fw, ctx_sl), hg_in(f_bw, ctx_sl),
                                             hg_in(f_fw, lat_sl), hg_in(f_bw, lat_sl), state0)
    out_gate = jax.nn.silu(gh)

    def hg_out(o, sl):
        return rmsnorm(o, hgrn_norm).reshape(B, -1, HGRN_V) * out_gate[:, sl]

    y = jnp.concatenate([att, hg_out(o_lat, lat_sl)], axis=-1).astype(h.dtype) @ w_out
    if not need_ctx:
        return y, None
    att_c = context_sink_attention(qa[:, ctx_sl], ka[:, ctx_sl], va[:, ctx_sl], attn_sink)
    yc = jnp.concatenate([att_c, hg_out(o_ctx, ctx_sl)], axis=-1).astype(h.dtype) @ w_out
    return y, yc


def odd_mixer(h, hc, w_in, w_out, gla_gate_up_f, gla_gate_up_b, gla_gate_bias_f, gla_gate_bias_b, gla_norm,
              rwkv_mu_prev, rwkv_mu_next, rwkv_w0_f, rwkv_w0_b, rwkv_w2_f, rwkv_w2_b, rwkv_a0, rwkv_a2,
              rwkv_g2, rwkv_k_k, rwkv_k_a, rwkv_r_k, rwkv_ln_w, rwkv_ln_b, need_ctx):
    B, S, _ = h.shape
    L = hc.shape[1]
    T = L + S
    ctx_sl, lat_sl = slice(0, L), slice(L, T)
    proj = (jnp.concatenate([hc, h], axis=1) @ w_in).astype(jnp.float32)
    gla_p, rwkv_p = proj[..., :GLA_IN], proj[..., GLA_IN:]
    gq, gk, gv, gd_f, gd_b, gr = split_last(gla_p, GLA_COLS)
    gq = to_heads(gq, GLA_HEADS) * (GLA_DK ** -0.5)
    gk = to_heads(gk, GLA_HEADS)
    gv = to_heads(gv, GLA_HEADS)
    lg_f = to_heads(jax.nn.log_sigmoid(gd_f @ gla_gate_up_f + gla_gate_bias_f) / GLA_GATE_NORMALIZER, GLA_HEADS)
    lg_b = to_heads(jax.nn.log_sigmoid(gd_b @ gla_gate_up_b + gla_gate_bias_b) / GLA_GATE_NORMALIZER, GLA_HEADS)

    def gla_in(lg, sl):
        return (gq[:, sl], gk[:, sl], gv[:, sl], lg[:, sl])

    gstate0 = jnp.zeros((B, GLA_HEADS, GLA_DK, GLA_DV), jnp.float32)
    go_ctx, go_lat = bidirectional_prefix_scan(chunk_gated_recurrence, gla_in(lg_f, ctx_sl), gla_in(lg_b, ctx_sl),
                                               gla_in(lg_f, lat_sl), gla_in(lg_b, lat_sl), gstate0)
    gla_gate = jax.nn.silu(gr)

    def gla_out(o, sl):
        return rmsnorm(o, gla_norm).reshape(B, -1, GLA_V) * gla_gate[:, sl]

    rw = jnp.concatenate([centred_token_shift(rwkv_p[:, ctx_sl], rwkv_mu_prev, rwkv_mu_next),
                          centred_token_shift(rwkv_p[:, lat_sl], rwkv_mu_prev, rwkv_mu_next)], axis=1)
    rr, rk, rv, wd_f, wd_b, ad, gd = split_last(rw, RWKV_COLS)

    def decay(w0, wd, w2):
        wl = -jax.nn.softplus(-(w0 + jnp.tanh(wd) @ w2)) - 0.5
        return to_heads(jnp.exp(-jnp.exp(wl)), RWKV_HEADS)

    dec_f = decay(rwkv_w0_f, wd_f, rwkv_w2_f)
    dec_b = decay(rwkv_w0_b, wd_b, rwkv_w2_b)
    a = jax.nn.sigmoid(rwkv_a0 + ad @ rwkv_a2)
    g_out = jax.nn.sigmoid(gd) @ rwkv_g2
    kk = to_heads(rk * rwkv_k_k, RWKV_HEADS)
    kk = kk * lax.rsqrt(jnp.sum(kk * kk, axis=-1, keepdims=True) + 1e-12)
    k_mod = to_heads(rk * (1.0 + (a - 1.0) * rwkv_k_a), RWKV_HEADS)
    r = to_heads(rr, RWKV_HEADS)
    v = to_heads(rv, RWKV_HEADS)
    b_vec = kk * to_heads(a, RWKV_HEADS)

    def rw_in(dec, sl):
        return (r[:, sl], dec[:, sl], k_mod[:, sl], v[:, sl], -kk[:, sl], b_vec[:, sl])

    rstate0 = jnp.zeros((B, RWKV_HEADS, RWKV_N, RWKV_N), jnp.float32)
    ro_ctx, ro_lat = bidirectional_prefix_scan(rwkv7_scan, rw_in(dec_f, ctx_sl), rw_in(dec_b, ctx_sl),
                                               rw_in(dec_f, lat_sl), rw_in(dec_b, lat_sl), rstate0)

    def rwkv_out(o, sl):
        mu = jnp.mean(o, axis=-1, keepdims=True)
        var = jnp.mean(jnp.square(o - mu), axis=-1, keepdims=True)
        on = ((o - mu) * lax.rsqrt(var + RWKV_LN_EPS)).reshape(B, -1, RWKV_C) * rwkv_ln_w + rwkv_ln_b
        bonus = (jnp.sum(r[:, sl] * k_mod[:, sl] * rwkv_r_k, axis=-1, keepdims=True) * v[:, sl]).reshape(B, -1, RWKV_C)
        return (on + bonus) * g_out[:, sl]

    y = jnp.concatenate([gla_out(go_lat, lat_sl), rwkv_out(ro_lat, lat_sl)], axis=-1).astype(h.dtype) @ w_out
    if not need_ctx:
        return y, None
    yc = jnp.concatenate([gla_out(go_ctx, ctx_sl), rwkv_out(ro_ctx, ctx_sl)], axis=-1).astype(h.dtype) @ w_out
    return y, yc


def swiglu(h, w_gate, w_up, w_down):
    return (jax.nn.silu(h @ w_gate) * (h @ w_up)) @ w_down


def moe_swiglu(h, router, w_gate, w_up, w_down):
    logits = (h @ router).astype(jnp.float32)
    top_val, top_idx = lax.top_k(logits, TOP_K)
    weights = jax.nn.softmax(top_val, axis=-1)
    gates = jnp.sum(jax.nn.one_hot(top_idx, N_EXPERTS, dtype=jnp.float32) * weights[..., None], axis=-2)
    out = jnp.zeros(h.shape, jnp.float32)
    for e in range(N_EXPERTS):
        hidden = jax.nn.silu(h @ w_gate[e]) * (h @ w_up[e])
        out = out + gates[..., e:e + 1] * (hidden @ w_down[e])
    return out.astype(h.dtype)


def setup_inputs(seed: int = 0) -> dict:
    key = jax.random.key(seed)
    keys = jax.random.split(key, 80)
    counter = [0]

    def nk():
        k = keys[counter[0]]
        counter[0] += 1
        return k

    def nrm(shape, scale):
        return jax.random.normal(nk(), shape, jnp.float32) * scale

    def unif(shape, lo, hi):
        return jax.random.uniform(nk(), shape, jnp.float32, lo, hi)

    def gain(n):
        return 1.0 + nrm((n,), 0.05)

    def lin(fan_in, fan_out):
        return nrm((fan_in, fan_out), fan_in ** -0.5)

    D = D_MODEL
    return {
        'x': nrm((BATCH, SEQ, D), 1.0),
        'c': nrm((BATCH, D), 1.0),
        'ctx': nrm((BATCH, CTX_LEN, D), 1.0),
        'c_ctx': nrm((D,), 1.0),
        'hgrn_lb_logits': nrm((N_HGRN_LAYERS + 1, HGRN_K), 0.5),
        'l0_ada_w': nrm((D, 6 * D), 0.5 * D ** -0.5),
        'l0_ada_b': nrm((6 * D,), 0.02),
        'l0_norm_mix_pre': gain(D),
        'l0_norm_mix_post': gain(D),
        'l0_norm_ffn_pre': gain(D),
        'l0_norm_ffn_post': gain(D),
        'l0_w_in': lin(D, EVEN_IN),
        'l0_w_out': lin(EVEN_OUT, D),
        'l0_attn_sink': nrm((ATT_HEADS,), 1.0),
        'l0_hgrn_norm': gain(HGRN_DV),
        'l0_ffn_w_gate': lin(D, D_FF),
        'l0_ffn_w_up': lin(D, D_FF),
        'l0_ffn_w_down': lin(D_FF, D),
        'l1_ada_w': nrm((D, 6 * D), 0.5 * D ** -0.5),
        'l1_ada_b': nrm((6 * D,), 0.02),
        'l1_norm_mix_pre': gain(D),
        'l1_norm_mix_post': gain(D),
        'l1_norm_ffn_pre': gain(D),
        'l1_norm_ffn_post': gain(D),
        'l1_w_in': lin(D, ODD_IN),
        'l1_w_out': lin(ODD_OUT, D),
        'l1_gla_gate_up_f': lin(GLA_GATE_RANK, GLA_K),
        'l1_gla_gate_up_b': lin(GLA_GATE_RANK, GLA_K),
        'l1_gla_gate_bias_f': nrm((GLA_K,), 0.1),
        'l1_gla_gate_bias_b': nrm((GLA_K,), 0.1),
        'l1_gla_norm': gain(GLA_DV),
        'l1_rwkv_mu_prev': unif((RWKV_IN,), 0.0, 0.5),
        'l1_rwkv_mu_next': unif((RWKV_IN,), 0.0, 0.5),
        'l1_rwkv_w0_f': unif((RWKV_C,), -6.0, -1.0),
        'l1_rwkv_w0_b': unif((RWKV_C,), -6.0, -1.0),
        'l1_rwkv_w2_f': nrm((RWKV_DECAY_RANK, RWKV_C), 0.5 * RWKV_DECAY_RANK ** -0.5),
        'l1_rwkv_w2_b': nrm((RWKV_DECAY_RANK, RWKV_C), 0.5 * RWKV_DECAY_RANK ** -0.5),
        'l1_rwkv_a0': nrm((RWKV_C,), 0.5),
        'l1_rwkv_a2': lin(RWKV_A_RANK, RWKV_C),
        'l1_rwkv_g2': lin(RWKV_GATE_RANK, RWKV_C),
        'l1_rwkv_k_k': 0.85 + nrm((RWKV_C,), 0.05),
        'l1_rwkv_k_a': 1.0 + nrm((RWKV_C,), 0.05),
        'l1_rwkv_r_k': nrm((RWKV_HEADS, RWKV_N), 0.1),
        'l1_rwkv_ln_w': gain(RWKV_C),
        'l1_rwkv_ln_b': nrm((RWKV_C,), 0.02),
        'l1_moe_router': lin(D, N_EXPERTS),
        'l1_moe_w_gate': nrm((N_EXPERTS, D, D_FF_EXPERT), D ** -0.5),
        'l1_moe_w_up': nrm((N_EXPERTS, D, D_FF_EXPERT), D ** -0.5),
        'l1_moe_w_down': nrm((N_EXPERTS, D_FF_EXPERT, D), D_FF_EXPERT ** -0.5),
    }


def reference(x, c, ctx, c_ctx, hgrn_lb_logits,
              l0_ada_w, l0_ada_b, l0_norm_mix_pre, l0_norm_mix_post, l0_norm_ffn_pre, l0_norm_ffn_post,
              l0_w_in, l0_w_out, l0_attn_sink, l0_hgrn_norm, l0_ffn_w_gate, l0_ffn_w_up, l0_ffn_w_down,
              l1_ada_w, l1_ada_b, l1_norm_mix_pre, l1_norm_mix_post, l1_norm_ffn_pre, l1_norm_ffn_post,
              l1_w_in, l1_w_out, l1_gla_gate_up_f, l1_gla_gate_up_b, l1_gla_gate_bias_f, l1_gla_gate_bias_b,
              l1_gla_norm, l1_rwkv_mu_prev, l1_rwkv_mu_next, l1_rwkv_w0_f, l1_rwkv_w0_b, l1_rwkv_w2_f,
              l1_rwkv_w2_b, l1_rwkv_a0, l1_rwkv_a2, l1_rwkv_g2, l1_rwkv_k_k, l1_rwkv_k_a, l1_rwkv_r_k,
              l1_rwkv_ln_w, l1_rwkv_ln_b, l1_moe_router, l1_moe_w_gate, l1_moe_w_up, l1_moe_w_down):
    S = x.shape[1]
    L = ctx.shape[1]
    rows = S // GRID_W
    rope_cos, rope_sin = axial_rope_tables(rows)
    hgrn_lb = jnp.cumsum(jax.nn.softmax(hgrn_lb_logits.astype(jnp.float32), axis=0), axis=0)
    layers = [
        dict(ada_w=l0_ada_w, ada_b=l0_ada_b,
             norms=(l0_norm_mix_pre, l0_norm_mix_post, l0_norm_ffn_pre, l0_norm_ffn_post),
             mixer=dict(w_in=l0_w_in, w_out=l0_w_out, attn_sink=l0_attn_sink, hgrn_norm=l0_hgrn_norm),
             ffn=(l0_ffn_w_gate, l0_ffn_w_up, l0_ffn_w_down)),
        dict(ada_w=l1_ada_w, ada_b=l1_ada_b,
             norms=(l1_norm_mix_pre, l1_norm_mix_post, l1_norm_ffn_pre, l1_norm_ffn_post),
             mixer=dict(w_in=l1_w_in, w_out=l1_w_out, gla_gate_up_f=l1_gla_gate_up_f,
                        gla_gate_up_b=l1_gla_gate_up_b, gla_gate_bias_f=l1_gla_gate_bias_f,
                        gla_gate_bias_b=l1_gla_gate_bias_b, gla_norm=l1_gla_norm,
                        rwkv_mu_prev=l1_rwkv_mu_prev, rwkv_mu_next=l1_rwkv_mu_next,
                        rwkv_w0_f=l1_rwkv_w0_f, rwkv_w0_b=l1_rwkv_w0_b, rwkv_w2_f=l1_rwkv_w2_f,
                        rwkv_w2_b=l1_rwkv_w2_b, rwkv_a0=l1_rwkv_a0, rwkv_a2=l1_rwkv_a2, rwkv_g2=l1_rwkv_g2,
                        rwkv_k_k=l1_rwkv_k_k, rwkv_k_a=l1_rwkv_k_a, rwkv_r_k=l1_rwkv_r_k,
                        rwkv_ln_w=l1_rwkv_ln_w, rwkv_ln_b=l1_rwkv_ln_b),
             ffn=(l1_moe_router, l1_moe_w_gate, l1_moe_w_up, l1_moe_w_down)),
    ]
    xc = ctx
    for l in range(DEPTH):
        p = layers[l]
        need_ctx = l < DEPTH - 1
        n_mix_pre, n_mix_post, n_ffn_pre, n_ffn_post = p['norms']
        sh1, sc1, g1, sh2, sc2, g2 = [m[:, None, :] for m in adaln(c, p['ada_w'], p['ada_b'])]
        sh1c, sc1c, g1c, sh2c, sc2c, g2c = adaln(c_ctx, p['ada_w'], p['ada_b'])
        h = rmsnorm(x, n_mix_pre) * (1.0 + sc1) + sh1
        hc = rmsnorm(xc, n_mix_pre) * (1.0 + sc1c) + sh1c
        if l % 2 == 0:
            y, yc = even_mixer(h, hc, hgrn_lb=hgrn_lb[l // 2], rope_cos=rope_cos, rope_sin=rope_sin,
                               need_ctx=need_ctx, **p['mixer'])
            ffn = swiglu
        else:
            y, yc = odd_mixer(h, hc, need_ctx=need_ctx, **p['mixer'])
            ffn = moe_swiglu
        x = x + g1 * rmsnorm(y, n_mix_post)
        h = rmsnorm(x, n_ffn_pre) * (1.0 + sc2) + sh2
        if need_ctx:
            xc = xc + g1c * rmsnorm(yc, n_mix_post)
            hc = rmsnorm(xc, n_ffn_pre) * (1.0 + sc2c) + sh2c
            f = ffn(jnp.concatenate([hc, h], axis=1), *p['ffn'])
            xc = xc + g2c * rmsnorm(f[:, :L], n_ffn_post)
            x = x + g2 * rmsnorm(f[:, L:], n_ffn_post)
        else:
            x = x + g2 * rmsnorm(ffn(h, *p['ffn']), n_ffn_post)
    return x
```

```python
import contextlib
import numpy as np
import ml_dtypes
import concourse.bass as bass
import concourse.mybir as mybir
from concourse.bass_utils import run_bass_kernel_spmd

F32 = mybir.dt.float32
BF16 = mybir.dt.bfloat16
U8 = mybir.dt.uint8
AF = mybir.ActivationFunctionType
ALU = mybir.AluOpType
NCORES = 8
NORM_EPS = 1e-6
DEBUG = False
DEBUG_BI = 0
RECYCLE_SEMS = True
ROUTER_ON = True


class Cfg:
    def __init__(self, S=16384, L=256, D=2048, D_FF=5632, N_EXP=8, D_FF_E=7168):
        self.S, self.L, self.D, self.D_FF, self.N_EXP, self.D_FF_E = S, L, D, D_FF, N_EXP, D_FF_E
        self.T = S + L
        self.KC = D // 128
        self.ntl = S // NCORES
        self.ntc = L // NCORES
        self.ntok = self.ntl + self.ntc


class Tok:
    __slots__ = ("sem", "count", "closed", "deps")

    def __init__(self, sem, count):
        self.sem, self.count, self.closed, self.deps = sem, count, False, []


class Buf:
    def __init__(self, name, t=None, excl=False, accum=False):
        self.name, self.t, self.excl, self.accum = name, t, excl, accum
        self.writers, self.readers = {}, {}
        self.dma_sem, self.dma_tok = None, None

    def __getitem__(self, idx):
        return self.t[idx]


class _Rec:
    def __getattr__(self, name):
        def f(*a, **k):
            self.call = (name, a, k)
        return f


class Eng:
    def __init__(self, name, sem):
        self.name, self.sem, self.count, self.items, self.seen = name, sem, 0, [], {}


class Sched:
    ENGS = ("sync", "scalar", "vector", "gpsimd", "tensor")

    def __init__(self, nc, stack):
        self.nc, self.stack, self.root = nc, stack, stack
        self.nsem = 0
        self.free_sems = []
        self._init_mem()
        self.eng = {n: Eng(n, self.new_sem("e_" + n)) for n in self.ENGS}
        self.dma_keys = []
        self.nbuf = 0

    def new_sem(self, name=None):
        self.nsem += 1
        return self.root.enter_context(self.nc.semaphore(name or f"s{self.nsem}"))

    ARENA_WORDS = 45056

    def _init_mem(self):
        self.arena = self.root.enter_context(self.nc.sbuf_tensor("arena", [128, self.ARENA_WORDS], F32))
        self.banks = [self.root.enter_context(self.nc.psum_tensor(f"bank{i}", [128, 512], F32)) for i in range(8)]
        self.aoff, self.pidx = 0, 0

    def sbuf(self, name, shape, dt=F32):
        esz = {F32: 4, BF16: 2, U8: 1}[dt]
        nel = int(np.prod(shape[1:]))
        words = (nel * esz + 3) // 4
        assert self.aoff + words <= self.ARENA_WORDS, f"SBUF arena overflow at {name}: {self.aoff}+{words}"
        ap = self.arena[:, self.aoff:self.aoff + words]
        self.aoff += words
        if dt != F32:
            ap = ap.bitcast(dt)[:, 0:nel]
        if len(shape) == 3:
            ap = ap.rearrange("p (a b) -> p a b", b=shape[2])
        if shape[0] < 128:
            ap = ap[0:shape[0]]
        return Buf(name, ap)

    def psum(self, name, shape=(128, 512), dt=F32):
        assert self.pidx < 8, "out of PSUM banks"
        b = Buf(name, self.banks[self.pidx][:], excl=True)
        self.pidx += 1
        return b

    def mark(self):
        return (self.aoff, self.pidx, len(self.dma_keys))

    def reset(self, m):
        self.aoff, self.pidx = m[0], m[1]
        for key in self.dma_keys[m[2]:]:
            if RECYCLE_SEMS:
                self.free_sems.append((key.dma_sem, key.dma_tok.count))
            key.dma_sem, key.dma_tok = None, None
        del self.dma_keys[m[2]:]

    def dram(self, name, shape, dt, kind="Internal"):
        t = self.nc.dram_tensor(name, list(shape), dt, kind=kind)
        return Buf(name, t.ap(), accum=True)

    def _wait(self, E, tok, raw=False):
        if tok.sem is E.sem and (not raw or E.name == "tensor"):
            return
        tok.closed = True
        k = id(tok.sem)
        if E.seen.get(k, 0) >= tok.count:
            return
        E.seen[k] = tok.count
        E.items.append(("wait", tok.sem, tok.count))

    def _hazards(self, E, reads, writes, skip=None):
        deps = []
        for b in reads:
            if b.excl:
                continue
            for t in b.writers.values():
                if t is not skip:
                    self._wait(E, t, raw=True)
                    deps.append(t)
        for b in list(writes) + [b for b in reads if b.excl]:
            if not b.accum:
                for t in b.writers.values():
                    if t is not skip:
                        self._wait(E, t, raw=(b.excl and b in reads))
                        deps.append(t)
            for t in b.readers.values():
                if t is not skip:
                    self._wait(E, t)
                    deps.append(t)
        return deps

    def _record(self, tok, reads, writes):
        k = id(tok.sem)
        for b in reads:
            if b.excl:
                b.writers, b.readers = {k: tok}, {}
            else:
                b.readers[k] = tok
        for b in writes:
            if b.accum:
                b.writers[k] = tok
                b.readers = {}
            else:
                b.writers, b.readers = {k: tok}, {}

    def op(self, eng, fn, reads=(), writes=()):
        E = self.eng[eng]
        self._hazards(E, reads, writes)
        E.count += 1
        tok = Tok(E.sem, E.count)
        rec = _Rec()
        fn(rec)
        E.items.append(("op", rec.call))
        self._record(tok, reads, writes)

    def dma(self, q, out, in_, key, reads=(), writes=(), **kw):
        E = self.eng[q]
        cur = key.dma_tok if (key.dma_tok is not None and not key.dma_tok.closed) else None
        deps = self._hazards(E, reads, writes, skip=cur)
        if cur is not None:
            for t in cur.deps:
                self._wait(E, t)
            cur.deps.extend(deps)
        if key.dma_sem is None:
            if self.free_sems:
                key.dma_sem, cnt = self.free_sems.pop()
                key.dma_tok = Tok(key.dma_sem, cnt)
                key.dma_tok.closed = True
            else:
                key.dma_sem = self.new_sem()
            self.dma_keys.append(key)
        t = key.dma_tok
        if t is None or t.closed:
            if t is not None:
                self._wait(E, t)
            t = Tok(key.dma_sem, t.count if t is not None else 0)
            t.deps = list(deps)
            key.dma_tok = t
        t.count += 16
        E.items.append(("dma", out, in_, key.dma_sem, kw))
        self._record(t, reads, writes)

    def load(self, q, dst, dst_ap, src_ap, src=None, **kw):
        self.dma(q, dst_ap, src_ap, key=dst, reads=([src] if src is not None else []), writes=[dst], **kw)

    def store(self, q, dst, dst_ap, src, src_ap, **kw):
        self.dma(q, dst_ap, src_ap, key=src, reads=[src], writes=[dst], **kw)

    def finish(self):
        E = self.eng["sync"]
        for key in self.dma_keys:
            if key.dma_tok is not None:
                self._wait(E, key.dma_tok)
        for n in self.ENGS:
            if n != "sync" and self.eng[n].count:
                self._wait(E, Tok(self.eng[n].sem, self.eng[n].count))

    def emit(self):
        self.finish()
        with self.nc.Block() as block:
            for n in self.ENGS:
                E = self.eng[n]

                def body(e, E=E):
                    for it in E.items:
                        if it[0] == "wait":
                            e.wait_ge(it[1], it[2])
                        elif it[0] == "op":
                            nm, a, k = it[1]
                            getattr(e, nm)(*a, **k).then_inc(E.sem, 1)
                        else:
                            e.dma_start(out=it[1], in_=it[2], **it[4]).then_inc(it[3], 16)

                getattr(block, n)(body)


def new_prog():
    nc = bass.Bass("TRN2", target_bir_lowering=False)
    stack = contextlib.ExitStack()
    return nc, stack, Sched(nc, stack)


def run_prog(nc, stack, S, in_maps):
    S.emit()
    stack.close()
    res = run_bass_kernel_spmd(nc, in_maps, core_ids=list(range(NCORES)))
    return res.results


def make_ones(S, name="ones", dt=F32):
    ones = S.sbuf(name, (128, 128), dt)
    S.op("gpsimd", lambda e: e.memset(ones[:], 1.0), writes=[ones])
    return ones


def load_const(S, name, shape, dram_ap, dt=F32, q="sync"):
    b = S.sbuf(name, shape, dt)
    S.load(q, b, b[:], dram_ap)
    return b


def mod_scalars(S, cfg, mod, gain, v_scale, v_shift, tag):
    KC = cfg.KC
    A, B = [], []
    for r in range(2):
        a = S.sbuf(f"A_{tag}{r}", (128, KC))
        bb = S.sbuf(f"B_{tag}{r}", (128, KC))
        S.op("vector", lambda e, a=a, r=r: e.tensor_scalar(
            out=a[:], in0=mod[:, v_scale * KC:(v_scale + 1) * KC, r], scalar1=1.0, scalar2=float(cfg.D) ** 0.5,
            op0=ALU.add, op1=ALU.mult), reads=[mod], writes=[a])
        S.op("vector", lambda e, a=a: e.tensor_tensor(out=a[:], in0=a[:], in1=gain[:], op=ALU.mult), reads=[a, gain], writes=[a])
        S.op("vector", lambda e, bb=bb, r=r: e.tensor_copy(out=bb[:], in_=mod[:, v_shift * KC:(v_shift + 1) * KC, r]),
             reads=[mod], writes=[bb])
        A.append(a)
        B.append(bb)
    return A, B


def rms_stats(S, cfg, x, n, sq, ones, ps, rstd, nchunks=None, c0=0):
    nch = nchunks or cfg.KC
    Dn = nch * 128
    for c in range(nch):
        q = sq.get()
        S.op("scalar", lambda e: e.activation(out=q[:, :n], in_=x[:, c0 + c, :n], func=AF.Square), reads=[x], writes=[q])
        S.op("tensor", lambda e: e.matmul(ps[:, :n], ones[:], q[:, :n], start=(c == 0), stop=(c == nch - 1)),
             reads=[ones, q], writes=[ps])
    S.op("vector", lambda e: e.tensor_scalar(out=rstd[:, :n], in0=ps[:, :n], scalar1=NORM_EPS * Dn, scalar2=None,
                                             op0=ALU.add), reads=[ps], writes=[rstd])
    S.op("scalar", lambda e: e.activation(out=rstd[:, :n], in_=rstd[:, :n], func=AF.Sqrt), reads=[rstd], writes=[rstd])
    S.op("vector", lambda e: e.reciprocal(out=rstd[:, :n], in_=rstd[:, :n]), reads=[rstd], writes=[rstd])


def norm_mod_apply(S, cfg, x, n, rstd, A, B, out, tmp):
    for c in range(cfg.KC):
        t = tmp.get()
        S.op("vector", lambda e: e.scalar_tensor_tensor(out=t[:, :n], in0=x[:, c, :n], scalar=A[:, c:c + 1],
                                                        in1=rstd[:, :n], op0=ALU.mult, op1=ALU.mult),
             reads=[x, A, rstd], writes=[t])
        S.op("gpsimd", lambda e: e.tensor_scalar(out=out[:, c, :n], in0=t[:, :n], scalar1=B[:, c:c + 1],
                                                 scalar2=None, op0=ALU.add), reads=[t, B], writes=[out])


def resid_norm_add(S, cfg, x, y, n, rstd, G, tmp):
    for c in range(cfg.KC):
        t = tmp.get()
        S.op("gpsimd", lambda e: e.tensor_tensor(out=t[:, :n], in0=y[:, c, :n], in1=rstd[:, :n], op=ALU.mult),
             reads=[y, rstd], writes=[t])
        S.op("vector", lambda e: e.scalar_tensor_tensor(out=x[:, c, :n], in0=t[:, :n], scalar=G[:, c:c + 1], in1=x[:, c, :n],
                                                        op0=ALU.mult, op1=ALU.add), reads=[t, G, x], writes=[x])


def gate_scalars(S, cfg, mod, gain, v_gate, tag):
    KC = cfg.KC
    G = []
    for r in range(2):
        g = S.sbuf(f"G_{tag}{r}", (128, KC))
        S.op("vector", lambda e: e.tensor_scalar(out=g[:], in0=mod[:, v_gate * KC:(v_gate + 1) * KC, r], scalar1=float(cfg.D) ** 0.5,
                                                 scalar2=None, op0=ALU.mult), reads=[mod], writes=[g])
        S.op("vector", lambda e: e.tensor_tensor(out=g[:], in0=g[:], in1=gain[:], op=ALU.mult), reads=[g, gain], writes=[g])
        G.append(g)
    return G


def token_blocks(cfg, bs=512):
    blocks = []
    for s in range(0, cfg.ntl, bs):
        blocks.append((s, min(bs, cfg.ntl - s), 0))
    blocks.append((cfg.ntl, cfg.ntc, 1))
    return blocks


def build_ada(cfg):
    nc, stack, S = new_prog()
    KC = cfg.KC
    NJ = 6 * KC // NCORES
    c2 = S.dram("c2", (128, KC, 2), F32, "ExternalInput")
    outs = []
    ones = None
    sT = S.sbuf("sT", (128, KC, 2))
    S.load("sync", sT, sT[:], c2[:])
    S.op("scalar", lambda e: e.activation(out=sT[:], in_=sT[:], func=AF.Silu), reads=[sT], writes=[sT])
    ps = [S.psum(f"ps{i}") for i in range(2)]
    for l in range(2):
        w = S.dram(f"w{l}", (128, KC, NJ * 128), F32, "ExternalInput")
        b = S.dram(f"b{l}", (128, NJ), F32, "ExternalInput")
        o = S.dram(f"mod{l}", (128, NJ, 2), F32, "ExternalOutput")
        if l == 0:
            wt_shared = S.sbuf("wt", (128, KC, NJ * 128))
        wt = wt_shared
        bt = S.sbuf(f"bt{l}", (128, NJ))
        ot = S.sbuf(f"ot{l}", (128, NJ, 2))
        for kc in range(KC):
            S.load("sync" if kc % 2 == 0 else "scalar", wt, wt[:, kc, :], w[:, kc, :])
        S.load("sync", bt, bt[:], b[:])
        for j in range(NJ):
            p = ps[j % 2]
            for kc in range(KC):
                S.op("tensor", lambda e, p=p, j=j, kc=kc, wt=wt: e.matmul(p[:, 0:2], wt[:, kc, j * 128:(j + 1) * 128], sT[:, kc, :],
                                                                      start=(kc == 0), stop=(kc == KC - 1)),
                     reads=[wt, sT], writes=[p])
            S.op("vector", lambda e, p=p, j=j, ot=ot, bt=bt: e.tensor_scalar(out=ot[:, j, :], in0=p[:, 0:2], scalar1=bt[:, j:j + 1],
                                                                           scalar2=None, op0=ALU.add),
                 reads=[p, bt], writes=[ot])
        S.store("sync", o, o[:], ot, ot[:])
    return nc, stack, S


def host_ada(cfg, inp):
    KC = cfg.KC
    NJ = 6 * KC // NCORES
    c2 = np.stack([inp["c"][0], inp["c_ctx"]], axis=-1)
    c2 = np.ascontiguousarray(c2.reshape(KC, 128, 2).transpose(1, 0, 2))
    maps = []
    for i in range(NCORES):
        m = {"c2": c2}
        for l in range(2):
            w = inp[f"l{l}_ada_w"][:, i * NJ * 128:(i + 1) * NJ * 128]
            m[f"w{l}"] = np.ascontiguousarray(w.reshape(KC, 128, NJ * 128).transpose(1, 0, 2))
            b = inp[f"l{l}_ada_b"][i * NJ * 128:(i + 1) * NJ * 128]
            m[f"b{l}"] = np.ascontiguousarray(b.reshape(NJ, 128).T)
        maps.append(m)
    return maps


def run_ada(cfg, inp):
    nc, stack, S = build_ada(cfg)
    res = run_prog(nc, stack, S, host_ada(cfg, inp))
    mods = []
    for l in range(2):
        mods.append(np.ascontiguousarray(np.concatenate([res[i][f"mod{l}"] for i in range(NCORES)], axis=1)))
    return mods


def build_pre(cfg):
    nc, stack, S = new_prog()
    KC = cfg.KC
    xT = S.dram("xT", (128, KC, cfg.ntok), F32, "ExternalInput")
    mod_d = S.dram("mod", (128, 6 * KC, 2), F32, "ExternalInput")
    gain_d = S.dram("gain", (128, KC), F32, "ExternalInput")
    hT = S.dram("hT", (128, KC, cfg.ntok), BF16, "ExternalOutput")
    mod = load_const(S, "mod_sb", (128, 6 * KC, 2), mod_d[:])
    gain = load_const(S, "gain_sb", (128, KC), gain_d[:])
    ones = make_ones(S)
    A, B = mod_scalars(S, cfg, mod, gain, 1, 0, "m")
    xb = [S.sbuf(f"xb{i}", (128, KC, 512)) for i in range(2)]
    sq = rot_sbuf(S, "sq", (128, 512))
    tmp = rot_sbuf(S, "tmp", (128, 512))
    hb = [S.sbuf(f"hb{i}", (128, KC, 512), BF16) for i in range(2)]
    rstd = S.sbuf("rstd", (128, 512))
    ps = S.psum("ps")
    for bi, (s0, n, kind) in enumerate(token_blocks(cfg)):
        x = xb[bi % 2]
        h = hb[bi % 2]
        S.load("sync", x, x[:, :, :n], xT[:, :, s0:s0 + n])
        rms_stats(S, cfg, x, n, sq, ones, ps, rstd)
        norm_mod_apply(S, cfg, x, n, rstd, A[kind], B[kind], h, tmp)
        S.store("sync", hT, hT[:, :, s0:s0 + n], h, h[:, :, :n])
    return nc, stack, S


def fm(a):
    D, n = a.shape
    return np.ascontiguousarray(a.reshape(D // 128, 128, n).transpose(1, 0, 2))


def unfm(a):
    p, C, n = a.shape
    return np.ascontiguousarray(a.transpose(1, 0, 2).reshape(C * 128, n))


def vec_fm(v):
    return np.ascontiguousarray(v.reshape(-1, 128).T)


def own_tokens_T(cfg, xT_lat, xT_ctx, i):
    return np.concatenate([xT_lat[:, i * cfg.ntl:(i + 1) * cfg.ntl], xT_ctx[:, i * cfg.ntc:(i + 1) * cfg.ntc]], axis=1)


def gather_tokens_T(cfg, per_core):
    lat = np.concatenate([a[:, :cfg.ntl] for a in per_core], axis=1)
    ctx = np.concatenate([a[:, cfg.ntl:] for a in per_core], axis=1)
    return ctx, lat


def run_pre(cfg, xT_lat, xT_ctx, mod, gain):
    nc, stack, S = build_pre(cfg)
    maps = [{"xT": fm(own_tokens_T(cfg, xT_lat, xT_ctx, i)), "mod": mod, "gain": vec_fm(gain)} for i in range(NCORES)]
    res = run_prog(nc, stack, S, maps)
    if DEBUG:
        global DBG
        DBG = res[0]
    ctx, lat = gather_tokens_T(cfg, [unfm(res[i]["hT"]) for i in range(NCORES)])
    return np.concatenate([ctx, lat], axis=1)


class Rot:
    def __init__(self, bufs):
        self.bufs, self.i = bufs, 0

    def get(self):
        b = self.bufs[self.i % len(self.bufs)]
        self.i += 1
        return b


def rot_sbuf(S, name, shape, dt=F32, n=2):
    return Rot([S.sbuf(f"{name}{i}", shape, dt) for i in range(n)])


def seq_blocks(cfg, bs=512):
    blocks = []
    for seg0, seglen in ((0, cfg.L), (cfg.L, cfg.S)):
        for s in range(0, seglen, bs):
            blocks.append((seg0 + s, min(bs, seglen - s), seg0, seglen))
    return blocks


def rev_pos(t0, n, seg0, seglen):
    return seg0 + seglen - (t0 - seg0) - n


def barrier(S):
    toks = [Tok(S.eng[n].sem, S.eng[n].count) for n in S.ENGS if S.eng[n].count]
    dtoks = [k.dma_tok for k in S.dma_keys if k.dma_tok is not None]
    for n in S.ENGS:
        E = S.eng[n]
        for t in toks + dtoks:
            if t.sem is not E.sem:
                S._wait(E, t)


def make_identity(S, ident_d, name="ident", dt=F32):
    return load_const(S, name, (128, 128), ident_d[:], dt)


def fm_group(S, ps, W, hb, g, n, KC):
    for kc in range(KC):
        S.op("tensor", lambda e, kc=kc: e.matmul(ps[:, :n], W[:, kc, g * 128:(g + 1) * 128], hb[:, kc, :n],
                                               start=(kc == 0), stop=(kc == KC - 1)), reads=[W, hb], writes=[ps])


def load_w_bf16(S, name, w_d, KC, ncols):
    W = S.sbuf(name, (128, KC, ncols), BF16)
    for kc in range(KC):
        S.load("gpsimd", W, W[:, kc, :], w_d[:, kc, :])
    return W


def chunk_scan(S, cfg, units, consts):
    T = cfg.T
    ident, zero1, cmask, mask_ui, m96 = consts["ident"], consts["zero1"], consts["cmask"], consts["mask_ui"], consts["m96"]
    nu = len(units)
    SB = 512
    for u, U in enumerate(units):
        K, V = U["K"], U["V"]
        U["in"] = {nm: rot_sbuf(S, f"u{u}_{nm}", (128, SB)) for nm in ("q", "k", "lw", "v")}
        U["L"] = S.sbuf(f"u{u}_L", (128, SB))
        U["Ep"] = S.sbuf(f"u{u}_Ep", (128, SB))
        U["En"] = S.sbuf(f"u{u}_En", (128, SB))
        U["qh"] = S.sbuf(f"u{u}_qh", (128, SB))
        U["kh"] = S.sbuf(f"u{u}_kh", (128, SB))
        U["AT"] = S.sbuf(f"u{u}_AT", (128, 128))
        U["ktok"] = S.sbuf(f"u{u}_ktok", (128, 128))
        U["vtok"] = S.sbuf(f"u{u}_vtok", (128, 128))
        U["ktokz"] = S.sbuf(f"u{u}_ktokz", (128, 128))
        U["kvd"] = S.sbuf(f"u{u}_kvd", (128, 128))
        U["Z"] = [S.sbuf(f"u{u}_Z{i}", (128, 128)) for i in range(2)]
        U["zi"] = 0
        U["osb"] = rot_sbuf(S, f"u{u}_osb", (128, SB))
        U["ps_g"] = S.psum(f"u{u}_psg")
        U["ps_t"] = S.psum(f"u{u}_pst")
        U["ps_o"] = S.psum(f"u{u}_pso")
        U["ps_kv"] = S.psum(f"u{u}_pskv")
        S.op("gpsimd", lambda e, U=U: e.memset(U["AT"][:], 0.0), writes=[U["AT"]])
        S.op("gpsimd", lambda e, U=U: e.memset(U["Z"][0][:], 0.0), writes=[U["Z"][0]])
    for t0 in range(0, T, SB):
        n = min(SB, T - t0)
        for U in units:
            K, V = U["K"], U["V"]
            cur = {}
            for nm in ("q", "k", "lw", "v"):
                b = U["in"][nm].get()
                P = V if nm == "v" else K
                S.load("sync", b, b[:P, :n], U[nm][:P, t0:t0 + n], src=U[nm])
                cur[nm] = b
            L, Ep, En, qh, kh = U["L"], U["Ep"], U["En"], U["qh"], U["kh"]
            S.op("vector", lambda e, L=L, cur=cur, K=K: e.tensor_tensor_scan(
                out=L[:K, :n], data0=cmask[:K, :n], data1=cur["lw"][:K, :n], initial=zero1[:K, 0:1],
                op0=ALU.mult, op1=ALU.add), reads=[cmask, cur["lw"], zero1], writes=[L])
            S.op("scalar", lambda e, L=L, Ep=Ep, K=K: e.activation(out=Ep[:K, :n], in_=L[:K, :n], func=AF.Exp),
                 reads=[L], writes=[Ep])
            S.op("vector", lambda e, Ep=Ep, En=En, K=K: e.reciprocal(out=En[:K, :n], in_=Ep[:K, :n]), reads=[Ep], writes=[En])
            S.op("gpsimd", lambda e, qh=qh, cur=cur, Ep=Ep, K=K: e.tensor_tensor(out=qh[:K, :n], in0=cur["q"][:K, :n], in1=Ep[:K, :n],
                                                                              op=ALU.mult), reads=[cur["q"], Ep], writes=[qh])
            S.op("gpsimd", lambda e, kh=kh, cur=cur, En=En, K=K: e.tensor_tensor(out=kh[:K, :n], in0=cur["k"][:K, :n], in1=En[:K, :n],
                                                                              op=ALU.mult), reads=[cur["k"], En], writes=[kh])
            U["cur"] = cur
            U["o"] = U["osb"].get()
            if DEBUG and t0 == 0 and U is units[0]:
                for nm, bb in (("L", L), ("Ep", Ep), ("qh", qh), ("kh", kh), ("lwin", cur["lw"]), ("qin", cur["q"])):
                    dd = S.dram("dbg_" + nm, (128, 512), F32, "ExternalOutput")
                    S.store("sync", dd, dd[:, :n], bb, bb[:, :n])
        for j in range(0, n, 128):
            for U in units:
                K, V = U["K"], U["V"]
                qh, kh, Ep, cur = U["qh"], U["kh"], U["Ep"], U["cur"]
                AT, ktok, vtok, kvd, o = U["AT"], U["ktok"], U["vtok"], U["kvd"], U["o"]
                ps_g, ps_t, ps_o, ps_kv = U["ps_g"], U["ps_t"], U["ps_o"], U["ps_kv"]
                js = slice(j, j + 128)
                S.op("tensor", lambda e, K=K, kh=kh, qh=qh, ps_g=ps_g, js=js: e.matmul(ps_g[:, 0:128], kh[:K, js], qh[:K, js], start=True, stop=True),
                     reads=[kh, qh], writes=[ps_g])
                S.op("vector", lambda e, AT=AT, ps_g=ps_g: e.copy_predicated(out=AT[:], mask=mask_ui[:], data=ps_g[:, 0:128]),
                     reads=[ps_g, mask_ui], writes=[AT])
                S.op("tensor", lambda e, K=K, kh=kh, ps_t=ps_t, js=js: e.matmul(ps_t[:, 0:K], kh[:K, js], ident[:K, :K], is_transpose=True, start=True, stop=True),
                     reads=[kh, ident], writes=[ps_t])
                S.op("tensor", lambda e, V=V, cur=cur, ps_t=ps_t, js=js: e.matmul(ps_t[:, 128:128 + V], cur["v"][:V, js], ident[:V, :V], is_transpose=True, start=True, stop=True),
                     reads=[cur["v"], ident], writes=[ps_t])
                S.op("scalar", lambda e, K=K, ktok=ktok, ps_t=ps_t: e.activation(out=ktok[:, :K], in_=ps_t[:, 0:K], func=AF.Copy),
                     reads=[ps_t], writes=[ktok])
                S.op("vector", lambda e, V=V, vtok=vtok, ps_t=ps_t: e.tensor_copy(out=vtok[:, :V], in_=ps_t[:, 128:128 + V]),
                     reads=[ps_t], writes=[vtok])
                ktz = U["ktokz"]
                S.op("vector", lambda e, K=K, ktz=ktz, ps_t=ps_t: e.tensor_scalar(out=ktz[64:128, :K], in0=ps_t[64:128, 0:K], scalar1=m96[64:128, 0:1],
                                                                              scalar2=None, op0=ALU.mult), reads=[ps_t, m96], writes=[ktz])
                S.op("tensor", lambda e, V=V, vtok=vtok, AT=AT, ps_o=ps_o: e.matmul(ps_o[:V, 0:128], vtok[:, :V], AT[:], start=True, stop=False),
                     reads=[vtok, AT], writes=[ps_o])
            for c in range(4):
                for U in units:
                    K, V = U["K"], U["V"]
                    qh, Ep = U["qh"], U["Ep"]
                    ktok, vtok, kvd, ps_o, ps_kv = U["ktok"], U["vtok"], U["kvd"], U["ps_o"], U["ps_kv"]
                    Zc, Zn = U["Z"][U["zi"]], U["Z"][1 - U["zi"]]
                    U["zi"] = 1 - U["zi"]
                    cs = slice(j + 32 * c, j + 32 * c + 32)
                    wc = j + 32 * c + 31
                    S.op("tensor", lambda e, K=K, V=V, Zc=Zc, qh=qh, ps_o=ps_o, cs=cs, c=c: e.matmul(
                        ps_o[:V, 32 * c:32 * c + 32], Zc[:K, :V], qh[:K, cs], start=False, stop=(c == 3)),
                        reads=[Zc, qh], writes=[ps_o])
                    if c < 3:
                        S.op("tensor", lambda e, K=K, V=V, ktok=ktok, vtok=vtok, ps_kv=ps_kv, c=c: e.matmul(
                            ps_kv[:K, :V], ktok[32 * c:32 * c + 32, :K], vtok[32 * c:32 * c + 32, :V], start=True, stop=True),
                            reads=[ktok, vtok], writes=[ps_kv])
                    else:
                        ktz = U["ktokz"]
                        S.op("tensor", lambda e, K=K, V=V, ktz=ktz, vtok=vtok, ps_kv=ps_kv: e.matmul(
                            ps_kv[:K, :V], ktz[64:128, :K], vtok[64:128, :V], start=True, stop=True),
                            reads=[ktz, vtok], writes=[ps_kv])
                    S.op("vector", lambda e, K=K, V=V, kvd=kvd, ps_kv=ps_kv, Ep=Ep, wc=wc: e.tensor_scalar(
                        out=kvd[:K, :V], in0=ps_kv[:K, :V], scalar1=Ep[:K, wc:wc + 1], scalar2=None, op0=ALU.mult),
                        reads=[ps_kv, Ep], writes=[kvd])
                    S.op("vector", lambda e, K=K, V=V, Zn=Zn, Zc=Zc, kvd=kvd, Ep=Ep, wc=wc: e.scalar_tensor_tensor(
                        out=Zn[:K, :V], in0=Zc[:K, :V], scalar=Ep[:K, wc:wc + 1], in1=kvd[:K, :V], op0=ALU.mult, op1=ALU.add),
                        reads=[Zc, Ep, kvd], writes=[Zn])
            for U in units:
                V = U["V"]
                o, ps_o = U["o"], U["ps_o"]
                S.op("scalar", lambda e, V=V, o=o, ps_o=ps_o, j=j: e.activation(out=o[:V, j:j + 128], in_=ps_o[:V, 0:128], func=AF.Copy),
                     reads=[ps_o], writes=[o])
        for U in units:
            V = U["V"]
            S.store("sync", U["out"], U["out"][:V, t0:t0 + n], U["o"], U["o"][:V, :n])


def scan_consts(S, cst_d):
    c = {}
    c["ident"] = load_const(S, "ident", (128, 128), cst_d["ident"][:])
    c["cmask"] = load_const(S, "cmask", (128, 512), cst_d["cmask"][:])
    c["mask_ui"] = load_const(S, "mask_ui", (128, 128), cst_d["mask_ui"][:], U8)
    c["m96"] = load_const(S, "m96", (128, 1), cst_d["m96"][:])
    if "mask_su" in cst_d:
        c["mask_su"] = load_const(S, "mask_su", (128, 128), cst_d["mask_su"][:], U8)
        c["mask_sl"] = load_const(S, "mask_sl", (128, 128), cst_d["mask_sl"][:], U8)
    z = S.sbuf("zero1", (128, 1))
    S.op("gpsimd", lambda e: e.memset(z[:], 0.0), writes=[z])
    c["zero1"] = z
    return c


def host_scan_consts(rwkv=False):
    ident = np.eye(128, dtype=np.float32)
    cmask = np.ones((128, 512), np.float32)
    cmask[:, ::32] = 0.0
    s = np.arange(128)[:, None]
    t = np.arange(128)[None, :]
    mask_ui = ((s // 32 == t // 32) & (t >= s)).astype(np.uint8)
    m96 = (np.arange(128) >= 96).astype(np.float32).reshape(128, 1)
    mask_su = ((s // 32 == t // 32) & (t > s)).astype(np.uint8)
    mask_sl = ((s // 32 == t // 32) & (t < s)).astype(np.uint8)
    d = {"ident": ident, "cmask": cmask, "mask_ui": mask_ui, "m96": m96}
    if rwkv:
        d.update({"mask_su": mask_su, "mask_sl": mask_sl})
    return d


def declare_scan_consts(S, rwkv=False):
    d = {"ident": S.dram("ident", (128, 128), F32, "ExternalInput"),
         "cmask": S.dram("cmask", (128, 512), F32, "ExternalInput"),
         "mask_ui": S.dram("mask_ui", (128, 128), U8, "ExternalInput"),
         "m96": S.dram("m96", (128, 1), F32, "ExternalInput")}
    if rwkv:
        d["mask_su"] = S.dram("mask_su", (128, 128), U8, "ExternalInput")
        d["mask_sl"] = S.dram("mask_sl", (128, 128), U8, "ExternalInput")
    return d


def rev_ap(ap):
    return ap[:, ::-1]


NG0 = 10


def build_mix0(cfg):
    nc, stack, S = new_prog()
    KC, T, L, SS = cfg.KC, cfg.T, cfg.L, cfg.S
    hT = S.dram("hT", (128, KC, T), BF16, "ExternalInput")
    w_d = S.dram("w", (128, KC, NG0 * 128), F32, "ExternalInput")
    cosT = S.dram("cosT", (128, SS), F32, "ExternalInput")
    sinT = S.dram("sinT", (128, SS), F32, "ExternalInput")
    lbl_d = S.dram("lbl", (128, 2), F32, "ExternalInput")
    hn_d = S.dram("hnorm", (128, 1), F32, "ExternalInput")
    sink_d = S.dram("sink", (128, 1), F32, "ExternalInput")
    mlo_d = S.dram("mask_lo", (128, 128), BF16, "ExternalInput")
    mup_d = S.dram("mask_up", (128, 128), BF16, "ExternalInput")
    cst_d = declare_scan_consts(S)
    out = S.dram("oT", (2, 128, T), BF16, "ExternalOutput")
    qr = S.dram("qr", (128, T), BF16)
    kr = S.dram("kr", (128, T), BF16)
    vt = S.dram("vt", (T, 128), BF16)
    names = ["q_f", "v_f", "k_f", "lw_f", "q_b", "v_b", "k_b", "lw_b", "gate", "o_f", "o_b"]
    scr = {nm: S.dram("scr_" + nm, (128, T), F32, "ExternalOutput" if DEBUG else "Internal") for nm in names}

    for _ph in (S.mark(),):
        W = load_w_bf16(S, "W", w_d, KC, NG0 * 128)
        lbl = load_const(S, "lbl", (128, 2), lbl_d[:])
        lb = S.sbuf("lb", (128, 1))
        oml = S.sbuf("oml", (128, 1))
        S.op("vector", lambda e: e.tensor_tensor(out=lb[:], in0=lbl[:, 0:1], in1=lbl[:, 1:2], op=ALU.subtract), reads=[lbl], writes=[lb])
        S.op("scalar", lambda e: e.activation(out=lb[:], in_=lb[:], func=AF.Sigmoid), reads=[lb], writes=[lb])
        S.op("vector", lambda e: e.tensor_scalar(out=oml[:], in0=lb[:], scalar1=-1.0, scalar2=1.0, op0=ALU.mult, op1=ALU.add),
             reads=[lb], writes=[oml])
        hbs = rot_sbuf(S, "hb", (128, KC, 512), BF16)
        pss = Rot([S.psum(f"pp{i}") for i in range(6)])
        st = {nm: rot_sbuf(S, "st_" + nm, (128, 512)) for nm in ["q", "v", "kf", "lwf", "kb", "lwb", "g", "qr_", "vr_", "kbr", "lwbr", "t1", "t2", "f"]}
        stb = {nm: rot_sbuf(S, "stb_" + nm, (128, 512), BF16) for nm in ["aq", "ak"]}
        stv = rot_sbuf(S, "stv", (128, 128), BF16, n=4)
        cosb = rot_sbuf(S, "cosb", (128, 512))
        sinb = rot_sbuf(S, "sinb", (128, 512))
        for (t0, n, seg0, seglen) in seq_blocks(cfg):
            rp = rev_pos(t0, n, seg0, seglen)
            lat = seg0 == L
            hb = hbs.get()
            S.load("sync", hb, hb[:, :, :n], hT[:, :, t0:t0 + n])
            if lat:
                cb, sb = cosb.get(), sinb.get()
                S.load("scalar", cb, cb[:, :n], cosT[:, t0 - L:t0 - L + n])
                S.load("scalar", sb, sb[:, :n], sinT[:, t0 - L:t0 - L + n])
            for (g, dst, key) in ((0, qr, "aq"), (2, kr, "ak")):
                p1 = pss.get()
                fm_group(S, p1, W, hb, g, n, KC)
                o = stb[key].get()
                if lat:
                    p2 = pss.get()
                    fm_group(S, p2, W, hb, g + 1, n, KC)
                    t1, t2 = st["t1"].get(), st["t2"].get()
                    S.op("vector", lambda e, t1=t1, p1=p1, cb=cb: e.tensor_tensor(out=t1[:, :n], in0=p1[:, :n], in1=cb[:, :n], op=ALU.mult),
                         reads=[p1, cb], writes=[t1])
                    S.op("vector", lambda e, t2=t2, p2=p2, sb=sb: e.tensor_tensor(out=t2[:, :n], in0=p2[:, :n], in1=sb[:, :n], op=ALU.mult),
                         reads=[p2, sb], writes=[t2])
                    S.op("gpsimd", lambda e, o=o, t1=t1, t2=t2: e.tensor_tensor(out=o[:, :n], in0=t1[:, :n], in1=t2[:, :n], op=ALU.add),
                         reads=[t1, t2], writes=[o])
                else:
                    S.op("scalar", lambda e, o=o, p1=p1: e.activation(out=o[:, :n], in_=p1[:, :n], func=AF.Copy), reads=[p1], writes=[o])
                S.store("sync", dst, dst[:, t0:t0 + n], o, o[:, :n])
            for sb_ in range(0, n, 128):
                p = pss.get()
                for kc in range(KC):
                    S.op("tensor", lambda e, p=p, kc=kc, sb_=sb_, hb=hb: e.matmul(p[:, 0:128], hb[:, kc, sb_:sb_ + 128], W[:, kc, 4 * 128:5 * 128],
                                                                           start=(kc == 0), stop=(kc == KC - 1)), reads=[hb, W], writes=[p])
                o = stv.get()
                S.op("vector", lambda e, o=o, p=p: e.tensor_copy(out=o[:], in_=p[:, 0:128]), reads=[p], writes=[o])
                S.store("sync", vt, vt[t0 + sb_:t0 + sb_ + 128, :], o, o[:])

            def put(nm_f, nm_b, tile, rkey):
                if nm_f is not None:
                    S.store("sync", scr[nm_f], scr[nm_f][:, t0:t0 + n], tile, tile[:, :n])
                if nm_b is not None:
                    r = st[rkey].get()
                    S.op("vector", lambda e, r=r, tile=tile: e.tensor_copy(out=r[:, :n], in_=rev_ap(tile[:, :n])), reads=[tile], writes=[r])
                    S.store("sync", scr[nm_b], scr[nm_b][:, rp:rp + n], r, r[:, :n])

            p = pss.get()
            fm_group(S, p, W, hb, 5, n, KC)
            o = st["q"].get()
            S.op("scalar", lambda e, o=o, p=p: e.activation(out=o[:, :n], in_=p[:, :n], func=AF.Silu), reads=[p], writes=[o])
            put("q_f", "q_b", o, "qr_")
            p = pss.get()
            fm_group(S, p, W, hb, 6, n, KC)
            o = st["v"].get()
            S.op("vector", lambda e, o=o, p=p: e.tensor_copy(out=o[:, :n], in_=p[:, :n]), reads=[p], writes=[o])
            put("v_f", "v_b", o, "vr_")
            for (g, kkey, lkey, fwd) in ((7, "kf", "lwf", True), (8, "kb", "lwb", False)):
                p = pss.get()
                fm_group(S, p, W, hb, g, n, KC)
                f = st["f"].get()
                S.op("scalar", lambda e, f=f, p=p: e.activation(out=f[:, :n], in_=p[:, :n], func=AF.Sigmoid), reads=[p], writes=[f])
                S.op("vector", lambda e, f=f: e.tensor_scalar(out=f[:, :n], in0=f[:, :n], scalar1=oml[:, 0:1], scalar2=lb[:, 0:1],
                                                             op0=ALU.mult, op1=ALU.add), reads=[f, oml, lb], writes=[f])
                lw = st[lkey].get()
                kk = st[kkey].get()
                S.op("scalar", lambda e, lw=lw, f=f: e.activation(out=lw[:, :n], in_=f[:, :n], func=AF.Ln), reads=[f], writes=[lw])
                S.op("vector", lambda e, kk=kk, f=f: e.tensor_scalar(out=kk[:, :n], in0=f[:, :n], scalar1=-1.0, scalar2=1.0,
                                                               op0=ALU.mult, op1=ALU.add), reads=[f], writes=[kk])
                if fwd:
                    put("k_f", None, kk, None)
                    put("lw_f", None, lw, None)
                else:
                    put(None, "k_b", kk, "kbr")
                    put(None, "lw_b", lw, "lwbr")
            p = pss.get()
            fm_group(S, p, W, hb, 9, n, KC)
            o = st["g"].get()
            S.op("scalar", lambda e, o=o, p=p: e.activation(out=o[:, :n], in_=p[:, :n], func=AF.Silu), reads=[p], writes=[o])
            put("gate", None, o, None)
        barrier(S)
        S.reset(_ph)
    for _ph in (S.mark(),):
        consts = scan_consts(S, cst_d)
        units = [dict(K=128, V=128, q=scr["q_f"], k=scr["k_f"], lw=scr["lw_f"], v=scr["v_f"], out=scr["o_f"]),
                 dict(K=128, V=128, q=scr["q_b"], k=scr["k_b"], lw=scr["lw_b"], v=scr["v_b"], out=scr["o_b"])]
        chunk_scan(S, cfg, units, consts)
        barrier(S)
        S.reset(_ph)
    for _ph in (S.mark(),):
        ones = make_ones(S)
        hn = load_const(S, "hn", (128, 1), hn_d[:])
        S.op("vector", lambda e: e.tensor_scalar(out=hn[:], in0=hn[:], scalar1=float(128) ** 0.5, scalar2=None, op0=ALU.mult), reads=[hn], writes=[hn])
        ofb, obb, gb = rot_sbuf(S, "ofb", (128, 512)), rot_sbuf(S, "obb", (128, 512)), rot_sbuf(S, "gb", (128, 512))
        sq = S.sbuf("hsq", (128, 1, 512))
        rstd = S.sbuf("hrstd", (128, 512))
        ps = S.psum("hps")
        res = rot_sbuf(S, "hres", (128, 512), BF16)
        for (t0, n, seg0, seglen) in seq_blocks(cfg):
            rp = rev_pos(t0, n, seg0, seglen)
            of, ob, g = ofb.get(), obb.get(), gb.get()
            S.load("sync", of, of[:, :n], scr["o_f"][:, t0:t0 + n], src=scr["o_f"])
            S.load("scalar", ob, ob[:, :n], scr["o_b"][:, rp:rp + n], src=scr["o_b"])
            S.load("sync", g, g[:, :n], scr["gate"][:, t0:t0 + n], src=scr["gate"])
            S.op("vector", lambda e, of=of, ob=ob: e.tensor_tensor(out=of[:, :n], in0=of[:, :n], in1=rev_ap(ob[:, :n]), op=ALU.add),
                 reads=[of, ob], writes=[of])
            S.op("scalar", lambda e, of=of: e.activation(out=sq[:, 0, :n], in_=of[:, :n], func=AF.Square), reads=[of], writes=[sq])
            S.op("tensor", lambda e: e.matmul(ps[:, :n], ones[:], sq[:, 0, :n], start=True, stop=True), reads=[ones, sq], writes=[ps])
            S.op("vector", lambda e: e.tensor_scalar(out=rstd[:, :n], in0=ps[:, :n], scalar1=NORM_EPS * 128, scalar2=None, op0=ALU.add),
                 reads=[ps], writes=[rstd])
            S.op("scalar", lambda e: e.activation(out=rstd[:, :n], in_=rstd[:, :n], func=AF.Sqrt), reads=[rstd], writes=[rstd])
            S.op("vector", lambda e: e.reciprocal(out=rstd[:, :n], in_=rstd[:, :n]), reads=[rstd], writes=[rstd])
            S.op("vector", lambda e, of=of: e.scalar_tensor_tensor(out=of[:, :n], in0=of[:, :n], scalar=hn[:, 0:1], in1=rstd[:, :n],
                                                                 op0=ALU.mult, op1=ALU.mult), reads=[of, hn, rstd], writes=[of])
            r = res.get()
            S.op("gpsimd", lambda e, r=r, of=of, g=g: e.tensor_tensor(out=r[:, :n], in0=of[:, :n], in1=g[:, :n], op=ALU.mult),
                 reads=[of, g], writes=[r])
            S.store("sync", out, out[1, :, t0:t0 + n], r, r[:, :n])
        attention(S, cfg, qr, kr, vt, sink_d, mlo_d, mup_d, out)
    return nc, stack, S


def attention(S, cfg, qr, kr, vt, sink_d, mlo_d, mup_d, out):
    L, SS, T = cfg.L, cfg.S, cfg.T
    NCB = L // 128
    scale = 128.0 ** -0.5
    ones_b = S.sbuf("ones_b", (128, 128), BF16)
    S.op("gpsimd", lambda e: e.memset(ones_b[:], 1.0), writes=[ones_b])
    mlo = load_const(S, "mlo", (128, 128), mlo_d[:], BF16)
    mup = load_const(S, "mup", (128, 128), mup_d[:], BF16)
    es = load_const(S, "es", (128, 1), sink_d[:])
    S.op("scalar", lambda e: e.activation(out=es[:], in_=es[:], func=AF.Exp), reads=[es], writes=[es])
    kc_sb = S.sbuf("kc_sb", (128, L), BF16)
    S.load("sync", kc_sb, kc_sb[:], kr[:, 0:L], src=kr)
    vc_sb = S.sbuf("vc_sb", (128, NCB, 128), BF16)
    for j in range(NCB):
        S.load("sync", vc_sb, vc_sb[:, j, :], vt[j * 128:(j + 1) * 128, :], src=vt)
    qb = rot_sbuf(S, "aqb", (128, 128), BF16, n=3)
    kb = rot_sbuf(S, "akb", (128, 384), BF16, n=3)
    vb = rot_sbuf(S, "avb", (128, 3, 128), BF16, n=3)
    pT = rot_sbuf(S, "apT", (128, 5, 128), BF16, n=2)
    den = rot_sbuf(S, "aden", (128, 128), F32, n=2)
    ob = rot_sbuf(S, "aob", (128, 128), BF16, n=3)
    ps_s = Rot([S.psum(f"aps_s{i}", (128, 512)) for i in range(2)])
    ps_s2 = Rot([S.psum(f"aps_t{i}", (128, 512)) for i in range(2)])
    ps_o = Rot([S.psum(f"aps_o{i}", (128, 512)) for i in range(2)])
    nqb = SS // 128

    def one_block(q0, ktiles):
        q = qb.get()
        S.load("sync", q, q[:], qr[:, q0:q0 + 128], src=qr)
        p1, p2, po = ps_s.get(), ps_s2.get(), ps_o.get()
        P = pT.get()
        nt = len(ktiles)
        for i, (kbuf, kap, vbuf, vap, mask) in enumerate(ktiles):
            pp = p1 if i < 4 else p2
            col = (i % 4) * 128
            S.op("tensor", lambda e, pp=pp, col=col, kap=kap, q=q: e.matmul(pp[:, col:col + 128], kap, q[:], start=True, stop=True),
                 reads=[kbuf, q], writes=[pp])
        n1 = min(nt, 4)
        S.op("scalar", lambda e, P=P, p1=p1, n1=n1: e.activation(out=P[:, 0:n1, :], in_=p1[:, 0:n1 * 128].rearrange("p (a b) -> p a b", b=128),
                                                              func=AF.Exp, scale=scale),
             reads=[p1], writes=[P])
        if nt > 4:
            S.op("scalar", lambda e, P=P, p2=p2: e.activation(out=P[:, 4, :], in_=p2[:, 0:128], func=AF.Exp, scale=scale),
                 reads=[p2], writes=[P])
        for i, (kbuf, kap, vbuf, vap, mask) in enumerate(ktiles):
            if mask is not None:
                S.op("gpsimd", lambda e, P=P, i=i, mask=mask: e.tensor_tensor(out=P[:, i, :], in0=P[:, i, :], in1=mask[:], op=ALU.mult),
                     reads=[P, mask], writes=[P])
        for i, (kbuf, kap, vbuf, vap, mask) in enumerate(ktiles):
            S.op("tensor", lambda e, po=po, vap=vap, P=P, i=i: e.matmul(po[:, 0:128], vap, P[:, i, :], start=(i == 0), stop=(i == nt - 1)),
                 reads=[vbuf, P], writes=[po])
        for i in range(nt):
            S.op("tensor", lambda e, po=po, P=P, i=i: e.matmul(po[:, 128:256], ones_b[:], P[:, i, :], start=(i == 0), stop=(i == nt - 1),
                                                              skip_group_check=True),
                 reads=[ones_b, P], writes=[po])
        d = den.get()
        S.op("vector", lambda e, d=d, po=po: e.tensor_scalar(out=d[:], in0=po[:, 128:256], scalar1=es[:, 0:1], scalar2=None, op0=ALU.add),
             reads=[po, es], writes=[d])
        S.op("vector", lambda e, d=d: e.reciprocal(out=d[:], in_=d[:]), reads=[d], writes=[d])
        o = ob.get()
        S.op("vector", lambda e, o=o, d=d, po=po: e.tensor_tensor(out=o[:], in0=d[:], in1=po[:, 0:128], op=ALU.mult),
             reads=[d, po], writes=[o])
        S.store("sync", out, out[0, :, q0:q0 + 128], o, o[:])

    ctx_tiles = [(kc_sb, kc_sb[:, j * 128:(j + 1) * 128], vc_sb, vc_sb[:, j, :], None) for j in range(NCB)]
    for b in range(NCB):
        one_block(b * 128, ctx_tiles)
    for b in range(nqb):
        lo = max(b - 1, 0)
        hi = min(b + 1, nqb - 1)
        nk = hi - lo + 1
        k = kb.get()
        v = vb.get()
        S.load("scalar", k, k[:, :nk * 128], kr[:, L + lo * 128:L + (hi + 1) * 128], src=kr)
        for i in range(nk):
            S.load("scalar", v, v[:, i, :], vt[L + (lo + i) * 128:L + (lo + i + 1) * 128, :], src=vt)
        tiles = []
        for i in range(nk):
            kbk = lo + i
            mask = mlo if kbk == b - 1 else (mup if kbk == b + 1 else None)
            tiles.append((k, k[:, i * 128:(i + 1) * 128], v, v[:, i, :], mask))
        one_block(L + b * 128, tiles + ctx_tiles)


def rope_tables(S_len):
    n_freq = 32
    t = np.arange(S_len)
    row = (t // 64).astype(np.float32)
    col = (t % 64).astype(np.float32)
    inv = (np.float32(10000.0) ** (-np.arange(n_freq, dtype=np.float32) / np.float32(n_freq))).astype(np.float32)
    d = np.arange(128)
    axis, half, f = d // 64, (d % 64) // 32, d % 32
    pos = np.where(axis[:, None] == 0, row[None, :], col[None, :]).astype(np.float32)
    ang = pos * inv[f][:, None]
    cosT = np.cos(ang).astype(np.float32)
    sinT = (np.sin(ang) * np.where(half[:, None] == 0, -1.0, 1.0)).astype(np.float32)
    partner = np.where(half == 0, d + 32, d - 32)
    return cosT, sinT, partner


def wfm(w):
    D, n = w.shape
    return np.ascontiguousarray(w.reshape(D // 128, 128, n).transpose(1, 0, 2))


def run_mix0(cfg, hT_all, inp):
    nc, stack, S = build_mix0(cfg)
    cosT, sinT, partner = rope_tables(cfg.S)
    w_in = inp["l0_w_in"]
    hfm = fm(hT_all)
    jj = np.arange(128)[:, None]
    ii = np.arange(128)[None, :]
    mlo = (ii <= jj).astype(ml_dtypes.bfloat16)
    mup = (jj <= ii).astype(ml_dtypes.bfloat16)
    sc = host_scan_consts()
    maps = []
    for c in range(NCORES):
        g = c // 4
        q = w_in[:, c * 128:(c + 1) * 128]
        k = w_in[:, 1024 + g * 128:1024 + (g + 1) * 128]
        v = w_in[:, 1280 + g * 128:1280 + (g + 1) * 128]
        cols = [q, q[:, partner], k, k[:, partner], v]
        for base in (1536, 2560, 3584, 4608, 5632):
            cols.append(w_in[:, base + c * 128:base + (c + 1) * 128])
        m = {"hT": hfm, "w": wfm(np.concatenate(cols, axis=1)), "cosT": cosT, "sinT": sinT,
             "lbl": np.ascontiguousarray(inp["hgrn_lb_logits"][:, c * 128:(c + 1) * 128].T),
             "hnorm": np.ascontiguousarray(inp["l0_hgrn_norm"].reshape(128, 1)),
             "sink": np.full((128, 1), inp["l0_attn_sink"][c], np.float32),
             "mask_lo": mlo, "mask_up": mup}
        m.update(sc)
        maps.append(m)
    res = run_prog(nc, stack, S, maps)
    if DEBUG:
        global DBG
        DBG = res
    att = np.concatenate([res[c]["oT"][0] for c in range(NCORES)], axis=0)
    hg = np.concatenate([res[c]["oT"][1] for c in range(NCORES)], axis=0)
    return np.concatenate([att, hg], axis=0)


CAST_ENGS = ("scalar", "gpsimd", "vector")


def convert_w(S, src, dst, NT, F, stage_f, stage_b, ctr):
    step = 2048
    for t in range(NT):
        for f0 in range(0, F, step):
            fn = min(step, F - f0)
            a, b = stage_f.get(), stage_b.get()
            S.load("sync" if ctr[0] % 2 == 0 else "scalar", a, a[:, :fn], src[t, :, f0:f0 + fn])
            eng = CAST_ENGS[ctr[0] % 3]
            if eng == "scalar":
                S.op("scalar", lambda e: e.activation(out=b[:, :fn], in_=a[:, :fn], func=AF.Copy), reads=[a], writes=[b])
            else:
                S.op(eng, lambda e: e.tensor_copy(out=b[:, :fn], in_=a[:, :fn]), reads=[a], writes=[b])
            S.store("sync" if ctr[0] % 2 == 1 else "scalar", dst, dst[t, :, f0:f0 + fn], b, b[:, :fn])
            ctr[0] += 1


def ffn_block(S, cfg, hb, n, NJ, wg_b, wu_b, wd_b, bufs, evac):
    KC = cfg.KC
    hid = bufs["hid"]
    for j in range(NJ):
        wg, wu = bufs["wg"].get(), bufs["wu"].get()
        S.load("sync", wg, wg[:], wg_b[j], src=wg_b)
        S.load("scalar", wu, wu[:], wu_b[j], src=wu_b)
        pg, pu = bufs["pg"].get(), bufs["pu"].get()
        for kc in range(KC):
            S.op("tensor", lambda e: e.matmul(pg[:, :n], wg[:, kc * 128:(kc + 1) * 128], hb[:, kc, :n], start=(kc == 0), stop=(kc == KC - 1)),
                 reads=[wg, hb], writes=[pg])
        for kc in range(KC):
            S.op("tensor", lambda e: e.matmul(pu[:, :n], wu[:, kc * 128:(kc + 1) * 128], hb[:, kc, :n], start=(kc == 0), stop=(kc == KC - 1)),
                 reads=[wu, hb], writes=[pu])
        sg = bufs["sg"].get()
        S.op("scalar", lambda e: e.activation(out=sg[:, :n], in_=pg[:, :n], func=AF.Silu), reads=[pg], writes=[sg])
        S.op("vector", lambda e: e.tensor_tensor(out=hid[:, j, :n], in0=sg[:, :n], in1=pu[:, :n], op=ALU.mult), reads=[sg, pu], writes=[hid])
    for dc in range(KC):
        wd = bufs["wd"].get()
        S.load("sync" if dc % 2 == 0 else "scalar", wd, wd[:, :NJ * 128], wd_b[dc], src=wd_b)
        po = bufs["po"].get()
        for j in range(NJ):
            S.op("tensor", lambda e: e.matmul(po[:, :n], wd[:, j * 128:(j + 1) * 128], hid[:, j, :n], start=(j == 0), stop=(j == NJ - 1)),
                 reads=[wd, hid], writes=[po])
        evac(dc, po)


def ffn_bufs(S, cfg, NJ, nmax):
    KC = cfg.KC
    return {"hid": S.sbuf("hid", (128, NJ, nmax), BF16),
            "wg": rot_sbuf(S, "wg", (128, KC * 128), BF16, n=3), "wu": rot_sbuf(S, "wu", (128, KC * 128), BF16, n=3),
            "wd": rot_sbuf(S, "wd", (128, NJ * 128), BF16, n=2),
            "sg": rot_sbuf(S, "sg", (128, nmax), F32, n=2),
            "pg": Rot([S.psum("pg0"), S.psum("pg1")]), "pu": Rot([S.psum("pu0"), S.psum("pu1")]),
            "po": Rot([S.psum("po0"), S.psum("po1")])}


def host_ffn_w(wg, wu, wd):
    D, FF = wg.shape
    KC, NJ = D // 128, FF // 128

    def gu(w):
        return np.ascontiguousarray(w.reshape(KC, 128, NJ, 128).transpose(2, 1, 0, 3).reshape(NJ, 128, KC * 128))
    wdl = np.ascontiguousarray(wd.reshape(NJ, 128, KC, 128).transpose(2, 1, 0, 3).reshape(KC, 128, NJ * 128))
    return gu(wg), gu(wu), wdl


def build_l3(cfg):
    nc, stack, S = new_prog()
    KC, NB = cfg.KC, 256
    NJ = cfg.D_FF // 128
    xT = S.dram("xT", (128, KC, cfg.ntok), F32, "ExternalInput")
    oT = S.dram("oT", (128, KC, cfg.ntok), BF16, "ExternalInput")
    mod_d = [S.dram(f"mod{l}", (128, 6 * KC, 2), F32, "ExternalInput") for l in range(2)]
    gains_d = S.dram("gains", (128, 4, KC), F32, "ExternalInput")
    wo_d = S.dram("wo", (KC, 128, KC * 128), F32, "ExternalInput")
    wg_d = S.dram("wg", (NJ, 128, KC * 128), F32, "ExternalInput")
    wu_d = S.dram("wu", (NJ, 128, KC * 128), F32, "ExternalInput")
    wd_d = S.dram("wd", (KC, 128, NJ * 128), F32, "ExternalInput")
    x2T = S.dram("x2T", (128, KC, cfg.ntok), F32, "ExternalOutput")
    hT1 = S.dram("hT1", (128, KC, cfg.ntok), BF16, "ExternalOutput")
    wo_b = S.dram("wo_b", (KC, 128, KC * 128), BF16)
    wg_b = S.dram("wg_b", (NJ, 128, KC * 128), BF16)
    wu_b = S.dram("wu_b", (NJ, 128, KC * 128), BF16)
    wd_b = S.dram("wd_b", (KC, 128, NJ * 128), BF16)
    for _ph in (S.mark(),):
        sf, sb = rot_sbuf(S, "cv_f", (128, 2048), F32, n=3), rot_sbuf(S, "cv_b", (128, 2048), BF16, n=3)
        ctr = [0]
        convert_w(S, wo_d, wo_b, KC, KC * 128, sf, sb, ctr)
        convert_w(S, wg_d, wg_b, NJ, KC * 128, sf, sb, ctr)
        convert_w(S, wu_d, wu_b, NJ, KC * 128, sf, sb, ctr)
        convert_w(S, wd_d, wd_b, KC, NJ * 128, sf, sb, ctr)
        barrier(S)
        S.reset(_ph)
    mod = [load_const(S, f"mod_sb{l}", (128, 6 * KC, 2), mod_d[l][:]) for l in range(2)]
    gains = load_const(S, "gains_sb", (128, 4, KC), gains_d[:])
    gn = [Buf(f"gain{i}", gains[:, i, :]) for i in range(4)]
    for g in gn:
        g.writers = gains.writers
    ones = make_ones(S)
    G1 = gate_scalars(S, cfg, mod[0], gn[0], 2, "g1")
    A2, B2 = mod_scalars(S, cfg, mod[0], gn[1], 4, 3, "m2")
    G2 = gate_scalars(S, cfg, mod[0], gn[2], 5, "g2")
    A3, B3 = mod_scalars(S, cfg, mod[1], gn[3], 1, 0, "m3")
    xb = S.sbuf("xb", (128, KC, NB))
    ob = S.sbuf("ob", (128, KC, NB), BF16)
    yb = S.sbuf("yb", (128, KC, NB))
    hb = S.sbuf("hb", (128, KC, NB), BF16)
    sq = rot_sbuf(S, "sq", (128, NB))
    tmp = rot_sbuf(S, "tmp", (128, NB))
    rstd = S.sbuf("rstd", (128, NB))
    wo = rot_sbuf(S, "wo", (128, KC * 128), BF16, n=3)
    ps_s = S.psum("ps_stat")
    ps_y = Rot([S.psum("ps_y0")])
    fb = ffn_bufs(S, cfg, NJ, NB)
    ps_y = fb["po"]
    for (s0, n, kind) in token_blocks(cfg, NB):
        S.load("sync", xb, xb[:, :, :n], xT[:, :, s0:s0 + n])
        S.load("scalar", ob, ob[:, :, :n], oT[:, :, s0:s0 + n])
        for dc in range(KC):
            w = wo.get()
            S.load("sync" if dc % 2 == 0 else "scalar", w, w[:], wo_b[dc], src=wo_b)
            p = ps_y.get()
            for kc in range(KC):
                S.op("tensor", lambda e: e.matmul(p[:, :n], w[:, kc * 128:(kc + 1) * 128], ob[:, kc, :n], start=(kc == 0), stop=(kc == KC - 1)),
                     reads=[w, ob], writes=[p])
            S.op("scalar", lambda e: e.activation(out=yb[:, dc, :n], in_=p[:, :n], func=AF.Copy), reads=[p], writes=[yb])
        rms_stats(S, cfg, yb, n, sq, ones, ps_s, rstd)
        resid_norm_add(S, cfg, xb, yb, n, rstd, G1[kind], tmp)
        rms_stats(S, cfg, xb, n, sq, ones, ps_s, rstd)
        norm_mod_apply(S, cfg, xb, n, rstd, A2[kind], B2[kind], hb, tmp)

        def evac(dc, po):
            S.op("scalar", lambda e: e.activation(out=yb[:, dc, :n], in_=po[:, :n], func=AF.Copy), reads=[po], writes=[yb])
        ffn_block(S, cfg, hb, n, NJ, wg_b, wu_b, wd_b, fb, evac)
        rms_stats(S, cfg, yb, n, sq, ones, ps_s, rstd)
        resid_norm_add(S, cfg, xb, yb, n, rstd, G2[kind], tmp)
        S.store("sync", x2T, x2T[:, :, s0:s0 + n], xb, xb[:, :, :n])
        rms_stats(S, cfg, xb, n, sq, ones, ps_s, rstd)
        norm_mod_apply(S, cfg, xb, n, rstd, A3[kind], B3[kind], hb, tmp)
        S.store("sync", hT1, hT1[:, :, s0:s0 + n], hb, hb[:, :, :n])
    return nc, stack, S


def run_l3(cfg, xT_lat, xT_ctx, oT_all, mods, inp):
    nc, stack, S = build_l3(cfg)
    L = cfg.L
    wo = inp["l0_w_out"]
    KC = cfg.KC
    wo_l = np.ascontiguousarray(wo.reshape(KC, 128, KC, 128).transpose(2, 1, 0, 3).reshape(KC, 128, KC * 128))
    wg, wu, wd = host_ffn_w(inp["l0_ffn_w_gate"], inp["l0_ffn_w_up"], inp["l0_ffn_w_down"])
    gains = np.ascontiguousarray(np.stack([vec_fm(inp[k]) for k in ("l0_norm_mix_post", "l0_norm_ffn_pre", "l0_norm_ffn_post", "l1_norm_mix_pre")], axis=1))
    maps = []
    for i in range(NCORES):
        maps.append({"xT": fm(own_tokens_T(cfg, xT_lat, xT_ctx, i)), "oT": fm(own_tokens_T(cfg, oT_all[:, L:], oT_all[:, :L], i)),
                     "mod0": mods[0], "mod1": mods[1], "gains": gains, "wo": wo_l, "wg": wg, "wu": wu, "wd": wd})
    res = run_prog(nc, stack, S, maps)
    x2c, x2l = gather_tokens_T(cfg, [unfm(res[i]["x2T"]) for i in range(NCORES)])
    hc, hl = gather_tokens_T(cfg, [unfm(res[i]["hT1"]) for i in range(NCORES)])
    return x2c, x2l, np.concatenate([hc, hl], axis=1)


def rwkv_scan(S, cfg, units, consts):
    T = cfg.T
    ident, zero1, cmask, m96 = consts["ident"], consts["zero1"], consts["cmask"], consts["m96"]
    m_ui, m_su, m_sl = consts["mask_ui"], consts["mask_su"], consts["mask_sl"]
    SB = 512
    K = V = 64
    names = ("q", "k", "v", "lw", "a", "b")
    for u, U in enumerate(units):
        U["in"] = {nm: rot_sbuf(S, f"r{u}_{nm}", (128, SB)) for nm in names}
        for nm in ("L", "Ep", "En", "Eex", "qh", "kh", "ah", "bh"):
            U[nm] = S.sbuf(f"r{u}_{nm}", (128, SB))
        for nm in ("NT", "Nn", "MrbT", "MakT", "MrkT", "Xa", "Xb", "PTa", "Pa", "PTb", "Pb"):
            U[nm] = S.sbuf(f"r{u}_{nm}", (128, 128))
        for nm in ("btok", "ktok", "vtok", "Apz", "bz", "kz", "Rp"):
            U[nm] = S.sbuf(f"r{u}_{nm}", (128, 128 if nm == "Rp" else 64))
        for nm in ("PTc", "Qd"):
            U[nm] = S.sbuf(f"r{u}_{nm}", (128, 64))
        U["Z"] = [S.sbuf(f"r{u}_Z{i}", (128, 64)) for i in range(2)]
        U["zi"] = 0
        U["osb"] = rot_sbuf(S, f"r{u}_osb", (128, SB))
        U["ps"] = [S.psum(f"r{u}_ps{i}") for i in range(4)]
        for nm in ("NT", "Nn", "MrbT", "MakT", "MrkT", "Apz", "bz", "kz"):
            S.op("gpsimd", lambda e: e.memset(U[nm][:], 0.0), writes=[U[nm]])
        S.op("gpsimd", lambda e: e.memset(U["Z"][0][:], 0.0), writes=[U["Z"][0]])
    for t0 in range(0, T, SB):
        n = min(SB, T - t0)
        for U in units:
            cur = {}
            for i, nm in enumerate(names):
                b = U["in"][nm].get()
                S.load("sync" if i % 2 == 0 else "scalar", b, b[:K, :n], U[nm][0:K, t0:t0 + n], src=U["src_" + nm])
                cur[nm] = b
            L, Ep, En, Eex, qh, kh, ah, bh = (U[x] for x in ("L", "Ep", "En", "Eex", "qh", "kh", "ah", "bh"))
            S.op("vector", lambda e: e.tensor_tensor_scan(out=L[:K, :n], data0=cmask[:K, :n], data1=cur["lw"][:K, :n], initial=zero1[:K, 0:1],
                                                          op0=ALU.mult, op1=ALU.add), reads=[cmask, cur["lw"], zero1], writes=[L])
            S.op("scalar", lambda e: e.activation(out=Ep[:K, :n], in_=L[:K, :n], func=AF.Exp), reads=[L], writes=[Ep])
            S.op("vector", lambda e: e.reciprocal(out=En[:K, :n], in_=Ep[:K, :n]), reads=[Ep], writes=[En])
            S.op("gpsimd", lambda e: e.tensor_tensor(out=Eex[:K, :n], in0=L[:K, :n], in1=cur["lw"][:K, :n], op=ALU.subtract),
                 reads=[L, cur["lw"]], writes=[Eex])
            S.op("scalar", lambda e: e.activation(out=Eex[:K, :n], in_=Eex[:K, :n], func=AF.Exp), reads=[Eex], writes=[Eex])
            S.op("gpsimd", lambda e: e.tensor_tensor(out=qh[:K, :n], in0=cur["q"][:K, :n], in1=Ep[:K, :n], op=ALU.mult), reads=[cur["q"], Ep], writes=[qh])
            S.op("gpsimd", lambda e: e.tensor_tensor(out=kh[:K, :n], in0=cur["k"][:K, :n], in1=En[:K, :n], op=ALU.mult), reads=[cur["k"], En], writes=[kh])
            S.op("vector", lambda e: e.tensor_tensor(out=ah[:K, :n], in0=cur["a"][:K, :n], in1=Eex[:K, :n], op=ALU.mult), reads=[cur["a"], Eex], writes=[ah])
            S.op("gpsimd", lambda e: e.tensor_tensor(out=bh[:K, :n], in0=cur["b"][:K, :n], in1=En[:K, :n], op=ALU.mult), reads=[cur["b"], En], writes=[bh])
            U["cur"] = cur
            U["o"] = U["osb"].get()
        for j in range(0, n, 128):
            js = slice(j, j + 128)
            for U in units:
                cur = U["cur"]
                qh, kh, ah, bh = U["qh"], U["kh"], U["ah"], U["bh"]
                b0, b1, b2, b3 = U["ps"]
                NT, Nn, MrbT, MakT, MrkT = U["NT"], U["Nn"], U["MrbT"], U["MakT"], U["MrkT"]
                btok, ktok, vtok, Apz, bz, kz, Rp = U["btok"], U["ktok"], U["vtok"], U["Apz"], U["bz"], U["kz"], U["Rp"]

                def mm(out, lhsT, rhs, rd, wr, start=True, stop=True, tr=False):
                    if tr:
                        S.op("tensor", lambda e: e.matmul(out, lhsT, rhs, is_transpose=True, start=True, stop=True), reads=rd, writes=[wr])
                    else:
                        S.op("tensor", lambda e: e.matmul(out, lhsT, rhs, start=start, stop=stop), reads=rd, writes=[wr])
                mm(b0[:, 0:128], bh[:K, js], ah[:K, js], [bh, ah], b0)
                mm(b0[:, 128:256], bh[:K, js], qh[:K, js], [bh, qh], b0)
                mm(b0[:, 256:384], kh[:K, js], ah[:K, js], [kh, ah], b0)
                mm(b0[:, 384:512], kh[:K, js], qh[:K, js], [kh, qh], b0)
                mm(b1[:, 0:128], ah[:K, js], bh[:K, js], [ah, bh], b1)
                for (dst, src, msk) in ((NT, b0[:, 0:128], m_su), (MrbT, b0[:, 128:256], m_ui), (MakT, b0[:, 256:384], m_su),
                                        (MrkT, b0[:, 384:512], m_ui)):
                    S.op("vector", lambda e: e.copy_predicated(out=dst[:], mask=msk[:], data=src), reads=[b0, msk], writes=[dst])
                S.op("vector", lambda e: e.copy_predicated(out=Nn[:], mask=m_sl[:], data=b1[:, 0:128]), reads=[b1, m_sl], writes=[Nn])
                mm(b1[:, 128:192], ah[:K, js], ident[:K, :K], [ah, ident], b1, tr=True)
                mm(b1[:, 192:256], bh[:K, js], ident[:K, :K], [bh, ident], b1, tr=True)
                mm(b1[:, 256:320], kh[:K, js], ident[:K, :K], [kh, ident], b1, tr=True)
                mm(b1[:, 320:384], cur["v"][:V, js], ident[:V, :V], [cur["v"], ident], b1, tr=True)
                X = U["Xa"]
                S.op("scalar", lambda e: e.activation(out=X[:, 0:K], in_=b1[:, 128:192], func=AF.Copy), reads=[b1], writes=[X])
                S.op("vector", lambda e: e.tensor_copy(out=btok[:, :K], in_=b1[:, 192:256]), reads=[b1], writes=[btok])
                S.op("scalar", lambda e: e.activation(out=ktok[:, :K], in_=b1[:, 256:320], func=AF.Copy), reads=[b1], writes=[ktok])
                S.op("vector", lambda e: e.tensor_copy(out=vtok[:, :V], in_=b1[:, 320:384]), reads=[b1], writes=[vtok])
                S.op("vector", lambda e: e.tensor_scalar(out=bz[64:128, :K], in0=b1[64:128, 192:256], scalar1=m96[64:128, 0:1], scalar2=None,
                                                         op0=ALU.mult), reads=[b1, m96], writes=[bz])
                S.op("vector", lambda e: e.tensor_scalar(out=kz[64:128, :K], in0=b1[64:128, 256:320], scalar1=m96[64:128, 0:1], scalar2=None,
                                                         op0=ALU.mult), reads=[b1, m96], writes=[kz])
                mm(b1[:, 384:448], MakT[:], vtok[:, :V], [MakT, vtok], b1)
                S.op("scalar", lambda e: e.activation(out=X[:, K:K + V], in_=b1[:, 384:448], func=AF.Copy), reads=[b1], writes=[X])
                PT, P = NT, Nn
                spare = [(U["PTa"], U["Pa"]), (U["PTb"], U["Pb"])]
                for i in range(5):
                    Xn = U["Xb"] if X is U["Xa"] else U["Xa"]
                    mm(b2[:, 0:128], PT[:], X[:], [PT, X], b2)
                    S.op("vector", lambda e: e.tensor_tensor(out=Xn[:], in0=X[:], in1=b2[:, 0:128], op=ALU.add), reads=[X, b2], writes=[Xn])
                    if i < 4:
                        PTn, Pn = spare[i % 2]
                        mm(b2[:, 128:256], P[:], PT[:], [P, PT], b2)
                        if i < 3:
                            mm(b2[:, 256:384], PT[:], P[:], [PT, P], b2)
                        S.op("scalar", lambda e: e.activation(out=PTn[:], in_=b2[:, 128:256], func=AF.Copy), reads=[b2], writes=[PTn])
                        if i < 3:
                            S.op("scalar", lambda e: e.activation(out=Pn[:], in_=b2[:, 256:384], func=AF.Copy), reads=[b2], writes=[Pn])
                        PT, P = PTn, Pn
                    X = Xn
                U["X5"] = X
                S.op("gpsimd", lambda e: e.tensor_scalar(out=Apz[64:128, :K], in0=X[64:128, 0:K], scalar1=m96[64:128, 0:1], scalar2=None,
                                                         op0=ALU.mult), reads=[X, m96], writes=[Apz])
                mm(b3[:K, 0:128], X[:, 0:K], MrbT[:], [X, MrbT], b3)
                S.op("vector", lambda e: e.tensor_tensor(out=Rp[:K, :], in0=qh[:K, js], in1=b3[:K, 0:128], op=ALU.add), reads=[qh, b3], writes=[Rp])
                mm(b3[:V, 128:256], X[:, K:K + V], MrbT[:], [X, MrbT], b3, start=True, stop=False)
                mm(b3[:V, 128:256], vtok[:, :V], MrkT[:], [vtok, MrkT], b3, start=False, stop=False)
            for c in range(4):
                for U in units:
                    b0, b1, b2, b3 = U["ps"]
                    X, Rp, btok, ktok, vtok, Apz, bz, kz = U["X5"], U["Rp"], U["btok"], U["ktok"], U["vtok"], U["Apz"], U["bz"], U["kz"]
                    PTc, Qd, Ep = U["PTc"], U["Qd"], U["Ep"]
                    Zc, Zn = U["Z"][U["zi"]], U["Z"][1 - U["zi"]]
                    U["zi"] = 1 - U["zi"]
                    wc = j + 32 * c + 31
                    rs = slice(32 * c, 32 * c + 32)
                    hi = slice(64, 128)
                    S.op("tensor", lambda e: e.matmul(b3[:V, 128 + 32 * c:128 + 32 * c + 32], Zc[:K, :V], Rp[:K, 32 * c:32 * c + 32],
                                                      start=False, stop=(c == 3)), reads=[Zc, Rp], writes=[b3])
                    if c < 3:
                        S.op("tensor", lambda e: e.matmul(b2[:K, 256:256 + K], X[rs, 0:K], btok[rs, :K], start=True, stop=True),
                             reads=[X, btok], writes=[b2])
                    else:
                        S.op("tensor", lambda e: e.matmul(b2[:K, 256:256 + K], Apz[hi, :K], btok[hi, :K], start=True, stop=True),
                             reads=[Apz, btok], writes=[b2])
                    S.op("vector", lambda e: e.tensor_tensor(out=PTc[:K, :K], in0=b2[:K, 256:256 + K], in1=ident[:K, :K], op=ALU.add),
                         reads=[b2, ident], writes=[PTc])
                    if c < 3:
                        S.op("tensor", lambda e: e.matmul(b2[:K, 320:320 + V], btok[rs, :K], X[rs, K:K + V], start=True, stop=False),
                             reads=[btok, X], writes=[b2])
                        S.op("tensor", lambda e: e.matmul(b2[:K, 320:320 + V], ktok[rs, :K], vtok[rs, :V], start=False, stop=True),
                             reads=[ktok, vtok], writes=[b2])
                    else:
                        S.op("tensor", lambda e: e.matmul(b2[:K, 320:320 + V], bz[hi, :K], X[hi, K:K + V], start=True, stop=False),
                             reads=[bz, X], writes=[b2])
                        S.op("tensor", lambda e: e.matmul(b2[:K, 320:320 + V], kz[hi, :K], vtok[hi, :V], start=False, stop=True),
                             reads=[kz, vtok], writes=[b2])
                    S.op("vector", lambda e: e.tensor_scalar(out=Qd[:K, :V], in0=b2[:K, 320:320 + V], scalar1=Ep[:K, wc:wc + 1], scalar2=None,
                                                             op0=ALU.mult), reads=[b2, Ep], writes=[Qd])
                    S.op("tensor", lambda e: e.matmul(b2[:K, 384:384 + V], PTc[:K, :K], Zc[:K, :V], start=True, stop=True),
                         reads=[PTc, Zc], writes=[b2])
                    S.op("vector", lambda e: e.scalar_tensor_tensor(out=Zn[:K, :V], in0=b2[:K, 384:384 + V], scalar=Ep[:K, wc:wc + 1], in1=Qd[:K, :V],
                                                                    op0=ALU.mult, op1=ALU.add), reads=[b2, Ep, Qd], writes=[Zn])
            for U in units:
                o, b3 = U["o"], U["ps"][3]
                S.op("scalar", lambda e: e.activation(out=o[:V, j:j + 128], in_=b3[:V, 128:256], func=AF.Copy), reads=[b3], writes=[o])
        for U in units:
            S.store("sync", U["src_out"], U["out"][0:V, t0:t0 + n], U["o"], U["o"][:V, :n])


NG1 = 13
RW_LN_EPS = 64e-5


def build_mix1(cfg):
    nc, stack, S = new_prog()
    KC, T, L, SS = cfg.KC, cfg.T, cfg.L, cfg.S
    PB = 256
    hT = S.dram("hT", (128, KC, T), BF16, "ExternalInput")
    w_d = S.dram("w", (128, KC, NG1 * 128), F32, "ExternalInput")
    sm_d = S.dram("smalls", (128, 40), F32, "ExternalInput")
    lr_d = S.dram("lowrank", (8, 128, 128), F32, "ExternalInput")
    cst_d = declare_scan_consts(S, rwkv=True)
    out = S.dram("oT", (3, 128, SS), BF16, "ExternalOutput")
    gnames = ["q_f", "k_f", "v_f", "lw_f", "q_b", "k_b", "v_b", "lw_b", "o_f", "o_b"]
    gs = {nm: S.dram("g_" + nm, (128, T), F32, "ExternalOutput" if DEBUG else "Internal") for nm in gnames}
    rnames = ["r_f", "k_f", "v_f", "a_f", "b_f", "lw_f", "r_b", "k_b", "v_b", "a_b", "b_b", "lw_b", "o_f", "o_b", "gout", "bonus"]
    rs = {nm: S.dram("r_" + nm, (128, T), F32, "ExternalOutput" if DEBUG else "Internal") for nm in rnames}

    for _ph in (S.mark(),):
        W = load_w_bf16(S, "W", w_d, KC, NG1 * 128)
        sm = load_const(S, "sm", (128, 40), sm_d[:])
        lr = S.sbuf("lr", (128, 8, 128))
        for i in range(8):
            S.load("scalar", lr, lr[:, i, :], lr_d[i])
        UPF, UPB, W2F, W2B, A2, G20, G21, BD = range(8)
        S.op("vector", lambda e: e.tensor_scalar(out=sm[:, 24:25], in0=sm[:, 6:7], scalar1=-1.0, scalar2=1.0, op0=ALU.mult, op1=ALU.add),
             reads=[sm], writes=[sm])
        S.op("vector", lambda e: e.tensor_tensor(out=sm[:, 25:33], in0=sm[:, 8:16], in1=sm[:, 16:24], op=ALU.add), reads=[sm], writes=[sm])
        S.op("vector", lambda e: e.tensor_scalar(out=sm[:, 25:33], in0=sm[:, 25:33], scalar1=-1.0, scalar2=1.0, op0=ALU.mult, op1=ALU.add),
             reads=[sm], writes=[sm])
        hbs = rot_sbuf(S, "hb", (128, KC, PB + 2), BF16)
        pss = Rot([S.psum(f"pp{i}") for i in range(8)])
        NT_ = 40
        pool = rot_sbuf(S, "tp", (128, PB + 2), F32, n=NT_)
        revp = rot_sbuf(S, "rv", (128, PB), F32, n=8)
        gob = rot_sbuf(S, "gob", (128, PB), BF16, n=2)

        for (t0, n, seg0, seglen) in seq_blocks(cfg, PB):
            rp = rev_pos(t0, n, seg0, seglen)
            lat = seg0 == L
            lo, hi = max(t0 - 1, 0), min(t0 + n + 1, T)
            hb = hbs.get()
            S.load("sync", hb, hb[:, :, lo - (t0 - 1):hi - (t0 - 1)], hT[:, :, lo:hi])
            ne = n + 2

            def fmg(g, c0_, nn):
                p = pss.get()
                for kc in range(KC):
                    S.op("tensor", lambda e: e.matmul(p[:, :nn], W[:, kc, g * 128:(g + 1) * 128], hb[:, kc, c0_:c0_ + nn],
                                                      start=(kc == 0), stop=(kc == KC - 1)), reads=[W, hb], writes=[p])
                return p

            def put(dst_f, dst_b, tile):
                if dst_f is not None:
                    S.store("sync", dst_f, dst_f[:, t0:t0 + n], tile, tile[:, :n])
                if dst_b is not None:
                    r = revp.get()
                    S.op("vector", lambda e: e.tensor_copy(out=r[:, :n], in_=rev_ap(tile[:, :n])), reads=[tile], writes=[r])
                    S.store("scalar", dst_b, dst_b[:, rp:rp + n], r, r[:, :n])

            p = fmg(0, 1, n)
            o = pool.get()
            S.op("vector", lambda e: e.tensor_scalar(out=o[:, :n], in0=p[:, :n], scalar1=128.0 ** -0.5, scalar2=None, op0=ALU.mult), reads=[p], writes=[o])
            put(gs["q_f"], gs["q_b"], o)
            for g, nm in ((1, "k"), (2, "v")):
                p = fmg(g, 1, n)
                o = pool.get()
                S.op("scalar", lambda e: e.activation(out=o[:, :n], in_=p[:, :n], func=AF.Copy), reads=[p], writes=[o])
                put(gs[nm + "_f"], gs[nm + "_b"], o)
            p = fmg(3, 1, n)
            gd = pool.get()
            S.op("vector", lambda e: e.tensor_copy(out=gd[:, :n], in_=p[:, :n]), reads=[p], writes=[gd])
            for (ui_, bcol, fwd) in ((UPF, 0, True), (UPB, 1, False)):
                p = pss.get()
                S.op("tensor", lambda e: e.matmul(p[:, :n], lr[:, ui_, :], gd[:, :n], start=True, stop=True), reads=[lr, gd], writes=[p])
                o = pool.get()
                S.op("scalar", lambda e: e.activation(out=o[:, :n], in_=p[:, :n], func=AF.Sigmoid, bias=sm[:, bcol:bcol + 1]), reads=[p, sm], writes=[o])
                S.op("scalar", lambda e: e.activation(out=o[:, :n], in_=o[:, :n], func=AF.Ln), reads=[o], writes=[o])
                S.op("vector", lambda e: e.tensor_scalar(out=o[:, :n], in0=o[:, :n], scalar1=1.0 / 16.0, scalar2=None, op0=ALU.mult), reads=[o], writes=[o])
                if fwd:
                    put(gs["lw_f"], None, o)
                else:
                    put(None, gs["lw_b"], o)
            if lat:
                p = fmg(4, 1, n)
                ob = gob.get()
                S.op("scalar", lambda e: e.activation(out=ob[:, :n], in_=p[:, :n], func=AF.Silu), reads=[p], writes=[ob])
                S.store("sync", out, out[1, :, t0 - L:t0 - L + n], ob, ob[:, :n])
            sh = {}
            for gi, g in enumerate(range(5, 13)):
                p = fmg(g, 0, ne)
                pe = pool.get()
                S.op("scalar", lambda e: e.activation(out=pe[:, :ne], in_=p[:, :ne], func=AF.Copy), reads=[p], writes=[pe])
                if t0 == seg0:
                    S.op("gpsimd", lambda e: e.memset(pe[:, 0:1], 0.0), reads=[pe], writes=[pe])
                if t0 + n == seg0 + seglen:
                    S.op("gpsimd", lambda e: e.memset(pe[:, n + 1:n + 2], 0.0), reads=[pe], writes=[pe])
                o = pool.get()
                S.op("vector", lambda e: e.tensor_scalar(out=o[:, :n], in0=pe[:, 1:n + 1], scalar1=sm[:, 25 + gi:26 + gi], scalar2=None, op0=ALU.mult),
                     reads=[pe, sm], writes=[o])
                S.op("vector", lambda e: e.scalar_tensor_tensor(out=o[:, :n], in0=pe[:, 0:n], scalar=sm[:, 8 + gi:9 + gi], in1=o[:, :n],
                                                                op0=ALU.mult, op1=ALU.add), reads=[pe, sm, o], writes=[o])
                S.op("vector", lambda e: e.scalar_tensor_tensor(out=o[:, :n], in0=pe[:, 2:n + 2], scalar=sm[:, 16 + gi:17 + gi], in1=o[:, :n],
                                                                op0=ALU.mult, op1=ALU.add), reads=[pe, sm, o], writes=[o])
                sh[g] = o
            rr, rk, rv, wdf, wdb, ad, gd0, gd1 = (sh[g] for g in range(5, 13))
            put(rs["r_f"], rs["r_b"], rr)
            put(rs["v_f"], rs["v_b"], rv)
            for (wd_, wi, bcol, fwd) in ((wdf, W2F, 2, True), (wdb, W2B, 3, False)):
                S.op("scalar", lambda e: e.activation(out=wd_[:, :n], in_=wd_[:, :n], func=AF.Tanh), reads=[wd_], writes=[wd_])
                p = pss.get()
                S.op("tensor", lambda e: e.matmul(p[:, :n], lr[:, wi, :], wd_[:, :n], start=True, stop=True), reads=[lr, wd_], writes=[p])
                o = pool.get()
                S.op("scalar", lambda e: e.activation(out=o[:, :n], in_=p[:, :n], func=AF.Sigmoid, bias=sm[:, bcol:bcol + 1]), reads=[p, sm], writes=[o])
                S.op("vector", lambda e: e.tensor_scalar(out=o[:, :n], in0=o[:, :n], scalar1=-float(np.exp(-0.5)), scalar2=None, op0=ALU.mult),
                     reads=[o], writes=[o])
                if fwd:
                    put(rs["lw_f"], None, o)
                else:
                    put(None, rs["lw_b"], o)
            p = pss.get()
            S.op("tensor", lambda e: e.matmul(p[:, :n], lr[:, A2, :], ad[:, :n], start=True, stop=True), reads=[lr, ad], writes=[p])
            a_ = pool.get()
            S.op("scalar", lambda e: e.activation(out=a_[:, :n], in_=p[:, :n], func=AF.Sigmoid, bias=sm[:, 4:5]), reads=[p, sm], writes=[a_])
            kk = pool.get()
            S.op("vector", lambda e: e.tensor_scalar(out=kk[:, :n], in0=rk[:, :n], scalar1=sm[:, 5:6], scalar2=None, op0=ALU.mult), reads=[rk, sm], writes=[kk])
            k2 = pool.get()
            S.op("scalar", lambda e: e.activation(out=k2[:, :n], in_=kk[:, :n], func=AF.Square), reads=[kk], writes=[k2])
            p = pss.get()
            S.op("tensor", lambda e: e.matmul(p[:, :n], lr[:, BD, :], k2[:, :n], start=True, stop=True), reads=[lr, k2], writes=[p])
            S.op("vector", lambda e: e.tensor_scalar(out=k2[:, :n], in0=p[:, :n], scalar1=1e-12, scalar2=None, op0=ALU.add), reads=[p], writes=[k2])
            S.op("scalar", lambda e: e.activation(out=k2[:, :n], in_=k2[:, :n], func=AF.Sqrt), reads=[k2], writes=[k2])
            S.op("vector", lambda e: e.reciprocal(out=k2[:, :n], in_=k2[:, :n]), reads=[k2], writes=[k2])
            S.op("gpsimd", lambda e: e.tensor_tensor(out=kk[:, :n], in0=kk[:, :n], in1=k2[:, :n], op=ALU.mult), reads=[kk, k2], writes=[kk])
            bv = pool.get()
            S.op("gpsimd", lambda e: e.tensor_tensor(out=bv[:, :n], in0=kk[:, :n], in1=a_[:, :n], op=ALU.mult), reads=[kk, a_], writes=[bv])
            put(rs["b_f"], rs["b_b"], bv)
            av = pool.get()
            S.op("vector", lambda e: e.tensor_scalar(out=av[:, :n], in0=kk[:, :n], scalar1=-1.0, scalar2=None, op0=ALU.mult), reads=[kk], writes=[av])
            put(rs["a_f"], rs["a_b"], av)
            km = pool.get()
            S.op("vector", lambda e: e.tensor_scalar(out=km[:, :n], in0=a_[:, :n], scalar1=sm[:, 6:7], scalar2=sm[:, 24:25], op0=ALU.mult, op1=ALU.add),
                 reads=[a_, sm], writes=[km])
            S.op("gpsimd", lambda e: e.tensor_tensor(out=km[:, :n], in0=km[:, :n], in1=rk[:, :n], op=ALU.mult), reads=[km, rk], writes=[km])
            put(rs["k_f"], rs["k_b"], km)
            if lat:
                t1 = pool.get()
                S.op("vector", lambda e: e.scalar_tensor_tensor(out=t1[:, :n], in0=rr[:, :n], scalar=sm[:, 7:8], in1=km[:, :n], op0=ALU.mult, op1=ALU.mult),
                     reads=[rr, sm, km], writes=[t1])
                p = pss.get()
                S.op("tensor", lambda e: e.matmul(p[:, :n], lr[:, BD, :], t1[:, :n], start=True, stop=True), reads=[lr, t1], writes=[p])
                bo = pool.get()
                S.op("vector", lambda e: e.tensor_tensor(out=bo[:, :n], in0=rv[:, :n], in1=p[:, :n], op=ALU.mult), reads=[rv, p], writes=[bo])
                put(rs["bonus"], None, bo)
                S.op("scalar", lambda e: e.activation(out=gd0[:, :n], in_=gd0[:, :n], func=AF.Sigmoid), reads=[gd0], writes=[gd0])
                S.op("scalar", lambda e: e.activation(out=gd1[:, :n], in_=gd1[:, :n], func=AF.Sigmoid), reads=[gd1], writes=[gd1])
                p = pss.get()
                S.op("tensor", lambda e: e.matmul(p[:, :n], lr[:, G20, :], gd0[:, :n], start=True, stop=False), reads=[lr, gd0], writes=[p])
                S.op("tensor", lambda e: e.matmul(p[:, :n], lr[:, G21, :], gd1[:, :n], start=False, stop=True), reads=[lr, gd1], writes=[p])
                go = pool.get()
                S.op("scalar", lambda e: e.activation(out=go[:, :n], in_=p[:, :n], func=AF.Copy), reads=[p], writes=[go])
                put(rs["gout"], None, go)
        barrier(S)
        S.reset(_ph)
    for _ph in (S.mark(),):
        consts = scan_consts(S, cst_d)
        units = [dict(K=128, V=128, q=gs["q_f"], k=gs["k_f"], lw=gs["lw_f"], v=gs["v_f"], out=gs["o_f"]),
                 dict(K=128, V=128, q=gs["q_b"], k=gs["k_b"], lw=gs["lw_b"], v=gs["v_b"], out=gs["o_b"])]
        chunk_scan(S, cfg, units, consts)
        barrier(S)
        S.reset(_ph)
    for hh in range(2):
        for _ph in (S.mark(),):
            consts = scan_consts(S, cst_d)
            units = []
            for d in ("f", "b"):
                U = {}
                for nm, key in (("q", "r"), ("k", "k"), ("v", "v"), ("lw", "lw"), ("a", "a"), ("b", "b")):
                    U[nm] = rs[f"{key}_{d}"][64 * hh:64 * hh + 64, :]
                    U["src_" + nm] = rs[f"{key}_{d}"]
                U["out"] = rs[f"o_{d}"][64 * hh:64 * hh + 64, :]
                U["src_out"] = rs[f"o_{d}"]
                units.append(U)
            rwkv_scan(S, cfg, units, consts)
            barrier(S)
            S.reset(_ph)
    for _ph in (S.mark(),):
        sm = load_const(S, "sm2", (128, 40), sm_d[:])
        bd = load_const(S, "bd", (128, 128), lr_d[7])
        S.op("vector", lambda e: e.tensor_scalar(out=bd[:], in0=bd[:], scalar1=1.0 / 64.0, scalar2=None, op0=ALU.mult), reads=[bd], writes=[bd])
        tp = rot_sbuf(S, "p5", (128, 512), F32, n=12)
        ob_ = rot_sbuf(S, "p5o", (128, 512), BF16, n=4)
        ps = Rot([S.psum(f"p5ps{i}") for i in range(4)])
        for s0 in range(0, SS, 512):
            n = min(512, SS - s0)
            t0 = L + s0
            rp = rev_pos(t0, n, L, SS)
            a, b = tp.get(), tp.get()
            S.load("sync", a, a[:, :n], gs["o_f"][:, t0:t0 + n], src=gs["o_f"])
            S.load("scalar", b, b[:, :n], gs["o_b"][:, rp:rp + n], src=gs["o_b"])
            o = ob_.get()
            S.op("vector", lambda e: e.tensor_tensor(out=o[:, :n], in0=a[:, :n], in1=rev_ap(b[:, :n]), op=ALU.add), reads=[a, b], writes=[o])
            S.store("sync", out, out[0, :, s0:s0 + n], o, o[:, :n])
            a, b, bo, go = tp.get(), tp.get(), tp.get(), tp.get()
            S.load("sync", a, a[:, :n], rs["o_f"][:, t0:t0 + n], src=rs["o_f"])
            S.load("scalar", b, b[:, :n], rs["o_b"][:, rp:rp + n], src=rs["o_b"])
            S.load("sync", bo, bo[:, :n], rs["bonus"][:, t0:t0 + n], src=rs["bonus"])
            S.load("scalar", go, go[:, :n], rs["gout"][:, t0:t0 + n], src=rs["gout"])
            S.op("vector", lambda e: e.tensor_tensor(out=a[:, :n], in0=a[:, :n], in1=rev_ap(b[:, :n]), op=ALU.add), reads=[a, b], writes=[a])
            p1 = ps.get()
            S.op("tensor", lambda e: e.matmul(p1[:, :n], bd[:], a[:, :n], start=True, stop=True), reads=[bd, a], writes=[p1])
            d_ = tp.get()
            S.op("vector", lambda e: e.tensor_tensor(out=d_[:, :n], in0=a[:, :n], in1=p1[:, :n], op=ALU.subtract), reads=[a, p1], writes=[d_])
            d2 = tp.get()
            S.op("scalar", lambda e: e.activation(out=d2[:, :n], in_=d_[:, :n], func=AF.Square), reads=[d_], writes=[d2])
            p2 = ps.get()
            S.op("tensor", lambda e: e.matmul(p2[:, :n], bd[:], d2[:, :n], start=True, stop=True), reads=[bd, d2], writes=[p2])
            S.op("vector", lambda e: e.tensor_scalar(out=d2[:, :n], in0=p2[:, :n], scalar1=RW_LN_EPS, scalar2=None, op0=ALU.add), reads=[p2], writes=[d2])
            S.op("scalar", lambda e: e.activation(out=d2[:, :n], in_=d2[:, :n], func=AF.Sqrt), reads=[d2], writes=[d2])
            S.op("vector", lambda e: e.reciprocal(out=d2[:, :n], in_=d2[:, :n]), reads=[d2], writes=[d2])
            S.op("gpsimd", lambda e: e.tensor_tensor(out=d_[:, :n], in0=d_[:, :n], in1=d2[:, :n], op=ALU.mult), reads=[d_, d2], writes=[d_])
            S.op("vector", lambda e: e.tensor_scalar(out=d_[:, :n], in0=d_[:, :n], scalar1=sm[:, 33:34], scalar2=sm[:, 34:35], op0=ALU.mult, op1=ALU.add),
                 reads=[d_, sm], writes=[d_])
            S.op("gpsimd", lambda e: e.tensor_tensor(out=d_[:, :n], in0=d_[:, :n], in1=bo[:, :n], op=ALU.add), reads=[d_, bo], writes=[d_])
            o = ob_.get()
            S.op("vector", lambda e: e.tensor_tensor(out=o[:, :n], in0=d_[:, :n], in1=go[:, :n], op=ALU.mult), reads=[d_, go], writes=[o])
            S.store("sync", out, out[2, :, s0:s0 + n], o, o[:, :n])
    return nc, stack, S


def run_mix1(cfg, hT_all, inp):
    nc, stack, S = build_mix1(cfg)
    w_in = inp["l1_w_in"]
    hfm = fm(hT_all)
    sc = host_scan_consts(rwkv=True)
    RW0 = 3104
    maps = []
    pidx = np.arange(128)
    bd = (pidx[:, None] // 64 == pidx[None, :] // 64).astype(np.float32)

    def pad_cols(w, n=128):
        return np.concatenate([w, np.zeros((w.shape[0], n - w.shape[1]), np.float32)], axis=1)

    def pad_rows(w, r0=0, n=128):
        o = np.zeros((n, w.shape[1]), np.float32)
        o[r0:r0 + w.shape[0]] = w
        return o

    def pad_vec(v, n=128):
        o = np.zeros((n,), np.float32)
        o[:v.shape[0]] = v
        return o
    for c in range(NCORES):
        gh, vh = c // 2, c % 2
        ch = slice(c * 128, (c + 1) * 128)
        cols = [w_in[:, gh * 128:(gh + 1) * 128], w_in[:, 512 + gh * 128:512 + (gh + 1) * 128],
                w_in[:, 1024 + gh * 256 + vh * 128:1024 + gh * 256 + (vh + 1) * 128],
                pad_cols(w_in[:, 2048:2080]), w_in[:, 2080 + gh * 256 + vh * 128:2080 + gh * 256 + (vh + 1) * 128]]
        rcols = [(0, ch), (1024, ch), (2048, ch)]
        mu_p, mu_n = inp["l1_rwkv_mu_prev"], inp["l1_rwkv_mu_next"]
        mup_g, mun_g = [], []
        for base, sl in rcols:
            cols.append(w_in[:, RW0 + base + sl.start:RW0 + base + sl.stop])
            mup_g.append(mu_p[base + sl.start:base + sl.stop])
            mun_g.append(mu_n[base + sl.start:base + sl.stop])
        for base, width in ((3072, 96), (3168, 96), (3264, 96), (3360, 128), (3488, 128)):
            cols.append(pad_cols(w_in[:, RW0 + base:RW0 + base + width]))
            mup_g.append(pad_vec(mu_p[base:base + width]))
            mun_g.append(pad_vec(mu_n[base:base + width]))
        sm = np.zeros((128, 40), np.float32)
        gk = slice(gh * 128, (gh + 1) * 128)
        sm[:, 0] = inp["l1_gla_gate_bias_f"][gk]
        sm[:, 1] = inp["l1_gla_gate_bias_b"][gk]
        sm[:, 2] = inp["l1_rwkv_w0_f"][ch]
        sm[:, 3] = inp["l1_rwkv_w0_b"][ch]
        sm[:, 4] = inp["l1_rwkv_a0"][ch]
        sm[:, 5] = inp["l1_rwkv_k_k"][ch]
        sm[:, 6] = inp["l1_rwkv_k_a"][ch]
        sm[:, 7] = inp["l1_rwkv_r_k"].reshape(-1)[ch]
        for gi in range(8):
            sm[:, 8 + gi] = mup_g[gi]
            sm[:, 16 + gi] = mun_g[gi]
        sm[:, 33] = inp["l1_rwkv_ln_w"][ch]
        sm[:, 34] = inp["l1_rwkv_ln_b"][ch]
        lrk = np.stack([pad_rows(inp["l1_gla_gate_up_f"][:, gk], 0), pad_rows(inp["l1_gla_gate_up_b"][:, gk], 16),
                        pad_rows(inp["l1_rwkv_w2_f"][:, ch]), pad_rows(inp["l1_rwkv_w2_b"][:, ch]), pad_rows(inp["l1_rwkv_a2"][:, ch]),
                        inp["l1_rwkv_g2"][0:128, ch], inp["l1_rwkv_g2"][128:256, ch], bd], axis=0)
        m = {"hT": hfm, "w": wfm(np.concatenate(cols, axis=1)), "smalls": sm, "lowrank": np.ascontiguousarray(lrk)}
        m.update(sc)
        maps.append(m)
    res = run_prog(nc, stack, S, maps)
    if DEBUG:
        global DBG
        DBG = res
    gla_o = np.concatenate([res[c]["oT"][0] for c in range(NCORES)], axis=0)
    gla_g = np.concatenate([res[c]["oT"][1] for c in range(NCORES)], axis=0)
    rw = np.concatenate([res[c]["oT"][2] for c in range(NCORES)], axis=0)
    return gla_o, gla_g, rw


def build_l5(cfg):
    nc, stack, S = new_prog()
    KC, NB, ntl = cfg.KC, 256, cfg.ntl
    NE = cfg.N_EXP
    xT = S.dram("xT", (128, KC, ntl), F32, "ExternalInput")
    mo = S.dram("mo", (3, 128, 8, ntl), BF16, "ExternalInput")
    mod_d = S.dram("mod", (128, 6 * KC, 2), F32, "ExternalInput")
    gains_d = S.dram("gains", (128, 2, KC), F32, "ExternalInput")
    gn_d = S.dram("gnorm", (128, 2), F32, "ExternalInput")
    wo_d = S.dram("wo", (KC, 128, KC * 128), F32, "ExternalInput")
    rt_d = S.dram("router", (128, KC, NE), F32, "ExternalInput")
    x3T = S.dram("x3T", (128, KC, ntl), F32, "ExternalOutput")
    h2T = S.dram("h2T", (128, KC, ntl), BF16, "ExternalOutput")
    gates = S.dram("gates", (ntl, NE), F32, "ExternalOutput")
    wo_b = S.dram("wo_b", (KC, 128, KC * 128), BF16)
    for _ph in (S.mark(),):
        sf, sb = rot_sbuf(S, "cv_f", (128, 2048), F32, n=3), rot_sbuf(S, "cv_b", (128, 2048), BF16, n=3)
        convert_w(S, wo_d, wo_b, KC, KC * 128, sf, sb, [0])
        barrier(S)
        S.reset(_ph)
    mod = load_const(S, "mod_sb", (128, 6 * KC, 2), mod_d[:])
    gains = load_const(S, "gains_sb", (128, 2, KC), gains_d[:])
    gn = [Buf(f"gain{i}", gains[:, i, :]) for i in range(2)]
    for g in gn:
        g.writers = gains.writers
    gnorm = load_const(S, "gnorm_sb", (128, 2), gn_d[:])
    S.op("vector", lambda e: e.tensor_scalar(out=gnorm[:], in0=gnorm[:], scalar1=16.0, scalar2=None, op0=ALU.mult), reads=[gnorm], writes=[gnorm])
    router = load_const(S, "router_sb", (128, KC, NE), rt_d[:])
    ones = make_ones(S)
    G1 = gate_scalars(S, cfg, mod, gn[0], 2, "g1")
    A2, B2 = mod_scalars(S, cfg, mod, gn[1], 4, 3, "m2")
    xb = S.sbuf("xb", (128, KC, NB))
    go = S.sbuf("go", (128, 8, NB), BF16)
    gg = S.sbuf("gg", (128, 8, NB), BF16)
    ob = S.sbuf("ob", (128, KC, NB), BF16)
    yb = S.sbuf("yb", (128, KC, NB))
    hf = S.sbuf("hf", (128, KC, NB))
    hb = S.sbuf("hb", (128, KC, NB), BF16)
    sq = rot_sbuf(S, "sq", (128, NB))
    tmp = rot_sbuf(S, "tmp", (128, NB), n=4)
    rstd = S.sbuf("rstd", (128, NB))
    wo = rot_sbuf(S, "wo", (128, KC * 128), BF16, n=3)
    ps_s = S.psum("ps_stat")
    ps_y = Rot([S.psum("ps_y0"), S.psum("ps_y1")])
    ps_r = S.psum("ps_r")
    lg = rot_sbuf(S, "lg", (128, 8), n=2)
    m8 = rot_sbuf(S, "m8", (128, 8), n=2)
    ex = rot_sbuf(S, "ex", (128, 8), n=2)
    mk = rot_sbuf(S, "mk", (128, 8), n=2)
    s1 = rot_sbuf(S, "s1", (128, 2), n=2)
    for s0 in range(0, ntl, NB):
        n = min(NB, ntl - s0)
        S.load("sync", xb, xb[:, :, :n], xT[:, :, s0:s0 + n])
        S.load("scalar", go, go[:, :, :n], mo[0, :, :, s0:s0 + n])
        S.load("sync", gg, gg[:, :, :n], mo[1, :, :, s0:s0 + n])
        S.load("scalar", ob, ob[:, 8:16, :n], mo[2, :, :, s0:s0 + n])
        for hh in range(4):
            for c in (2 * hh, 2 * hh + 1):
                q = sq.get()
                S.op("scalar", lambda e: e.activation(out=q[:, :n], in_=go[:, c, :n], func=AF.Square), reads=[go], writes=[q])
                S.op("tensor", lambda e: e.matmul(ps_s[:, :n], ones[:], q[:, :n], start=(c % 2 == 0), stop=(c % 2 == 1)), reads=[ones, q], writes=[ps_s])
            S.op("vector", lambda e: e.tensor_scalar(out=rstd[:, :n], in0=ps_s[:, :n], scalar1=NORM_EPS * 256, scalar2=None, op0=ALU.add),
                 reads=[ps_s], writes=[rstd])
            S.op("scalar", lambda e: e.activation(out=rstd[:, :n], in_=rstd[:, :n], func=AF.Sqrt), reads=[rstd], writes=[rstd])
            S.op("vector", lambda e: e.reciprocal(out=rstd[:, :n], in_=rstd[:, :n]), reads=[rstd], writes=[rstd])
            for c in (2 * hh, 2 * hh + 1):
                t = tmp.get()
                S.op("vector", lambda e: e.scalar_tensor_tensor(out=t[:, :n], in0=go[:, c, :n], scalar=gnorm[:, c % 2:c % 2 + 1], in1=rstd[:, :n],
                                                                op0=ALU.mult, op1=ALU.mult), reads=[go, gnorm, rstd], writes=[t])
                S.op("gpsimd", lambda e: e.tensor_tensor(out=ob[:, c, :n], in0=t[:, :n], in1=gg[:, c, :n], op=ALU.mult), reads=[t, gg], writes=[ob])
        for dc in range(KC):
            w = wo.get()
            S.load("sync" if dc % 2 == 0 else "scalar", w, w[:], wo_b[dc], src=wo_b)
            p = ps_y.get()
            for kc in range(KC):
                S.op("tensor", lambda e: e.matmul(p[:, :n], w[:, kc * 128:(kc + 1) * 128], ob[:, kc, :n], start=(kc == 0), stop=(kc == KC - 1)),
                     reads=[w, ob], writes=[p])
            S.op("scalar", lambda e: e.activation(out=yb[:, dc, :n], in_=p[:, :n], func=AF.Copy), reads=[p], writes=[yb])
        rms_stats(S, cfg, yb, n, sq, ones, ps_s, rstd)
        resid_norm_add(S, cfg, xb, yb, n, rstd, G1[0], tmp)
        S.store("sync", x3T, x3T[:, :, s0:s0 + n], xb, xb[:, :, :n])
        rms_stats(S, cfg, xb, n, sq, ones, ps_s, rstd)
        norm_mod_apply(S, cfg, xb, n, rstd, A2[0], B2[0], hf, tmp)
        S.op("scalar", lambda e: e.activation(out=hb[:, :, :n], in_=hf[:, :, :n], func=AF.Copy), reads=[hf], writes=[hb])
        S.store("scalar", h2T, h2T[:, :, s0:s0 + n], hb, hb[:, :, :n])
        for tb in range(0, n if ROUTER_ON else 0, 128):
            for kc in range(KC):
                S.op("tensor", lambda e: e.matmul(ps_r[:, 0:NE], hf[:, kc, tb:tb + 128], router[:, kc, :], start=(kc == 0), stop=(kc == KC - 1)),
                     reads=[hf, router], writes=[ps_r])
            l_, m_, e_, k_, s_ = lg.get(), m8.get(), ex.get(), mk.get(), s1.get()
            X = mybir.AxisListType.X
            BIGV = 1.0e30
            S.op("vector", lambda e: e.tensor_copy(out=l_[:], in_=ps_r[:, 0:NE]), reads=[ps_r], writes=[l_])
            S.op("vector", lambda e: e.reduce_max(out=s_[:, 0:1], in_=l_[:], axis=X), reads=[l_], writes=[s_])
            S.op("vector", lambda e: e.tensor_scalar(out=l_[:], in0=l_[:], scalar1=s_[:, 0:1], scalar2=None, op0=ALU.subtract), reads=[l_, s_], writes=[l_])
            S.op("vector", lambda e: e.tensor_scalar(out=m_[:], in0=l_[:], scalar1=BIGV, scalar2=1.0, op0=ALU.mult, op1=ALU.add), reads=[l_], writes=[m_])
            S.op("vector", lambda e: e.tensor_scalar(out=m_[:], in0=m_[:], scalar1=0.0, scalar2=-BIGV, op0=ALU.max, op1=ALU.mult), reads=[m_], writes=[m_])
            S.op("vector", lambda e: e.tensor_tensor(out=m_[:], in0=m_[:], in1=l_[:], op=ALU.add), reads=[m_, l_], writes=[m_])
            S.op("vector", lambda e: e.reduce_max(out=s_[:, 1:2], in_=m_[:], axis=X), reads=[m_], writes=[s_])
            S.op("vector", lambda e: e.tensor_scalar(out=k_[:], in0=l_[:], scalar1=s_[:, 1:2], scalar2=BIGV, op0=ALU.subtract, op1=ALU.mult),
                 reads=[l_, s_], writes=[k_])
            S.op("vector", lambda e: e.tensor_scalar(out=k_[:], in0=k_[:], scalar1=1.0, scalar2=0.0, op0=ALU.add, op1=ALU.max), reads=[k_], writes=[k_])
            S.op("vector", lambda e: e.tensor_scalar(out=k_[:], in0=k_[:], scalar1=1.0, scalar2=None, op0=ALU.min), reads=[k_], writes=[k_])
            S.op("scalar", lambda e: e.activation(out=e_[:], in_=l_[:], func=AF.Exp), reads=[l_], writes=[e_])
            S.op("vector", lambda e: e.tensor_tensor(out=e_[:], in0=e_[:], in1=k_[:], op=ALU.mult), reads=[e_, k_], writes=[e_])
            S.op("vector", lambda e: e.reduce_sum(out=s_[:, 0:1], in_=e_[:], axis=X), reads=[e_], writes=[s_])
            S.op("vector", lambda e: e.reciprocal(out=s_[:, 0:1], in_=s_[:, 0:1]), reads=[s_], writes=[s_])
            S.op("vector", lambda e: e.tensor_scalar(out=e_[:], in0=e_[:], scalar1=s_[:, 0:1], scalar2=None, op0=ALU.mult), reads=[e_, s_], writes=[e_])
            S.store("sync", gates, gates[s0 + tb:s0 + tb + 128, :], e_, e_[:])
    return nc, stack, S


def run_l5(cfg, x2T_lat, gla_o, gla_g, rw, mod1, inp):
    nc, stack, S = build_l5(cfg)
    KC, ntl = cfg.KC, cfg.ntl
    wo = inp["l1_w_out"]
    wo_l = np.ascontiguousarray(wo.reshape(KC, 128, KC, 128).transpose(2, 1, 0, 3).reshape(KC, 128, KC * 128))
    gains = np.ascontiguousarray(np.stack([vec_fm(inp[k]) for k in ("l1_norm_mix_post", "l1_norm_ffn_pre")], axis=1))
    gnorm = np.ascontiguousarray(inp["l1_gla_norm"].reshape(2, 128).T)
    router = wfm(inp["l1_moe_router"])
    maps = []
    for i in range(NCORES):
        sl = slice(i * ntl, (i + 1) * ntl)
        mo = np.stack([fm(np.ascontiguousarray(a[:, sl])) for a in (gla_o, gla_g, rw)], axis=0)
        maps.append({"xT": fm(np.ascontiguousarray(x2T_lat[:, sl])), "mo": mo, "mod": mod1, "gains": gains, "gnorm": gnorm, "wo": wo_l, "router": router})
    res = run_prog(nc, stack, S, maps)
    x3T = np.concatenate([unfm(res[i]["x3T"]) for i in range(NCORES)], axis=1)
    h2T = np.concatenate([unfm(res[i]["h2T"]) for i in range(NCORES)], axis=1)
    gates = np.concatenate([res[i]["gates"] for i in range(NCORES)], axis=0)
    return x3T, h2T, gates


def build_moe(cfg):
    nc, stack, S = new_prog()
    KC, SS = cfg.KC, cfg.S
    NJ = cfg.D_FF_E // 128
    NB = 512
    hT = S.dram("hT", (128, KC, SS), BF16, "ExternalInput")
    gate_d = S.dram("gate", (128, SS), F32, "ExternalInput")
    wg_d = S.dram("wg", (NJ, 128, KC * 128), F32, "ExternalInput")
    wu_d = S.dram("wu", (NJ, 128, KC * 128), F32, "ExternalInput")
    wd_d = S.dram("wd", (KC, 128, NJ * 128), F32, "ExternalInput")
    out = S.dram("part", (128, KC, SS), BF16, "ExternalOutput")
    wg_b = S.dram("wg_b", (NJ, 128, KC * 128), BF16)
    wu_b = S.dram("wu_b", (NJ, 128, KC * 128), BF16)
    wd_b = S.dram("wd_b", (KC, 128, NJ * 128), BF16)
    for _ph in (S.mark(),):
        sf, sb = rot_sbuf(S, "cv_f", (128, 2048), F32, n=4), rot_sbuf(S, "cv_b", (128, 2048), BF16, n=4)
        ctr = [0]
        convert_w(S, wg_d, wg_b, NJ, KC * 128, sf, sb, ctr)
        convert_w(S, wu_d, wu_b, NJ, KC * 128, sf, sb, ctr)
        convert_w(S, wd_d, wd_b, KC, NJ * 128, sf, sb, ctr)
        barrier(S)
        S.reset(_ph)
    fb = ffn_bufs(S, cfg, NJ, NB)
    hbs = rot_sbuf(S, "hb", (128, KC, NB), BF16, n=2)
    gbs = rot_sbuf(S, "gb", (128, NB), F32, n=2)
    obs = rot_sbuf(S, "obs", (128, NB), BF16, n=4)
    for s0 in range(0, SS, NB):
        n = min(NB, SS - s0)
        hb, gb = hbs.get(), gbs.get()
        S.load("sync", hb, hb[:, :, :n], hT[:, :, s0:s0 + n])
        S.load("scalar", gb, gb[:, :n], gate_d[:, s0:s0 + n])

        def evac(dc, po):
            o = obs.get()
            S.op("vector", lambda e: e.tensor_tensor(out=o[:, :n], in0=gb[:, :n], in1=po[:, :n], op=ALU.mult), reads=[gb, po], writes=[o])
            S.store("sync" if dc % 2 == 0 else "scalar", out, out[:, dc, s0:s0 + n], o, o[:, :n])
        ffn_block(S, cfg, hb, n, NJ, wg_b, wu_b, wd_b, fb, evac)
    return nc, stack, S


def run_moe(cfg, h2T, gates, inp):
    nc, stack, S = build_moe(cfg)
    hfm = fm(h2T)
    maps = []
    for e in range(NCORES):
        wg, wu, wd = host_ffn_w(inp["l1_moe_w_gate"][e], inp["l1_moe_w_up"][e], inp["l1_moe_w_down"][e])
        g = np.ascontiguousarray(np.broadcast_to(gates[:, e][None, :], (128, cfg.S)))
        maps.append({"hT": hfm, "gate": g, "wg": wg, "wu": wu, "wd": wd})
    res = run_prog(nc, stack, S, maps)
    return [res[e]["part"] for e in range(NCORES)]


def build_fin(cfg):
    nc, stack, S = new_prog()
    KC, NB, ntl = cfg.KC, 256, cfg.ntl
    NE = cfg.N_EXP
    xT = S.dram("xT", (128, KC, ntl), F32, "ExternalInput")
    parts = S.dram("parts", (NE, 128, KC, ntl), BF16, "ExternalInput")
    mod_d = S.dram("mod", (128, 6 * KC, 2), F32, "ExternalInput")
    gain_d = S.dram("gain", (128, KC), F32, "ExternalInput")
    outT = S.dram("outT", (128, KC, ntl), F32, "ExternalOutput")
    mod = load_const(S, "mod_sb", (128, 6 * KC, 2), mod_d[:])
    gain = load_const(S, "gain_sb", (128, KC), gain_d[:])
    ones = make_ones(S)
    G2 = gate_scalars(S, cfg, mod, gain, 5, "g2")
    xb = rot_sbuf(S, "xb", (128, KC, NB), n=2)
    pb = rot_sbuf(S, "pb", (128, KC, NB), BF16, n=4)
    fbuf = S.sbuf("fb", (128, KC, NB))
    sq = rot_sbuf(S, "sq", (128, NB))
    tmp = rot_sbuf(S, "tmp", (128, NB), n=4)
    rstd = S.sbuf("rstd", (128, NB))
    ps_s = S.psum("ps_stat")
    for s0 in range(0, ntl, NB):
        n = min(NB, ntl - s0)
        x = xb.get()
        S.load("sync", x, x[:, :, :n], xT[:, :, s0:s0 + n])
        for e_ in range(NE):
            p = pb.get()
            S.load("scalar" if e_ % 2 else "sync", p, p[:, :, :n], parts[e_, :, :, s0:s0 + n])
            eng = "vector" if e_ % 2 == 0 else "gpsimd"
            if e_ == 0:
                S.op(eng, lambda e: e.tensor_copy(out=fbuf[:, :, :n], in_=p[:, :, :n]), reads=[p], writes=[fbuf])
            else:
                S.op(eng, lambda e: e.tensor_tensor(out=fbuf[:, :, :n], in0=fbuf[:, :, :n], in1=p[:, :, :n], op=ALU.add), reads=[fbuf, p], writes=[fbuf])
        rms_stats(S, cfg, fbuf, n, sq, ones, ps_s, rstd)
        resid_norm_add(S, cfg, x, fbuf, n, rstd, G2[0], tmp)
        S.store("sync", outT, outT[:, :, s0:s0 + n], x, x[:, :, :n])
    return nc, stack, S


def run_fin(cfg, x3T, parts, mod1, inp):
    nc, stack, S = build_fin(cfg)
    ntl = cfg.ntl
    maps = []
    for i in range(NCORES):
        sl = slice(i * ntl, (i + 1) * ntl)
        maps.append({"xT": fm(np.ascontiguousarray(x3T[:, sl])), "parts": np.ascontiguousarray(np.stack([p[:, :, sl] for p in parts], axis=0)),
                     "mod": mod1, "gain": vec_fm(inp["l1_norm_ffn_post"])})
    res = run_prog(nc, stack, S, maps)
    return np.concatenate([unfm(res[i]["outT"]) for i in range(NCORES)], axis=1)


def kernel(**inp):
    inp = {k: np.asarray(v) for k, v in inp.items()}
    S_len, L_len = inp["x"].shape[1], inp["ctx"].shape[1]
    cfg = Cfg(S=S_len, L=L_len, D=inp["x"].shape[2], D_FF=inp["l0_ffn_w_gate"].shape[1], N_EXP=inp["l1_moe_w_gate"].shape[0],
              D_FF_E=inp["l1_moe_w_gate"].shape[2])
    xT_lat = np.ascontiguousarray(inp["x"][0].T)
    xT_ctx = np.ascontiguousarray(inp["ctx"][0].T)
    mods = run_ada(cfg, inp)
    hT0 = run_pre(cfg, xT_lat, xT_ctx, mods[0], inp["l0_norm_mix_pre"])
    oT0 = run_mix0(cfg, hT0, inp)
    x2c, x2l, hT1 = run_l3(cfg, xT_lat, xT_ctx, oT0, mods, inp)
    gla_o, gla_g, rw = run_mix1(cfg, hT1, inp)
    x3T, h2T, gates = run_l5(cfg, x2l, gla_o, gla_g, rw, mods[1], inp)
    parts = run_moe(cfg, h2T, gates, inp)
    outT = run_fin(cfg, x3T, parts, mods[1], inp)
    return np.ascontiguousarray(outT.T)[None].astype(np.float32)
```

```python
import contextlib
import numpy as np
import ml_dtypes
import concourse.bass as bass
import concourse.mybir as mybir
from concourse.bass_utils import run_bass_kernel_spmd

F32 = mybir.dt.float32
BF16 = mybir.dt.bfloat16
U8 = mybir.dt.uint8
AF = mybir.ActivationFunctionType
ALU = mybir.AluOpType
NCORES = 8
NORM_EPS = 1e-6
DEBUG = False
DEBUG_BI = 0
RECYCLE_SEMS = True
ROUTER_ON = True
SKIP = set()


class Cfg:
    def __init__(self, S=16384, L=256, D=2048, D_FF=5632, N_EXP=8, D_FF_E=7168):
        self.S, self.L, self.D, self.D_FF, self.N_EXP, self.D_FF_E = S, L, D, D_FF, N_EXP, D_FF_E
        self.T = S + L
        self.KC = D // 128
        self.ntl = S // NCORES
        self.ntc = L // NCORES
        self.ntok = self.ntl + self.ntc


class Tok:
    __slots__ = ("sem", "count", "closed", "deps")

    def __init__(self, sem, count):
        self.sem, self.count, self.closed, self.deps = sem, count, False, []


class Buf:
    def __init__(self, name, t=None, excl=False, accum=False):
        self.name, self.t, self.excl, self.accum = name, t, excl, accum
        self.writers, self.readers = {}, {}
        self.dma_sem, self.dma_tok = None, None

    def __getitem__(self, idx):
        return self.t[idx]


class _Rec:
    def __getattr__(self, name):
        def f(*a, **k):
            self.call = (name, a, k)
        return f


class Eng:
    def __init__(self, name, sem):
        self.name, self.sem, self.count, self.items, self.seen = name, sem, 0, [], {}


class Sched:
    ENGS = ("sync", "scalar", "vector", "gpsimd", "tensor")

    def __init__(self, nc, stack):
        self.nc, self.stack, self.root = nc, stack, stack
        self.nsem = 0
        self.free_sems = []
        self._init_mem()
        self.eng = {n: Eng(n, self.new_sem("e_" + n)) for n in self.ENGS}
        self.dma_keys = []
        self.nbuf = 0

    def new_sem(self, name=None):
        self.nsem += 1
        return self.root.enter_context(self.nc.semaphore(name or f"s{self.nsem}"))

    ARENA_WORDS = 45056

    def _init_mem(self):
        self.arena = self.root.enter_context(self.nc.sbuf_tensor("arena", [128, self.ARENA_WORDS], F32))
        self.banks = [self.root.enter_context(self.nc.psum_tensor(f"bank{i}", [128, 512], F32)) for i in range(8)]
        self.aoff, self.pidx = 0, 0

    def sbuf(self, name, shape, dt=F32):
        esz = {F32: 4, BF16: 2, U8: 1}[dt]
        nel = int(np.prod(shape[1:]))
        words = (nel * esz + 3) // 4
        assert self.aoff + words <= self.ARENA_WORDS, f"SBUF arena overflow at {name}: {self.aoff}+{words}"
        ap = self.arena[:, self.aoff:self.aoff + words]
        self.aoff += words
        if dt != F32:
            ap = ap.bitcast(dt)[:, 0:nel]
        if len(shape) == 3:
            ap = ap.rearrange("p (a b) -> p a b", b=shape[2])
        if shape[0] < 128:
            ap = ap[0:shape[0]]
        return Buf(name, ap)

    def psum(self, name, shape=(128, 512), dt=F32):
        assert self.pidx < 8, "out of PSUM banks"
        b = Buf(name, self.banks[self.pidx][:], excl=True)
        self.pidx += 1
        return b

    def mark(self):
        return (self.aoff, self.pidx, len(self.dma_keys))

    def reset(self, m):
        self.aoff, self.pidx = m[0], m[1]
        for key in self.dma_keys[m[2]:]:
            if RECYCLE_SEMS:
                self.free_sems.append((key.dma_sem, key.dma_tok.count))
            key.dma_sem, key.dma_tok = None, None
        del self.dma_keys[m[2]:]

    def dram(self, name, shape, dt, kind="Internal"):
        t = self.nc.dram_tensor(name, list(shape), dt, kind=kind)
        return Buf(name, t.ap(), accum=True)

    def _wait(self, E, tok, raw=False):
        if tok.sem is E.sem and (not raw or E.name == "tensor"):
            return
        tok.closed = True
        k = id(tok.sem)
        if E.seen.get(k, 0) >= tok.count:
            return
        E.seen[k] = tok.count
        E.items.append(("wait", tok.sem, tok.count))

    def _hazards(self, E, reads, writes, skip=None):
        deps = []
        for b in reads:
            if b.excl:
                continue
            for t in b.writers.values():
                if t is not skip:
                    self._wait(E, t, raw=True)
                    deps.append(t)
        for b in list(writes) + [b for b in reads if b.excl]:
            if not b.accum:
                for t in b.writers.values():
                    if t is not skip:
                        self._wait(E, t, raw=(b.excl and b in reads))
                        deps.append(t)
            for t in b.readers.values():
                if t is not skip:
                    self._wait(E, t)
                    deps.append(t)
        return deps

    def _record(self, tok, reads, writes):
        k = id(tok.sem)
        for b in reads:
            if b.excl:
                b.writers, b.readers = {k: tok}, {}
            else:
                b.readers[k] = tok
        for b in writes:
            if b.accum:
                b.writers[k] = tok
                b.readers = {}
            else:
                b.writers, b.readers = {k: tok}, {}

    def op(self, eng, fn, reads=(), writes=()):
        E = self.eng[eng]
        self._hazards(E, reads, writes)
        E.count += 1
        tok = Tok(E.sem, E.count)
        rec = _Rec()
        fn(rec)
        E.items.append(("op", rec.call))
        self._record(tok, reads, writes)

    def dma(self, q, out, in_, key, reads=(), writes=(), **kw):
        E = self.eng[q]
        cur = key.dma_tok if (key.dma_tok is not None and not key.dma_tok.closed) else None
        deps = self._hazards(E, reads, writes, skip=cur)
        if cur is not None:
            for t in cur.deps:
                self._wait(E, t)
            cur.deps.extend(deps)
        if key.dma_sem is None:
            if self.free_sems:
                key.dma_sem, cnt = self.free_sems.pop()
                key.dma_tok = Tok(key.dma_sem, cnt)
                key.dma_tok.closed = True
            else:
                key.dma_sem = self.new_sem()
            self.dma_keys.append(key)
        t = key.dma_tok
        if t is None or t.closed:
            if t is not None:
                self._wait(E, t)
            t = Tok(key.dma_sem, t.count if t is not None else 0)
            t.deps = list(deps)
            key.dma_tok = t
        t.count += 16
        E.items.append(("dma", out, in_, key.dma_sem, kw))
        self._record(t, reads, writes)

    def load(self, q, dst, dst_ap, src_ap, src=None, **kw):
        self.dma(q, dst_ap, src_ap, key=dst, reads=([src] if src is not None else []), writes=[dst], **kw)

    def store(self, q, dst, dst_ap, src, src_ap, **kw):
        self.dma(q, dst_ap, src_ap, key=src, reads=[src], writes=[dst], **kw)

    def finish(self):
        E = self.eng["sync"]
        for key in self.dma_keys:
            if key.dma_tok is not None:
                self._wait(E, key.dma_tok)
        for n in self.ENGS:
            if n != "sync" and self.eng[n].count:
                self._wait(E, Tok(self.eng[n].sem, self.eng[n].count))

    def emit(self):
        self.finish()
        with self.nc.Block() as block:
            for n in self.ENGS:
                E = self.eng[n]

                def body(e, E=E):
                    for it in E.items:
                        if it[0] == "wait":
                            e.wait_ge(it[1], it[2])
                        elif it[0] == "op":
                            nm, a, k = it[1]
                            getattr(e, nm)(*a, **k).then_inc(E.sem, 1)
                        else:
                            e.dma_start(out=it[1], in_=it[2], **it[4]).then_inc(it[3], 16)

                getattr(block, n)(body)


def new_prog():
    nc = bass.Bass("TRN2", target_bir_lowering=False)
    stack = contextlib.ExitStack()
    return nc, stack, Sched(nc, stack)


def run_prog(nc, stack, S, in_maps):
    S.emit()
    stack.close()
    res = run_bass_kernel_spmd(nc, in_maps, core_ids=list(range(NCORES)))
    return res.results


def make_ones(S, name="ones", dt=F32):
    ones = S.sbuf(name, (128, 128), dt)
    S.op("gpsimd", lambda e: e.memset(ones[:], 1.0), writes=[ones])
    return ones


def load_const(S, name, shape, dram_ap, dt=F32, q="sync"):
    b = S.sbuf(name, shape, dt)
    S.load(q, b, b[:], dram_ap)
    return b


def mod_scalars(S, cfg, mod, gain, v_scale, v_shift, tag):
    KC = cfg.KC
    A, B = [], []
    for r in range(2):
        a = S.sbuf(f"A_{tag}{r}", (128, KC))
        bb = S.sbuf(f"B_{tag}{r}", (128, KC))
        S.op("vector", lambda e, a=a, r=r: e.tensor_scalar(
            out=a[:], in0=mod[:, v_scale * KC:(v_scale + 1) * KC, r], scalar1=1.0, scalar2=float(cfg.D) ** 0.5,
            op0=ALU.add, op1=ALU.mult), reads=[mod], writes=[a])
        S.op("vector", lambda e, a=a: e.tensor_tensor(out=a[:], in0=a[:], in1=gain[:], op=ALU.mult), reads=[a, gain], writes=[a])
        S.op("vector", lambda e, bb=bb, r=r: e.tensor_copy(out=bb[:], in_=mod[:, v_shift * KC:(v_shift + 1) * KC, r]),
             reads=[mod], writes=[bb])
        A.append(a)
        B.append(bb)
    return A, B


def rms_stats(S, cfg, x, n, sq, ones, ps, rstd, nchunks=None, c0=0):
    nch = nchunks or cfg.KC
    Dn = nch * 128
    for c in range(nch):
        q = sq.get()
        S.op("scalar", lambda e: e.activation(out=q[:, :n], in_=x[:, c0 + c, :n], func=AF.Square), reads=[x], writes=[q])
        S.op("tensor", lambda e: e.matmul(ps[:, :n], ones[:], q[:, :n], start=(c == 0), stop=(c == nch - 1)),
             reads=[ones, q], writes=[ps])
    S.op("vector", lambda e: e.tensor_scalar(out=rstd[:, :n], in0=ps[:, :n], scalar1=NORM_EPS * Dn, scalar2=None,
                                             op0=ALU.add), reads=[ps], writes=[rstd])
    S.op("scalar", lambda e: e.activation(out=rstd[:, :n], in_=rstd[:, :n], func=AF.Sqrt), reads=[rstd], writes=[rstd])
    S.op("vector", lambda e: e.reciprocal(out=rstd[:, :n], in_=rstd[:, :n]), reads=[rstd], writes=[rstd])


def norm_mod_apply(S, cfg, x, n, rstd, A, B, out, tmp):
    for c in range(cfg.KC):
        t = tmp.get()
        S.op("vector", lambda e: e.scalar_tensor_tensor(out=t[:, :n], in0=x[:, c, :n], scalar=A[:, c:c + 1],
                                                        in1=rstd[:, :n], op0=ALU.mult, op1=ALU.mult),
             reads=[x, A, rstd], writes=[t])
        S.op("gpsimd", lambda e: e.tensor_scalar(out=out[:, c, :n], in0=t[:, :n], scalar1=B[:, c:c + 1],
                                                 scalar2=None, op0=ALU.add), reads=[t, B], writes=[out])


def resid_norm_add(S, cfg, x, y, n, rstd, G, tmp):
    for c in range(cfg.KC):
        t = tmp.get()
        S.op("gpsimd", lambda e: e.tensor_tensor(out=t[:, :n], in0=y[:, c, :n], in1=rstd[:, :n], op=ALU.mult),
             reads=[y, rstd], writes=[t])
        S.op("vector", lambda e: e.scalar_tensor_tensor(out=x[:, c, :n], in0=t[:, :n], scalar=G[:, c:c + 1], in1=x[:, c, :n],
                                                        op0=ALU.mult, op1=ALU.add), reads=[t, G, x], writes=[x])


def gate_scalars(S, cfg, mod, gain, v_gate, tag):
    KC = cfg.KC
    G = []
    for r in range(2):
        g = S.sbuf(f"G_{tag}{r}", (128, KC))
        S.op("vector", lambda e: e.tensor_scalar(out=g[:], in0=mod[:, v_gate * KC:(v_gate + 1) * KC, r], scalar1=float(cfg.D) ** 0.5,
                                                 scalar2=None, op0=ALU.mult), reads=[mod], writes=[g])
        S.op("vector", lambda e: e.tensor_tensor(out=g[:], in0=g[:], in1=gain[:], op=ALU.mult), reads=[g, gain], writes=[g])
        G.append(g)
    return G


def token_blocks(cfg, bs=512):
    blocks = []
    for s in range(0, cfg.ntl, bs):
        blocks.append((s, min(bs, cfg.ntl - s), 0))
    blocks.append((cfg.ntl, cfg.ntc, 1))
    return blocks


def build_ada(cfg):
    nc, stack, S = new_prog()
    KC = cfg.KC
    NJ = 6 * KC // NCORES
    c2 = S.dram("c2", (128, KC, 2), F32, "ExternalInput")
    outs = []
    ones = None
    sT = S.sbuf("sT", (128, KC, 2))
    S.load("sync", sT, sT[:], c2[:])
    S.op("scalar", lambda e: e.activation(out=sT[:], in_=sT[:], func=AF.Silu), reads=[sT], writes=[sT])
    ps = [S.psum(f"ps{i}") for i in range(2)]
    for l in range(2):
        w = S.dram(f"w{l}", (128, KC, NJ * 128), F32, "ExternalInput")
        b = S.dram(f"b{l}", (128, NJ), F32, "ExternalInput")
        o = S.dram(f"mod{l}", (128, NJ, 2), F32, "ExternalOutput")
        if l == 0:
            wt_shared = S.sbuf("wt", (128, KC, NJ * 128))
        wt = wt_shared
        bt = S.sbuf(f"bt{l}", (128, NJ))
        ot = S.sbuf(f"ot{l}", (128, NJ, 2))
        for kc in range(KC):
            S.load("sync" if kc % 2 == 0 else "scalar", wt, wt[:, kc, :], w[:, kc, :])
        S.load("sync", bt, bt[:], b[:])
        for j in range(NJ):
            p = ps[j % 2]
            for kc in range(KC):
                S.op("tensor", lambda e, p=p, j=j, kc=kc, wt=wt: e.matmul(p[:, 0:2], wt[:, kc, j * 128:(j + 1) * 128], sT[:, kc, :],
                                                                      start=(kc == 0), stop=(kc == KC - 1)),
                     reads=[wt, sT], writes=[p])
            S.op("vector", lambda e, p=p, j=j, ot=ot, bt=bt: e.tensor_scalar(out=ot[:, j, :], in0=p[:, 0:2], scalar1=bt[:, j:j + 1],
                                                                           scalar2=None, op0=ALU.add),
                 reads=[p, bt], writes=[ot])
        S.store("sync", o, o[:], ot, ot[:])
    return nc, stack, S


def host_ada(cfg, inp):
    KC = cfg.KC
    NJ = 6 * KC // NCORES
    c2 = np.stack([inp["c"][0], inp["c_ctx"]], axis=-1)
    c2 = np.ascontiguousarray(c2.reshape(KC, 128, 2).transpose(1, 0, 2))
    maps = []
    for i in range(NCORES):
        m = {"c2": c2}
        for l in range(2):
            w = inp[f"l{l}_ada_w"][:, i * NJ * 128:(i + 1) * NJ * 128]
            m[f"w{l}"] = np.ascontiguousarray(w.reshape(KC, 128, NJ * 128).transpose(1, 0, 2))
            b = inp[f"l{l}_ada_b"][i * NJ * 128:(i + 1) * NJ * 128]
            m[f"b{l}"] = np.ascontiguousarray(b.reshape(NJ, 128).T)
        maps.append(m)
    return maps


def run_ada(cfg, inp):
    nc, stack, S = build_ada(cfg)
    res = run_prog(nc, stack, S, host_ada(cfg, inp))
    mods = []
    for l in range(2):
        mods.append(np.ascontiguousarray(np.concatenate([res[i][f"mod{l}"] for i in range(NCORES)], axis=1)))
    return mods


def build_pre(cfg):
    nc, stack, S = new_prog()
    KC = cfg.KC
    xT = S.dram("xT", (128, KC, cfg.ntok), F32, "ExternalInput")
    mod_d = S.dram("mod", (128, 6 * KC, 2), F32, "ExternalInput")
    gain_d = S.dram("gain", (128, KC), F32, "ExternalInput")
    hT = S.dram("hT", (128, KC, cfg.ntok), BF16, "ExternalOutput")
    mod = load_const(S, "mod_sb", (128, 6 * KC, 2), mod_d[:])
    gain = load_const(S, "gain_sb", (128, KC), gain_d[:])
    ones = make_ones(S)
    A, B = mod_scalars(S, cfg, mod, gain, 1, 0, "m")
    xb = [S.sbuf(f"xb{i}", (128, KC, 512)) for i in range(2)]
    sq = rot_sbuf(S, "sq", (128, 512))
    tmp = rot_sbuf(S, "tmp", (128, 512))
    hb = [S.sbuf(f"hb{i}", (128, KC, 512), BF16) for i in range(2)]
    rstd = S.sbuf("rstd", (128, 512))
    ps = S.psum("ps")
    for bi, (s0, n, kind) in enumerate(token_blocks(cfg)):
        x = xb[bi % 2]
        h = hb[bi % 2]
        S.load("sync", x, x[:, :, :n], xT[:, :, s0:s0 + n])
        rms_stats(S, cfg, x, n, sq, ones, ps, rstd)
        norm_mod_apply(S, cfg, x, n, rstd, A[kind], B[kind], h, tmp)
        S.store("sync", hT, hT[:, :, s0:s0 + n], h, h[:, :, :n])
    return nc, stack, S


def fm(a):
    D, n = a.shape
    return np.ascontiguousarray(a.reshape(D // 128, 128, n).transpose(1, 0, 2))


def unfm(a):
    p, C, n = a.shape
    return np.ascontiguousarray(a.transpose(1, 0, 2).reshape(C * 128, n))


def vec_fm(v):
    return np.ascontiguousarray(v.reshape(-1, 128).T)


def own_tokens_T(cfg, xT_lat, xT_ctx, i):
    return np.concatenate([xT_lat[:, i * cfg.ntl:(i + 1) * cfg.ntl], xT_ctx[:, i * cfg.ntc:(i + 1) * cfg.ntc]], axis=1)


def gather_tokens_T(cfg, per_core):
    lat = np.concatenate([a[:, :cfg.ntl] for a in per_core], axis=1)
    ctx = np.concatenate([a[:, cfg.ntl:] for a in per_core], axis=1)
    return ctx, lat


def run_pre(cfg, xT_lat, xT_ctx, mod, gain):
    nc, stack, S = build_pre(cfg)
    maps = [{"xT": fm(own_tokens_T(cfg, xT_lat, xT_ctx, i)), "mod": mod, "gain": vec_fm(gain)} for i in range(NCORES)]
    res = run_prog(nc, stack, S, maps)
    if DEBUG:
        global DBG
        DBG = res[0]
    ctx, lat = gather_tokens_T(cfg, [unfm(res[i]["hT"]) for i in range(NCORES)])
    return np.concatenate([ctx, lat], axis=1)


class Rot:
    def __init__(self, bufs):
        self.bufs, self.i = bufs, 0

    def get(self):
        b = self.bufs[self.i % len(self.bufs)]
        self.i += 1
        return b


def rot_sbuf(S, name, shape, dt=F32, n=2):
    return Rot([S.sbuf(f"{name}{i}", shape, dt) for i in range(n)])


def seq_blocks(cfg, bs=512):
    blocks = []
    for seg0, seglen in ((0, cfg.L), (cfg.L, cfg.S)):
        for s in range(0, seglen, bs):
            blocks.append((seg0 + s, min(bs, seglen - s), seg0, seglen))
    return blocks


def rev_pos(t0, n, seg0, seglen):
    return seg0 + seglen - (t0 - seg0) - n


def barrier(S):
    toks = [Tok(S.eng[n].sem, S.eng[n].count) for n in S.ENGS if S.eng[n].count]
    dtoks = [k.dma_tok for k in S.dma_keys if k.dma_tok is not None]
    for n in S.ENGS:
        E = S.eng[n]
        for t in toks + dtoks:
            if t.sem is not E.sem:
                S._wait(E, t)


def make_identity(S, ident_d, name="ident", dt=F32):
    return load_const(S, name, (128, 128), ident_d[:], dt)


def fm_group(S, ps, W, hb, g, n, KC):
    for kc in range(KC):
        S.op("tensor", lambda e, kc=kc: e.matmul(ps[:, :n], W[:, kc, g * 128:(g + 1) * 128], hb[:, kc, :n],
                                               start=(kc == 0), stop=(kc == KC - 1)), reads=[W, hb], writes=[ps])


def load_w_bf16(S, name, w_d, KC, ncols):
    W = S.sbuf(name, (128, KC, ncols), BF16)
    for kc in range(KC):
        S.load("gpsimd", W, W[:, kc, :], w_d[:, kc, :])
    return W


def chunk_scan(S, cfg, units, consts):
    T = cfg.T
    ident, zero1, cmask, mask_ui, m96 = consts["ident"], consts["zero1"], consts["cmask"], consts["mask_ui"], consts["m96"]
    nu = len(units)
    SB = 512
    for u, U in enumerate(units):
        K, V = U["K"], U["V"]
        U["in"] = {nm: rot_sbuf(S, f"u{u}_{nm}", (128, SB)) for nm in ("q", "k", "lw", "v")}
        U["L"] = S.sbuf(f"u{u}_L", (128, SB))
        U["Ep"] = S.sbuf(f"u{u}_Ep", (128, SB))
        U["En"] = S.sbuf(f"u{u}_En", (128, SB))
        U["qh"] = S.sbuf(f"u{u}_qh", (128, SB))
        U["kh"] = S.sbuf(f"u{u}_kh", (128, SB))
        U["AT"] = S.sbuf(f"u{u}_AT", (128, 128))
        U["ktok"] = S.sbuf(f"u{u}_ktok", (128, 128))
        U["vtok"] = S.sbuf(f"u{u}_vtok", (128, 128))
        U["ktokz"] = S.sbuf(f"u{u}_ktokz", (128, 128))
        U["kvd"] = S.sbuf(f"u{u}_kvd", (128, 128))
        U["Z"] = [S.sbuf(f"u{u}_Z{i}", (128, 128)) for i in range(2)]
        U["zi"] = 0
        U["osb"] = rot_sbuf(S, f"u{u}_osb", (128, SB))
        U["ps_g"] = S.psum(f"u{u}_psg")
        U["ps_t"] = S.psum(f"u{u}_pst")
        U["ps_o"] = S.psum(f"u{u}_pso")
        U["ps_kv"] = S.psum(f"u{u}_pskv")
        S.op("gpsimd", lambda e, U=U: e.memset(U["AT"][:], 0.0), writes=[U["AT"]])
        S.op("gpsimd", lambda e, U=U: e.memset(U["Z"][0][:], 0.0), writes=[U["Z"][0]])
    for t0 in range(0, T, SB):
        n = min(SB, T - t0)
        for U in units:
            K, V = U["K"], U["V"]
            cur = {}
            for nm in ("q", "k", "lw", "v"):
                b = U["in"][nm].get()
                P = V if nm == "v" else K
                S.load("sync", b, b[:P, :n], U[nm][:P, t0:t0 + n], src=U[nm])
                cur[nm] = b
            L, Ep, En, qh, kh = U["L"], U["Ep"], U["En"], U["qh"], U["kh"]
            S.op("vector", lambda e, L=L, cur=cur, K=K: e.tensor_tensor_scan(
                out=L[:K, :n], data0=cmask[:K, :n], data1=cur["lw"][:K, :n], initial=zero1[:K, 0:1],
                op0=ALU.mult, op1=ALU.add), reads=[cmask, cur["lw"], zero1], writes=[L])
            S.op("scalar", lambda e, L=L, Ep=Ep, K=K: e.activation(out=Ep[:K, :n], in_=L[:K, :n], func=AF.Exp),
                 reads=[L], writes=[Ep])
            S.op("vector", lambda e, Ep=Ep, En=En, K=K: e.reciprocal(out=En[:K, :n], in_=Ep[:K, :n]), reads=[Ep], writes=[En])
            S.op("gpsimd", lambda e, qh=qh, cur=cur, Ep=Ep, K=K: e.tensor_tensor(out=qh[:K, :n], in0=cur["q"][:K, :n], in1=Ep[:K, :n],
                                                                              op=ALU.mult), reads=[cur["q"], Ep], writes=[qh])
            S.op("gpsimd", lambda e, kh=kh, cur=cur, En=En, K=K: e.tensor_tensor(out=kh[:K, :n], in0=cur["k"][:K, :n], in1=En[:K, :n],
                                                                              op=ALU.mult), reads=[cur["k"], En], writes=[kh])
            U["cur"] = cur
            U["o"] = U["osb"].get()
            if DEBUG and t0 == 0 and U is units[0]:
                for nm, bb in (("L", L), ("Ep", Ep), ("qh", qh), ("kh", kh), ("lwin", cur["lw"]), ("qin", cur["q"])):
                    dd = S.dram("dbg_" + nm, (128, 512), F32, "ExternalOutput")
                    S.store("sync", dd, dd[:, :n], bb, bb[:, :n])
        for j in range(0, n, 128):
            for U in units:
                K, V = U["K"], U["V"]
                qh, kh, Ep, cur = U["qh"], U["kh"], U["Ep"], U["cur"]
                AT, ktok, vtok, kvd, o = U["AT"], U["ktok"], U["vtok"], U["kvd"], U["o"]
                ps_g, ps_t, ps_o, ps_kv = U["ps_g"], U["ps_t"], U["ps_o"], U["ps_kv"]
                js = slice(j, j + 128)
                S.op("tensor", lambda e, K=K, kh=kh, qh=qh, ps_g=ps_g, js=js: e.matmul(ps_g[:, 0:128], kh[:K, js], qh[:K, js], start=True, stop=True),
                     reads=[kh, qh], writes=[ps_g])
                S.op("vector", lambda e, AT=AT, ps_g=ps_g: e.copy_predicated(out=AT[:], mask=mask_ui[:], data=ps_g[:, 0:128]),
                     reads=[ps_g, mask_ui], writes=[AT])
                S.op("tensor", lambda e, K=K, kh=kh, ps_t=ps_t, js=js: e.matmul(ps_t[:, 0:K], kh[:K, js], ident[:K, :K], is_transpose=True, start=True, stop=True),
                     reads=[kh, ident], writes=[ps_t])
                S.op("tensor", lambda e, V=V, cur=cur, ps_t=ps_t, js=js: e.matmul(ps_t[:, 128:128 + V], cur["v"][:V, js], ident[:V, :V], is_transpose=True, start=True, stop=True),
                     reads=[cur["v"], ident], writes=[ps_t])
                S.op("scalar", lambda e, K=K, ktok=ktok, ps_t=ps_t: e.activation(out=ktok[:, :K], in_=ps_t[:, 0:K], func=AF.Copy),
                     reads=[ps_t], writes=[ktok])
                S.op("vector", lambda e, V=V, vtok=vtok, ps_t=ps_t: e.tensor_copy(out=vtok[:, :V], in_=ps_t[:, 128:128 + V]),
                     reads=[ps_t], writes=[vtok])
                ktz = U["ktokz"]
                S.op("vector", lambda e, K=K, ktz=ktz, ps_t=ps_t: e.tensor_scalar(out=ktz[64:128, :K], in0=ps_t[64:128, 0:K], scalar1=m96[64:128, 0:1],
                                                                              scalar2=None, op0=ALU.mult), reads=[ps_t, m96], writes=[ktz])
                S.op("tensor", lambda e, V=V, vtok=vtok, AT=AT, ps_o=ps_o: e.matmul(ps_o[:V, 0:128], vtok[:, :V], AT[:], start=True, stop=False),
                     reads=[vtok, AT], writes=[ps_o])
            for c in range(4):
                for U in units:
                    K, V = U["K"], U["V"]
                    qh, Ep = U["qh"], U["Ep"]
                    ktok, vtok, kvd, ps_o, ps_kv = U["ktok"], U["vtok"], U["kvd"], U["ps_o"], U["ps_kv"]
                    Zc, Zn = U["Z"][U["zi"]], U["Z"][1 - U["zi"]]
                    U["zi"] = 1 - U["zi"]
                    cs = slice(j + 32 * c, j + 32 * c + 32)
                    wc = j + 32 * c + 31
                    S.op("tensor", lambda e, K=K, V=V, Zc=Zc, qh=qh, ps_o=ps_o, cs=cs, c=c: e.matmul(
                        ps_o[:V, 32 * c:32 * c + 32], Zc[:K, :V], qh[:K, cs], start=False, stop=(c == 3)),
                        reads=[Zc, qh], writes=[ps_o])
                    if c < 3:
                        S.op("tensor", lambda e, K=K, V=V, ktok=ktok, vtok=vtok, ps_kv=ps_kv, c=c: e.matmul(
                            ps_kv[:K, :V], ktok[32 * c:32 * c + 32, :K], vtok[32 * c:32 * c + 32, :V], start=True, stop=True),
                            reads=[ktok, vtok], writes=[ps_kv])
                    else:
                        ktz = U["ktokz"]
                        S.op("tensor", lambda e, K=K, V=V, ktz=ktz, vtok=vtok, ps_kv=ps_kv: e.matmul(
                            ps_kv[:K, :V], ktz[64:128, :K], vtok[64:128, :V], start=True, stop=True),
                            reads=[ktz, vtok], writes=[ps_kv])
                    S.op("vector", lambda e, K=K, V=V, kvd=kvd, ps_kv=ps_kv, Ep=Ep, wc=wc: e.tensor_scalar(
                        out=kvd[:K, :V], in0=ps_kv[:K, :V], scalar1=Ep[:K, wc:wc + 1], scalar2=None, op0=ALU.mult),
                        reads=[ps_kv, Ep], writes=[kvd])
                    S.op("vector", lambda e, K=K, V=V, Zn=Zn, Zc=Zc, kvd=kvd, Ep=Ep, wc=wc: e.scalar_tensor_tensor(
                        out=Zn[:K, :V], in0=Zc[:K, :V], scalar=Ep[:K, wc:wc + 1], in1=kvd[:K, :V], op0=ALU.mult, op1=ALU.add),
                        reads=[Zc, Ep, kvd], writes=[Zn])
            for U in units:
                V = U["V"]
                o, ps_o = U["o"], U["ps_o"]
                S.op("scalar", lambda e, V=V, o=o, ps_o=ps_o, j=j: e.activation(out=o[:V, j:j + 128], in_=ps_o[:V, 0:128], func=AF.Copy),
                     reads=[ps_o], writes=[o])
        for U in units:
            V = U["V"]
            S.store("sync", U["out"], U["out"][:V, t0:t0 + n], U["o"], U["o"][:V, :n])


def scan_consts(S, cst_d):
    c = {}
    c["ident"] = load_const(S, "ident", (128, 128), cst_d["ident"][:])
    c["cmask"] = load_const(S, "cmask", (128, 512), cst_d["cmask"][:])
    c["mask_ui"] = load_const(S, "mask_ui", (128, 128), cst_d["mask_ui"][:], U8)
    c["m96"] = load_const(S, "m96", (128, 1), cst_d["m96"][:])
    if "mask_su" in cst_d:
        c["mask_su"] = load_const(S, "mask_su", (128, 128), cst_d["mask_su"][:], U8)
        c["mask_sl"] = load_const(S, "mask_sl", (128, 128), cst_d["mask_sl"][:], U8)
    z = S.sbuf("zero1", (128, 1))
    S.op("gpsimd", lambda e: e.memset(z[:], 0.0), writes=[z])
    c["zero1"] = z
    return c


def host_scan_consts(rwkv=False):
    ident = np.eye(128, dtype=np.float32)
    cmask = np.ones((128, 512), np.float32)
    cmask[:, ::32] = 0.0
    s = np.arange(128)[:, None]
    t = np.arange(128)[None, :]
    mask_ui = ((s // 32 == t // 32) & (t >= s)).astype(np.uint8)
    m96 = (np.arange(128) >= 96).astype(np.float32).reshape(128, 1)
    mask_su = ((s // 32 == t // 32) & (t > s)).astype(np.uint8)
    mask_sl = ((s // 32 == t // 32) & (t < s)).astype(np.uint8)
    d = {"ident": ident, "cmask": cmask, "mask_ui": mask_ui, "m96": m96}
    if rwkv:
        d.update({"mask_su": mask_su, "mask_sl": mask_sl})
    return d


def declare_scan_consts(S, rwkv=False):
    d = {"ident": S.dram("ident", (128, 128), F32, "ExternalInput"),
         "cmask": S.dram("cmask", (128, 512), F32, "ExternalInput"),
         "mask_ui": S.dram("mask_ui", (128, 128), U8, "ExternalInput"),
         "m96": S.dram("m96", (128, 1), F32, "ExternalInput")}
    if rwkv:
        d["mask_su"] = S.dram("mask_su", (128, 128), U8, "ExternalInput")
        d["mask_sl"] = S.dram("mask_sl", (128, 128), U8, "ExternalInput")
    return d


def rev_ap(ap):
    return ap[:, ::-1]


NG0 = 10


def build_mix0(cfg):
    nc, stack, S = new_prog()
    KC, T, L, SS = cfg.KC, cfg.T, cfg.L, cfg.S
    hT = S.dram("hT", (128, KC, T), BF16, "ExternalInput")
    w_d = S.dram("w", (128, KC, NG0 * 128), F32, "ExternalInput")
    cosT = S.dram("cosT", (128, SS), F32, "ExternalInput")
    sinT = S.dram("sinT", (128, SS), F32, "ExternalInput")
    lbl_d = S.dram("lbl", (128, 2), F32, "ExternalInput")
    hn_d = S.dram("hnorm", (128, 1), F32, "ExternalInput")
    sink_d = S.dram("sink", (128, 1), F32, "ExternalInput")
    mlo_d = S.dram("mask_lo", (128, 128), BF16, "ExternalInput")
    mup_d = S.dram("mask_up", (128, 128), BF16, "ExternalInput")
    cst_d = declare_scan_consts(S)
    out = S.dram("oT", (2, 128, T), BF16, "ExternalOutput")
    qr = S.dram("qr", (128, T), BF16)
    kr = S.dram("kr", (128, T), BF16)
    vt = S.dram("vt", (T, 128), BF16)
    names = ["q_f", "v_f", "k_f", "lw_f", "q_b", "v_b", "k_b", "lw_b", "gate", "o_f", "o_b"]
    scr = {nm: S.dram("scr_" + nm, (128, T), F32, "ExternalOutput" if DEBUG else "Internal") for nm in names}

    for _ph in (S.mark(),):
        W = load_w_bf16(S, "W", w_d, KC, NG0 * 128)
        lbl = load_const(S, "lbl", (128, 2), lbl_d[:])
        lb = S.sbuf("lb", (128, 1))
        oml = S.sbuf("oml", (128, 1))
        S.op("vector", lambda e: e.tensor_tensor(out=lb[:], in0=lbl[:, 0:1], in1=lbl[:, 1:2], op=ALU.subtract), reads=[lbl], writes=[lb])
        S.op("scalar", lambda e: e.activation(out=lb[:], in_=lb[:], func=AF.Sigmoid), reads=[lb], writes=[lb])
        S.op("vector", lambda e: e.tensor_scalar(out=oml[:], in0=lb[:], scalar1=-1.0, scalar2=1.0, op0=ALU.mult, op1=ALU.add),
             reads=[lb], writes=[oml])
        hbs = rot_sbuf(S, "hb", (128, KC, 512), BF16)
        pss = Rot([S.psum(f"pp{i}") for i in range(6)])
        st = {nm: rot_sbuf(S, "st_" + nm, (128, 512)) for nm in ["q", "v", "kf", "lwf", "kb", "lwb", "g", "qr_", "vr_", "kbr", "lwbr", "t1", "t2", "f"]}
        stb = {nm: rot_sbuf(S, "stb_" + nm, (128, 512), BF16) for nm in ["aq", "ak"]}
        stv = rot_sbuf(S, "stv", (128, 128), BF16, n=4)
        cosb = rot_sbuf(S, "cosb", (128, 512))
        sinb = rot_sbuf(S, "sinb", (128, 512))
        for (t0, n, seg0, seglen) in seq_blocks(cfg):
            rp = rev_pos(t0, n, seg0, seglen)
            lat = seg0 == L
            hb = hbs.get()
            S.load("sync", hb, hb[:, :, :n], hT[:, :, t0:t0 + n])
            if lat:
                cb, sb = cosb.get(), sinb.get()
                S.load("scalar", cb, cb[:, :n], cosT[:, t0 - L:t0 - L + n])
                S.load("scalar", sb, sb[:, :n], sinT[:, t0 - L:t0 - L + n])
            for (g, dst, key) in ((0, qr, "aq"), (2, kr, "ak")):
                p1 = pss.get()
                fm_group(S, p1, W, hb, g, n, KC)
                o = stb[key].get()
                if lat:
                    p2 = pss.get()
                    fm_group(S, p2, W, hb, g + 1, n, KC)
                    t1, t2 = st["t1"].get(), st["t2"].get()
                    S.op("vector", lambda e, t1=t1, p1=p1, cb=cb: e.tensor_tensor(out=t1[:, :n], in0=p1[:, :n], in1=cb[:, :n], op=ALU.mult),
                         reads=[p1, cb], writes=[t1])
                    S.op("vector", lambda e, t2=t2, p2=p2, sb=sb: e.tensor_tensor(out=t2[:, :n], in0=p2[:, :n], in1=sb[:, :n], op=ALU.mult),
                         reads=[p2, sb], writes=[t2])
                    S.op("gpsimd", lambda e, o=o, t1=t1, t2=t2: e.tensor_tensor(out=o[:, :n], in0=t1[:, :n], in1=t2[:, :n], op=ALU.add),
                         reads=[t1, t2], writes=[o])
                else:
                    S.op("scalar", lambda e, o=o, p1=p1: e.activation(out=o[:, :n], in_=p1[:, :n], func=AF.Copy), reads=[p1], writes=[o])
                S.store("sync", dst, dst[:, t0:t0 + n], o, o[:, :n])
            for sb_ in range(0, n, 128):
                p = pss.get()
                for kc in range(KC):
                    S.op("tensor", lambda e, p=p, kc=kc, sb_=sb_, hb=hb: e.matmul(p[:, 0:128], hb[:, kc, sb_:sb_ + 128], W[:, kc, 4 * 128:5 * 128],
                                                                           start=(kc == 0), stop=(kc == KC - 1)), reads=[hb, W], writes=[p])
                o = stv.get()
                S.op("vector", lambda e, o=o, p=p: e.tensor_copy(out=o[:], in_=p[:, 0:128]), reads=[p], writes=[o])
                S.store("sync", vt, vt[t0 + sb_:t0 + sb_ + 128, :], o, o[:])

            def put(nm_f, nm_b, tile, rkey):
                if nm_f is not None:
                    S.store("sync", scr[nm_f], scr[nm_f][:, t0:t0 + n], tile, tile[:, :n])
                if nm_b is not None:
                    r = st[rkey].get()
                    S.op("vector", lambda e, r=r, tile=tile: e.tensor_copy(out=r[:, :n], in_=rev_ap(tile[:, :n])), reads=[tile], writes=[r])
                    S.store("sync", scr[nm_b], scr[nm_b][:, rp:rp + n], r, r[:, :n])

            p = pss.get()
            fm_group(S, p, W, hb, 5, n, KC)
            o = st["q"].get()
            S.op("scalar", lambda e, o=o, p=p: e.activation(out=o[:, :n], in_=p[:, :n], func=AF.Silu), reads=[p], writes=[o])
            put("q_f", "q_b", o, "qr_")
            p = pss.get()
            fm_group(S, p, W, hb, 6, n, KC)
            o = st["v"].get()
            S.op("vector", lambda e, o=o, p=p: e.tensor_copy(out=o[:, :n], in_=p[:, :n]), reads=[p], writes=[o])
            put("v_f", "v_b", o, "vr_")
            for (g, kkey, lkey, fwd) in ((7, "kf", "lwf", True), (8, "kb", "lwb", False)):
                p = pss.get()
                fm_group(S, p, W, hb, g, n, KC)
                f = st["f"].get()
                S.op("scalar", lambda e, f=f, p=p: e.activation(out=f[:, :n], in_=p[:, :n], func=AF.Sigmoid), reads=[p], writes=[f])
                S.op("vector", lambda e, f=f: e.tensor_scalar(out=f[:, :n], in0=f[:, :n], scalar1=oml[:, 0:1], scalar2=lb[:, 0:1],
                                                             op0=ALU.mult, op1=ALU.add), reads=[f, oml, lb], writes=[f])
                lw = st[lkey].get()
                kk = st[kkey].get()
                S.op("scalar", lambda e, lw=lw, f=f: e.activation(out=lw[:, :n], in_=f[:, :n], func=AF.Ln), reads=[f], writes=[lw])
                S.op("vector", lambda e, kk=kk, f=f: e.tensor_scalar(out=kk[:, :n], in0=f[:, :n], scalar1=-1.0, scalar2=1.0,
                                                               op0=ALU.mult, op1=ALU.add), reads=[f], writes=[kk])
                if fwd:
                    put("k_f", None, kk, None)
                    put("lw_f", None, lw, None)
                else:
                    put(None, "k_b", kk, "kbr")
                    put(None, "lw_b", lw, "lwbr")
            p = pss.get()
            fm_group(S, p, W, hb, 9, n, KC)
            o = st["g"].get()
            S.op("scalar", lambda e, o=o, p=p: e.activation(out=o[:, :n], in_=p[:, :n], func=AF.Silu), reads=[p], writes=[o])
            put("gate", None, o, None)
        barrier(S)
        S.reset(_ph)
    for _ph in (S.mark(),):
        consts = scan_consts(S, cst_d)
        units = [dict(K=128, V=128, q=scr["q_f"], k=scr["k_f"], lw=scr["lw_f"], v=scr["v_f"], out=scr["o_f"]),
                 dict(K=128, V=128, q=scr["q_b"], k=scr["k_b"], lw=scr["lw_b"], v=scr["v_b"], out=scr["o_b"])]
        chunk_scan(S, cfg, units, consts)
        barrier(S)
        S.reset(_ph)
    for _ph in (S.mark(),):
        ones = make_ones(S)
        hn = load_const(S, "hn", (128, 1), hn_d[:])
        S.op("vector", lambda e: e.tensor_scalar(out=hn[:], in0=hn[:], scalar1=float(128) ** 0.5, scalar2=None, op0=ALU.mult), reads=[hn], writes=[hn])
        ofb, obb, gb = rot_sbuf(S, "ofb", (128, 512)), rot_sbuf(S, "obb", (128, 512)), rot_sbuf(S, "gb", (128, 512))
        sq = S.sbuf("hsq", (128, 1, 512))
        rstd = S.sbuf("hrstd", (128, 512))
        ps = S.psum("hps")
        res = rot_sbuf(S, "hres", (128, 512), BF16)
        for (t0, n, seg0, seglen) in seq_blocks(cfg):
            rp = rev_pos(t0, n, seg0, seglen)
            of, ob, g = ofb.get(), obb.get(), gb.get()
            S.load("sync", of, of[:, :n], scr["o_f"][:, t0:t0 + n], src=scr["o_f"])
            S.load("scalar", ob, ob[:, :n], scr["o_b"][:, rp:rp + n], src=scr["o_b"])
            S.load("sync", g, g[:, :n], scr["gate"][:, t0:t0 + n], src=scr["gate"])
            S.op("vector", lambda e, of=of, ob=ob: e.tensor_tensor(out=of[:, :n], in0=of[:, :n], in1=rev_ap(ob[:, :n]), op=ALU.add),
                 reads=[of, ob], writes=[of])
            S.op("scalar", lambda e, of=of: e.activation(out=sq[:, 0, :n], in_=of[:, :n], func=AF.Square), reads=[of], writes=[sq])
            S.op("tensor", lambda e: e.matmul(ps[:, :n], ones[:], sq[:, 0, :n], start=True, stop=True), reads=[ones, sq], writes=[ps])
            S.op("vector", lambda e: e.tensor_scalar(out=rstd[:, :n], in0=ps[:, :n], scalar1=NORM_EPS * 128, scalar2=None, op0=ALU.add),
                 reads=[ps], writes=[rstd])
            S.op("scalar", lambda e: e.activation(out=rstd[:, :n], in_=rstd[:, :n], func=AF.Sqrt), reads=[rstd], writes=[rstd])
            S.op("vector", lambda e: e.reciprocal(out=rstd[:, :n], in_=rstd[:, :n]), reads=[rstd], writes=[rstd])
            S.op("vector", lambda e, of=of: e.scalar_tensor_tensor(out=of[:, :n], in0=of[:, :n], scalar=hn[:, 0:1], in1=rstd[:, :n],
                                                                 op0=ALU.mult, op1=ALU.mult), reads=[of, hn, rstd], writes=[of])
            r = res.get()
            S.op("gpsimd", lambda e, r=r, of=of, g=g: e.tensor_tensor(out=r[:, :n], in0=of[:, :n], in1=g[:, :n], op=ALU.mult),
                 reads=[of, g], writes=[r])
            S.store("sync", out, out[1, :, t0:t0 + n], r, r[:, :n])
        attention(S, cfg, qr, kr, vt, sink_d, mlo_d, mup_d, out)
    return nc, stack, S


def attention(S, cfg, qr, kr, vt, sink_d, mlo_d, mup_d, out):
    L, SS, T = cfg.L, cfg.S, cfg.T
    NCB = L // 128
    scale = 128.0 ** -0.5
    ones_b = S.sbuf("ones_b", (128, 128), BF16)
    S.op("gpsimd", lambda e: e.memset(ones_b[:], 1.0), writes=[ones_b])
    mlo = load_const(S, "mlo", (128, 128), mlo_d[:], BF16)
    mup = load_const(S, "mup", (128, 128), mup_d[:], BF16)
    es = load_const(S, "es", (128, 1), sink_d[:])
    S.op("scalar", lambda e: e.activation(out=es[:], in_=es[:], func=AF.Exp), reads=[es], writes=[es])
    kc_sb = S.sbuf("kc_sb", (128, L), BF16)
    S.load("sync", kc_sb, kc_sb[:], kr[:, 0:L], src=kr)
    vc_sb = S.sbuf("vc_sb", (128, NCB, 128), BF16)
    for j in range(NCB):
        S.load("sync", vc_sb, vc_sb[:, j, :], vt[j * 128:(j + 1) * 128, :], src=vt)
    qb = rot_sbuf(S, "aqb", (128, 128), BF16, n=3)
    kb = rot_sbuf(S, "akb", (128, 384), BF16, n=3)
    vb = rot_sbuf(S, "avb", (128, 3, 128), BF16, n=3)
    pT = rot_sbuf(S, "apT", (128, 5, 128), BF16, n=2)
    den = rot_sbuf(S, "aden", (128, 128), F32, n=2)
    ob = rot_sbuf(S, "aob", (128, 128), BF16, n=3)
    ps_s = Rot([S.psum(f"aps_s{i}", (128, 512)) for i in range(2)])
    ps_s2 = Rot([S.psum(f"aps_t{i}", (128, 512)) for i in range(2)])
    ps_o = Rot([S.psum(f"aps_o{i}", (128, 512)) for i in range(2)])
    nqb = SS // 128

    def one_block(q0, ktiles):
        q = qb.get()
        S.load("sync", q, q[:], qr[:, q0:q0 + 128], src=qr)
        p1, p2, po = ps_s.get(), ps_s2.get(), ps_o.get()
        P = pT.get()
        nt = len(ktiles)
        for i, (kbuf, kap, vbuf, vap, mask) in enumerate(ktiles):
            pp = p1 if i < 4 else p2
            col = (i % 4) * 128
            S.op("tensor", lambda e, pp=pp, col=col, kap=kap, q=q: e.matmul(pp[:, col:col + 128], kap, q[:], start=True, stop=True),
                 reads=[kbuf, q], writes=[pp])
        n1 = min(nt, 4)
        S.op("scalar", lambda e, P=P, p1=p1, n1=n1: e.activation(out=P[:, 0:n1, :], in_=p1[:, 0:n1 * 128].rearrange("p (a b) -> p a b", b=128),
                                                              func=AF.Exp, scale=scale),
             reads=[p1], writes=[P])
        if nt > 4:
            S.op("scalar", lambda e, P=P, p2=p2: e.activation(out=P[:, 4, :], in_=p2[:, 0:128], func=AF.Exp, scale=scale),
                 reads=[p2], writes=[P])
        for i, (kbuf, kap, vbuf, vap, mask) in enumerate(ktiles):
            if mask is not None:
                S.op("gpsimd", lambda e, P=P, i=i, mask=mask: e.tensor_tensor(out=P[:, i, :], in0=P[:, i, :], in1=mask[:], op=ALU.mult),
                     reads=[P, mask], writes=[P])
        for i, (kbuf, kap, vbuf, vap, mask) in enumerate(ktiles):
            S.op("tensor", lambda e, po=po, vap=vap, P=P, i=i: e.matmul(po[:, 0:128], vap, P[:, i, :], start=(i == 0), stop=(i == nt - 1)),
                 reads=[vbuf, P], writes=[po])
        for i in range(nt):
            S.op("tensor", lambda e, po=po, P=P, i=i: e.matmul(po[:, 128:256], ones_b[:], P[:, i, :], start=(i == 0), stop=(i == nt - 1),
                                                              skip_group_check=True),
                 reads=[ones_b, P], writes=[po])
        d = den.get()
        S.op("vector", lambda e, d=d, po=po: e.tensor_scalar(out=d[:], in0=po[:, 128:256], scalar1=es[:, 0:1], scalar2=None, op0=ALU.add),
             reads=[po, es], writes=[d])
        S.op("vector", lambda e, d=d: e.reciprocal(out=d[:], in_=d[:]), reads=[d], writes=[d])
        o = ob.get()
        S.op("vector", lambda e, o=o, d=d, po=po: e.tensor_tensor(out=o[:], in0=d[:], in1=po[:, 0:128], op=ALU.mult),
             reads=[d, po], writes=[o])
        S.store("sync", out, out[0, :, q0:q0 + 128], o, o[:])

    ctx_tiles = [(kc_sb, kc_sb[:, j * 128:(j + 1) * 128], vc_sb, vc_sb[:, j, :], None) for j in range(NCB)]
    for b in range(NCB):
        one_block(b * 128, ctx_tiles)
    for b in range(nqb):
        lo = max(b - 1, 0)
        hi = min(b + 1, nqb - 1)
        nk = hi - lo + 1
        k = kb.get()
        v = vb.get()
        S.load("scalar", k, k[:, :nk * 128], kr[:, L + lo * 128:L + (hi + 1) * 128], src=kr)
        for i in range(nk):
            S.load("scalar", v, v[:, i, :], vt[L + (lo + i) * 128:L + (lo + i + 1) * 128, :], src=vt)
        tiles = []
        for i in range(nk):
            kbk = lo + i
            mask = mlo if kbk == b - 1 else (mup if kbk == b + 1 else None)
            tiles.append((k, k[:, i * 128:(i + 1) * 128], v, v[:, i, :], mask))
        one_block(L + b * 128, tiles + ctx_tiles)


def rope_tables(S_len):
    n_freq = 32
    t = np.arange(S_len)
    row = (t // 64).astype(np.float32)
    col = (t % 64).astype(np.float32)
    inv = (np.float32(10000.0) ** (-np.arange(n_freq, dtype=np.float32) / np.float32(n_freq))).astype(np.float32)
    d = np.arange(128)
    axis, half, f = d // 64, (d % 64) // 32, d % 32
    pos = np.where(axis[:, None] == 0, row[None, :], col[None, :]).astype(np.float32)
    ang = pos * inv[f][:, None]
    cosT = np.cos(ang).astype(np.float32)
    sinT = (np.sin(ang) * np.where(half[:, None] == 0, -1.0, 1.0)).astype(np.float32)
    partner = np.where(half == 0, d + 32, d - 32)
    return cosT, sinT, partner


def wfm(w):
    D, n = w.shape
    return np.ascontiguousarray(w.reshape(D // 128, 128, n).transpose(1, 0, 2))


def run_mix0(cfg, hT_all, inp):
    nc, stack, S = build_mix0(cfg)
    cosT, sinT, partner = rope_tables(cfg.S)
    w_in = inp["l0_w_in"]
    hfm = fm(hT_all)
    jj = np.arange(128)[:, None]
    ii = np.arange(128)[None, :]
    mlo = (ii <= jj).astype(ml_dtypes.bfloat16)
    mup = (jj <= ii).astype(ml_dtypes.bfloat16)
    sc = host_scan_consts()
    maps = []
    for c in range(NCORES):
        g = c // 4
        q = w_in[:, c * 128:(c + 1) * 128]
        k = w_in[:, 1024 + g * 128:1024 + (g + 1) * 128]
        v = w_in[:, 1280 + g * 128:1280 + (g + 1) * 128]
        cols = [q, q[:, partner], k, k[:, partner], v]
        for base in (1536, 2560, 3584, 4608, 5632):
            cols.append(w_in[:, base + c * 128:base + (c + 1) * 128])
        m = {"hT": hfm, "w": wfm(np.concatenate(cols, axis=1)), "cosT": cosT, "sinT": sinT,
             "lbl": np.ascontiguousarray(inp["hgrn_lb_logits"][:, c * 128:(c + 1) * 128].T),
             "hnorm": np.ascontiguousarray(inp["l0_hgrn_norm"].reshape(128, 1)),
             "sink": np.full((128, 1), inp["l0_attn_sink"][c], np.float32),
             "mask_lo": mlo, "mask_up": mup}
        m.update(sc)
        maps.append(m)
    res = run_prog(nc, stack, S, maps)
    if DEBUG:
        global DBG
        DBG = res
    att = np.concatenate([res[c]["oT"][0] for c in range(NCORES)], axis=0)
    hg = np.concatenate([res[c]["oT"][1] for c in range(NCORES)], axis=0)
    return np.concatenate([att, hg], axis=0)


CAST_ENGS = ("scalar", "gpsimd", "vector")


def convert_w(S, src, dst, NT, F, stage_f, stage_b, ctr):
    step = 2048
    for t in range(NT):
        for f0 in range(0, F, step):
            fn = min(step, F - f0)
            a, b = stage_f.get(), stage_b.get()
            S.load("sync" if ctr[0] % 2 == 0 else "scalar", a, a[:, :fn], src[t, :, f0:f0 + fn])
            eng = CAST_ENGS[ctr[0] % 3]
            if eng == "scalar":
                S.op("scalar", lambda e: e.activation(out=b[:, :fn], in_=a[:, :fn], func=AF.Copy), reads=[a], writes=[b])
            else:
                S.op(eng, lambda e: e.tensor_copy(out=b[:, :fn], in_=a[:, :fn]), reads=[a], writes=[b])
            S.store("sync" if ctr[0] % 2 == 1 else "scalar", dst, dst[t, :, f0:f0 + fn], b, b[:, :fn])
            ctr[0] += 1


def ffn_block(S, cfg, hb, n, NJ, wg_b, wu_b, wd_b, bufs, evac):
    KC = cfg.KC
    hid = bufs["hid"]
    for j in range(NJ):
        wg, wu = bufs["wg"].get(), bufs["wu"].get()
        S.load("sync", wg, wg[:], wg_b[j], src=wg_b)
        S.load("sync", wu, wu[:], wu_b[j], src=wu_b)
        pg, pu = bufs["pg"].get(), bufs["pu"].get()
        for kc in range(KC):
            S.op("tensor", lambda e: e.matmul(pg[:, :n], wg[:, kc * 128:(kc + 1) * 128], hb[:, kc, :n], start=(kc == 0), stop=(kc == KC - 1)),
                 reads=[wg, hb], writes=[pg])
        for kc in range(KC):
            S.op("tensor", lambda e: e.matmul(pu[:, :n], wu[:, kc * 128:(kc + 1) * 128], hb[:, kc, :n], start=(kc == 0), stop=(kc == KC - 1)),
                 reads=[wu, hb], writes=[pu])
        sg = bufs["sg"].get()
        S.op("scalar", lambda e: e.activation(out=sg[:, :n], in_=pg[:, :n], func=AF.Silu), reads=[pg], writes=[sg])
        S.op("vector", lambda e: e.tensor_tensor(out=hid[:, j, :n], in0=sg[:, :n], in1=pu[:, :n], op=ALU.mult), reads=[sg, pu], writes=[hid])
    for dc in range(KC):
        wd = bufs["wd"].get()
        S.load("sync", wd, wd[:, :NJ * 128], wd_b[dc], src=wd_b)
        po = bufs["po"].get()
        for j in range(NJ):
            S.op("tensor", lambda e: e.matmul(po[:, :n], wd[:, j * 128:(j + 1) * 128], hid[:, j, :n], start=(j == 0), stop=(j == NJ - 1)),
                 reads=[wd, hid], writes=[po])
        evac(dc, po)


def ffn_bufs(S, cfg, NJ, nmax):
    KC = cfg.KC
    return {"hid": S.sbuf("hid", (128, NJ, nmax), BF16),
            "wg": rot_sbuf(S, "wg", (128, KC * 128), BF16, n=3), "wu": rot_sbuf(S, "wu", (128, KC * 128), BF16, n=3),
            "wd": rot_sbuf(S, "wd", (128, NJ * 128), BF16, n=2),
            "sg": rot_sbuf(S, "sg", (128, nmax), F32, n=2),
            "pg": Rot([S.psum("pg0"), S.psum("pg1")]), "pu": Rot([S.psum("pu0"), S.psum("pu1")]),
            "po": Rot([S.psum("po0"), S.psum("po1")])}


def host_ffn_w(wg, wu, wd):
    D, FF = wg.shape
    KC, NJ = D // 128, FF // 128

    def gu(w):
        return np.ascontiguousarray(w.reshape(KC, 128, NJ, 128).transpose(2, 1, 0, 3).reshape(NJ, 128, KC * 128))
    wdl = np.ascontiguousarray(wd.reshape(NJ, 128, KC, 128).transpose(2, 1, 0, 3).reshape(KC, 128, NJ * 128))
    return gu(wg), gu(wu), wdl


def build_l3(cfg):
    nc, stack, S = new_prog()
    KC, NB = cfg.KC, 256
    NJ = cfg.D_FF // 128
    xT = S.dram("xT", (128, KC, cfg.ntok), F32, "ExternalInput")
    oT = S.dram("oT", (128, KC, cfg.ntok), BF16, "ExternalInput")
    mod_d = [S.dram(f"mod{l}", (128, 6 * KC, 2), F32, "ExternalInput") for l in range(2)]
    gains_d = S.dram("gains", (128, 4, KC), F32, "ExternalInput")
    wo_d = S.dram("wo", (KC, 128, KC * 128), F32, "ExternalInput")
    wg_d = S.dram("wg", (NJ, 128, KC * 128), F32, "ExternalInput")
    wu_d = S.dram("wu", (NJ, 128, KC * 128), F32, "ExternalInput")
    wd_d = S.dram("wd", (KC, 128, NJ * 128), F32, "ExternalInput")
    x2T = S.dram("x2T", (128, KC, cfg.ntok), F32, "ExternalOutput")
    hT1 = S.dram("hT1", (128, KC, cfg.ntok), BF16, "ExternalOutput")
    wo_b = S.dram("wo_b", (KC, 128, KC * 128), BF16)
    wg_b = S.dram("wg_b", (NJ, 128, KC * 128), BF16)
    wu_b = S.dram("wu_b", (NJ, 128, KC * 128), BF16)
    wd_b = S.dram("wd_b", (KC, 128, NJ * 128), BF16)
    for _ph in (S.mark(),):
        sf, sb = rot_sbuf(S, "cv_f", (128, 2048), F32, n=3), rot_sbuf(S, "cv_b", (128, 2048), BF16, n=3)
        ctr = [0]
        convert_w(S, wo_d, wo_b, KC, KC * 128, sf, sb, ctr)
        convert_w(S, wg_d, wg_b, NJ, KC * 128, sf, sb, ctr)
        convert_w(S, wu_d, wu_b, NJ, KC * 128, sf, sb, ctr)
        convert_w(S, wd_d, wd_b, KC, NJ * 128, sf, sb, ctr)
        barrier(S)
        S.reset(_ph)
    mod = [load_const(S, f"mod_sb{l}", (128, 6 * KC, 2), mod_d[l][:]) for l in range(2)]
    gains = load_const(S, "gains_sb", (128, 4, KC), gains_d[:])
    gn = [Buf(f"gain{i}", gains[:, i, :]) for i in range(4)]
    for g in gn:
        g.writers = gains.writers
    ones = make_ones(S)
    G1 = gate_scalars(S, cfg, mod[0], gn[0], 2, "g1")
    A2, B2 = mod_scalars(S, cfg, mod[0], gn[1], 4, 3, "m2")
    G2 = gate_scalars(S, cfg, mod[0], gn[2], 5, "g2")
    A3, B3 = mod_scalars(S, cfg, mod[1], gn[3], 1, 0, "m3")
    xb = S.sbuf("xb", (128, KC, NB))
    ob = S.sbuf("ob", (128, KC, NB), BF16)
    yb = S.sbuf("yb", (128, KC, NB))
    hb = S.sbuf("hb", (128, KC, NB), BF16)
    sq = rot_sbuf(S, "sq", (128, NB))
    tmp = rot_sbuf(S, "tmp", (128, NB))
    rstd = S.sbuf("rstd", (128, NB))
    wo = rot_sbuf(S, "wo", (128, KC * 128), BF16, n=3)
    ps_s = S.psum("ps_stat")
    ps_y = Rot([S.psum("ps_y0")])
    fb = ffn_bufs(S, cfg, NJ, NB)
    ps_y = fb["po"]
    for (s0, n, kind) in token_blocks(cfg, NB):
        S.load("sync", xb, xb[:, :, :n], xT[:, :, s0:s0 + n])
        S.load("scalar", ob, ob[:, :, :n], oT[:, :, s0:s0 + n])
        for dc in range(KC):
            w = wo.get()
            S.load("sync", w, w[:], wo_b[dc], src=wo_b)
            p = ps_y.get()
            for kc in range(KC):
                S.op("tensor", lambda e: e.matmul(p[:, :n], w[:, kc * 128:(kc + 1) * 128], ob[:, kc, :n], start=(kc == 0), stop=(kc == KC - 1)),
                     reads=[w, ob], writes=[p])
            S.op("scalar", lambda e: e.activation(out=yb[:, dc, :n], in_=p[:, :n], func=AF.Copy), reads=[p], writes=[yb])
        rms_stats(S, cfg, yb, n, sq, ones, ps_s, rstd)
        resid_norm_add(S, cfg, xb, yb, n, rstd, G1[kind], tmp)
        rms_stats(S, cfg, xb, n, sq, ones, ps_s, rstd)
        norm_mod_apply(S, cfg, xb, n, rstd, A2[kind], B2[kind], hb, tmp)

        def evac(dc, po):
            S.op("scalar", lambda e: e.activation(out=yb[:, dc, :n], in_=po[:, :n], func=AF.Copy), reads=[po], writes=[yb])
        ffn_block(S, cfg, hb, n, NJ, wg_b, wu_b, wd_b, fb, evac)
        rms_stats(S, cfg, yb, n, sq, ones, ps_s, rstd)
        resid_norm_add(S, cfg, xb, yb, n, rstd, G2[kind], tmp)
        S.store("gpsimd", x2T, x2T[:, :, s0:s0 + n], xb, xb[:, :, :n])
        rms_stats(S, cfg, xb, n, sq, ones, ps_s, rstd)
        norm_mod_apply(S, cfg, xb, n, rstd, A3[kind], B3[kind], hb, tmp)
        S.store("gpsimd", hT1, hT1[:, :, s0:s0 + n], hb, hb[:, :, :n])
    return nc, stack, S


def run_l3(cfg, xT_lat, xT_ctx, oT_all, mods, inp):
    nc, stack, S = build_l3(cfg)
    L = cfg.L
    wo = inp["l0_w_out"]
    KC = cfg.KC
    wo_l = np.ascontiguousarray(wo.reshape(KC, 128, KC, 128).transpose(2, 1, 0, 3).reshape(KC, 128, KC * 128))
    wg, wu, wd = host_ffn_w(inp["l0_ffn_w_gate"], inp["l0_ffn_w_up"], inp["l0_ffn_w_down"])
    gains = np.ascontiguousarray(np.stack([vec_fm(inp[k]) for k in ("l0_norm_mix_post", "l0_norm_ffn_pre", "l0_norm_ffn_post", "l1_norm_mix_pre")], axis=1))
    maps = []
    for i in range(NCORES):
        maps.append({"xT": fm(own_tokens_T(cfg, xT_lat, xT_ctx, i)), "oT": fm(own_tokens_T(cfg, oT_all[:, L:], oT_all[:, :L], i)),
                     "mod0": mods[0], "mod1": mods[1], "gains": gains, "wo": wo_l, "wg": wg, "wu": wu, "wd": wd})
    res = run_prog(nc, stack, S, maps)
    x2c, x2l = gather_tokens_T(cfg, [unfm(res[i]["x2T"]) for i in range(NCORES)])
    hc, hl = gather_tokens_T(cfg, [unfm(res[i]["hT1"]) for i in range(NCORES)])
    return x2c, x2l, np.concatenate([hc, hl], axis=1)


def rwkv_scan(S, cfg, units, consts, SB=256):
    T = cfg.T
    ident, zero1, cmask, m96 = consts["ident"], consts["zero1"], consts["cmask"], consts["m96"]
    m_ui, m_su, m_sl = consts["mask_ui"], consts["mask_su"], consts["mask_sl"]
    K = V = 64
    names = ("q", "k", "v", "lw", "a", "b")
    for u, U in enumerate(units):
        U["in"] = {nm: rot_sbuf(S, f"r{u}_{nm}", (128, SB)) for nm in names}
        for nm in ("L", "Ep", "En", "Eex", "qh", "kh", "ah", "bh"):
            U[nm] = S.sbuf(f"r{u}_{nm}", (128, SB))
        for nm in ("NT", "Nn", "MrbT", "MakT", "MrkT", "Xa", "Xb", "PTa", "Pa", "PTb", "Pb"):
            U[nm] = S.sbuf(f"r{u}_{nm}", (128, 128))
        for nm in ("btok", "ktok", "vtok", "Apz", "bz", "kz", "Rp"):
            U[nm] = S.sbuf(f"r{u}_{nm}", (128, 128 if nm == "Rp" else 64))
        for nm in ("PTc", "Qd"):
            U[nm] = S.sbuf(f"r{u}_{nm}", (128, 64))
        U["Z"] = [S.sbuf(f"r{u}_Z{i}", (128, 64)) for i in range(2)]
        U["zi"] = 0
        U["osb"] = rot_sbuf(S, f"r{u}_osb", (128, SB))
        bA, bB = S.psum(f"r{u}_psA"), S.psum(f"r{u}_psB")
        U["ps"] = [bA, bB, bA, bB]
        for nm in ("NT", "Nn", "MrbT", "MakT", "MrkT", "Apz", "bz", "kz"):
            S.op("gpsimd", lambda e: e.memset(U[nm][:], 0.0), writes=[U[nm]])
        S.op("gpsimd", lambda e: e.memset(U["Z"][0][:], 0.0), writes=[U["Z"][0]])
    for t0 in range(0, T, SB):
        n = min(SB, T - t0)
        for U in units:
            cur = {}
            for i, nm in enumerate(names):
                b = U["in"][nm].get()
                S.load("sync" if i % 2 == 0 else "scalar", b, b[:K, :n], U[nm][0:K, t0:t0 + n], src=U["src_" + nm])
                cur[nm] = b
            L, Ep, En, Eex, qh, kh, ah, bh = (U[x] for x in ("L", "Ep", "En", "Eex", "qh", "kh", "ah", "bh"))
            S.op("vector", lambda e: e.tensor_tensor_scan(out=L[:K, :n], data0=cmask[:K, :n], data1=cur["lw"][:K, :n], initial=zero1[:K, 0:1],
                                                          op0=ALU.mult, op1=ALU.add), reads=[cmask, cur["lw"], zero1], writes=[L])
            S.op("scalar", lambda e: e.activation(out=Ep[:K, :n], in_=L[:K, :n], func=AF.Exp), reads=[L], writes=[Ep])
            S.op("vector", lambda e: e.reciprocal(out=En[:K, :n], in_=Ep[:K, :n]), reads=[Ep], writes=[En])
            S.op("gpsimd", lambda e: e.tensor_tensor(out=Eex[:K, :n], in0=L[:K, :n], in1=cur["lw"][:K, :n], op=ALU.subtract),
                 reads=[L, cur["lw"]], writes=[Eex])
            S.op("scalar", lambda e: e.activation(out=Eex[:K, :n], in_=Eex[:K, :n], func=AF.Exp), reads=[Eex], writes=[Eex])
            S.op("gpsimd", lambda e: e.tensor_tensor(out=qh[:K, :n], in0=cur["q"][:K, :n], in1=Ep[:K, :n], op=ALU.mult), reads=[cur["q"], Ep], writes=[qh])
            S.op("gpsimd", lambda e: e.tensor_tensor(out=kh[:K, :n], in0=cur["k"][:K, :n], in1=En[:K, :n], op=ALU.mult), reads=[cur["k"], En], writes=[kh])
            S.op("vector", lambda e: e.tensor_tensor(out=ah[:K, :n], in0=cur["a"][:K, :n], in1=Eex[:K, :n], op=ALU.mult), reads=[cur["a"], Eex], writes=[ah])
            S.op("gpsimd", lambda e: e.tensor_tensor(out=bh[:K, :n], in0=cur["b"][:K, :n], in1=En[:K, :n], op=ALU.mult), reads=[cur["b"], En], writes=[bh])
            U["cur"] = cur
            U["o"] = U["osb"].get()
        for j in range(0, n, 128):
            js = slice(j, j + 128)
            for U in units:
                cur = U["cur"]
                qh, kh, ah, bh = U["qh"], U["kh"], U["ah"], U["bh"]
                b0, b1, b2, b3 = U["ps"]
                NT, Nn, MrbT, MakT, MrkT = U["NT"], U["Nn"], U["MrbT"], U["MakT"], U["MrkT"]
                btok, ktok, vtok, Apz, bz, kz, Rp = U["btok"], U["ktok"], U["vtok"], U["Apz"], U["bz"], U["kz"], U["Rp"]

                def mm(out, lhsT, rhs, rd, wr, start=True, stop=True, tr=False):
                    if tr:
                        S.op("tensor", lambda e: e.matmul(out, lhsT, rhs, is_transpose=True, start=True, stop=True), reads=rd, writes=[wr])
                    else:
                        S.op("tensor", lambda e: e.matmul(out, lhsT, rhs, start=start, stop=stop), reads=rd, writes=[wr])
                mm(b0[:, 0:128], bh[:K, js], ah[:K, js], [bh, ah], b0)
                mm(b0[:, 128:256], bh[:K, js], qh[:K, js], [bh, qh], b0)
                mm(b0[:, 256:384], kh[:K, js], ah[:K, js], [kh, ah], b0)
                mm(b0[:, 384:512], kh[:K, js], qh[:K, js], [kh, qh], b0)
                mm(b1[:, 0:128], ah[:K, js], bh[:K, js], [ah, bh], b1)
                for (dst, src, msk) in ((NT, b0[:, 0:128], m_su), (MrbT, b0[:, 128:256], m_ui), (MakT, b0[:, 256:384], m_su),
                                        (MrkT, b0[:, 384:512], m_ui)):
                    S.op("vector", lambda e: e.copy_predicated(out=dst[:], mask=msk[:], data=src), reads=[b0, msk], writes=[dst])
                S.op("vector", lambda e: e.copy_predicated(out=Nn[:], mask=m_sl[:], data=b1[:, 0:128]), reads=[b1, m_sl], writes=[Nn])
                mm(b1[:, 128:192], ah[:K, js], ident[:K, :K], [ah, ident], b1, tr=True)
                mm(b1[:, 192:256], bh[:K, js], ident[:K, :K], [bh, ident], b1, tr=True)
                mm(b1[:, 256:320], kh[:K, js], ident[:K, :K], [kh, ident], b1, tr=True)
                mm(b1[:, 320:384], cur["v"][:V, js], ident[:V, :V], [cur["v"], ident], b1, tr=True)
                X = U["Xa"]
                S.op("scalar", lambda e: e.activation(out=X[:, 0:K], in_=b1[:, 128:192], func=AF.Copy), reads=[b1], writes=[X])
                S.op("vector", lambda e: e.tensor_copy(out=btok[:, :K], in_=b1[:, 192:256]), reads=[b1], writes=[btok])
                S.op("scalar", lambda e: e.activation(out=ktok[:, :K], in_=b1[:, 256:320], func=AF.Copy), reads=[b1], writes=[ktok])
                S.op("vector", lambda e: e.tensor_copy(out=vtok[:, :V], in_=b1[:, 320:384]), reads=[b1], writes=[vtok])
                S.op("vector", lambda e: e.tensor_scalar(out=bz[64:128, :K], in0=b1[64:128, 192:256], scalar1=m96[64:128, 0:1], scalar2=None,
                                                         op0=ALU.mult), reads=[b1, m96], writes=[bz])
                S.op("vector", lambda e: e.tensor_scalar(out=kz[64:128, :K], in0=b1[64:128, 256:320], scalar1=m96[64:128, 0:1], scalar2=None,
                                                         op0=ALU.mult), reads=[b1, m96], writes=[kz])
                mm(b1[:, 384:448], MakT[:], vtok[:, :V], [MakT, vtok], b1)
                S.op("scalar", lambda e: e.activation(out=X[:, K:K + V], in_=b1[:, 384:448], func=AF.Copy), reads=[b1], writes=[X])
                PT, P = NT, Nn
                spare = [(U["PTa"], U["Pa"]), (U["PTb"], U["Pb"])]
                for i in range(5):
                    Xn = U["Xb"] if X is U["Xa"] else U["Xa"]
                    mm(b2[:, 0:128], PT[:], X[:], [PT, X], b2)
                    S.op("vector", lambda e: e.tensor_tensor(out=Xn[:], in0=X[:], in1=b2[:, 0:128], op=ALU.add), reads=[X, b2], writes=[Xn])
                    if i < 4:
                        PTn, Pn = spare[i % 2]
                        mm(b2[:, 128:256], P[:], PT[:], [P, PT], b2)
                        if i < 3:
                            mm(b2[:, 256:384], PT[:], P[:], [PT, P], b2)
                        S.op("scalar", lambda e: e.activation(out=PTn[:], in_=b2[:, 128:256], func=AF.Copy), reads=[b2], writes=[PTn])
                        if i < 3:
                            S.op("scalar", lambda e: e.activation(out=Pn[:], in_=b2[:, 256:384], func=AF.Copy), reads=[b2], writes=[Pn])
                        PT, P = PTn, Pn
                    X = Xn
                U["X5"] = X
                S.op("gpsimd", lambda e: e.tensor_scalar(out=Apz[64:128, :K], in0=X[64:128, 0:K], scalar1=m96[64:128, 0:1], scalar2=None,
                                                         op0=ALU.mult), reads=[X, m96], writes=[Apz])
                mm(b3[:K, 0:128], X[:, 0:K], MrbT[:], [X, MrbT], b3)
                S.op("vector", lambda e: e.tensor_tensor(out=Rp[:K, :], in0=qh[:K, js], in1=b3[:K, 0:128], op=ALU.add), reads=[qh, b3], writes=[Rp])
                mm(b3[:V, 128:256], X[:, K:K + V], MrbT[:], [X, MrbT], b3, start=True, stop=False)
                mm(b3[:V, 128:256], vtok[:, :V], MrkT[:], [vtok, MrkT], b3, start=False, stop=False)
            for c in range(4):
                for U in units:
                    b0, b1, b2, b3 = U["ps"]
                    X, Rp, btok, ktok, vtok, Apz, bz, kz = U["X5"], U["Rp"], U["btok"], U["ktok"], U["vtok"], U["Apz"], U["bz"], U["kz"]
                    PTc, Qd, Ep = U["PTc"], U["Qd"], U["Ep"]
                    Zc, Zn = U["Z"][U["zi"]], U["Z"][1 - U["zi"]]
                    U["zi"] = 1 - U["zi"]
                    wc = j + 32 * c + 31
                    rs = slice(32 * c, 32 * c + 32)
                    hi = slice(64, 128)
                    S.op("tensor", lambda e: e.matmul(b3[:V, 128 + 32 * c:128 + 32 * c + 32], Zc[:K, :V], Rp[:K, 32 * c:32 * c + 32],
                                                      start=False, stop=(c == 3)), reads=[Zc, Rp], writes=[b3])
                    if c < 3:
                        S.op("tensor", lambda e: e.matmul(b2[:K, 256:256 + K], X[rs, 0:K], btok[rs, :K], start=True, stop=True),
                             reads=[X, btok], writes=[b2])
                    else:
                        S.op("tensor", lambda e: e.matmul(b2[:K, 256:256 + K], Apz[hi, :K], btok[hi, :K], start=True, stop=True),
                             reads=[Apz, btok], writes=[b2])
                    S.op("vector", lambda e: e.tensor_tensor(out=PTc[:K, :K], in0=b2[:K, 256:256 + K], in1=ident[:K, :K], op=ALU.add),
                         reads=[b2, ident], writes=[PTc])
                    if c < 3:
                        S.op("tensor", lambda e: e.matmul(b2[:K, 320:320 + V], btok[rs, :K], X[rs, K:K + V], start=True, stop=False),
                             reads=[btok, X], writes=[b2])
                        S.op("tensor", lambda e: e.matmul(b2[:K, 320:320 + V], ktok[rs, :K], vtok[rs, :V], start=False, stop=True),
                             reads=[ktok, vtok], writes=[b2])
                    else:
                        S.op("tensor", lambda e: e.matmul(b2[:K, 320:320 + V], bz[hi, :K], X[hi, K:K + V], start=True, stop=False),
                             reads=[bz, X], writes=[b2])
                        S.op("tensor", lambda e: e.matmul(b2[:K, 320:320 + V], kz[hi, :K], vtok[hi, :V], start=False, stop=True),
                             reads=[kz, vtok], writes=[b2])
                    S.op("vector", lambda e: e.tensor_scalar(out=Qd[:K, :V], in0=b2[:K, 320:320 + V], scalar1=Ep[:K, wc:wc + 1], scalar2=None,
                                                             op0=ALU.mult), reads=[b2, Ep], writes=[Qd])
                    S.op("tensor", lambda e: e.matmul(b2[:K, 384:384 + V], PTc[:K, :K], Zc[:K, :V], start=True, stop=True),
                         reads=[PTc, Zc], writes=[b2])
                    S.op("vector", lambda e: e.scalar_tensor_tensor(out=Zn[:K, :V], in0=b2[:K, 384:384 + V], scalar=Ep[:K, wc:wc + 1], in1=Qd[:K, :V],
                                                                    op0=ALU.mult, op1=ALU.add), reads=[b2, Ep, Qd], writes=[Zn])
            for U in units:
                o, b3 = U["o"], U["ps"][3]
                S.op("scalar", lambda e: e.activation(out=o[:V, j:j + 128], in_=b3[:V, 128:256], func=AF.Copy), reads=[b3], writes=[o])
        for U in units:
            S.store("sync", U["src_out"], U["out"][0:V, t0:t0 + n], U["o"], U["o"][:V, :n])


NG1 = 13
RW_LN_EPS = 64e-5


def build_mix1(cfg):
    nc, stack, S = new_prog()
    KC, T, L, SS = cfg.KC, cfg.T, cfg.L, cfg.S
    PB = 256
    hT = S.dram("hT", (128, KC, T), BF16, "ExternalInput")
    w_d = S.dram("w", (128, KC, NG1 * 128), F32, "ExternalInput")
    sm_d = S.dram("smalls", (128, 40), F32, "ExternalInput")
    lr_d = S.dram("lowrank", (8, 128, 128), F32, "ExternalInput")
    cst_d = declare_scan_consts(S, rwkv=True)
    out = S.dram("oT", (3, 128, SS), BF16, "ExternalOutput")
    gnames = ["q_f", "k_f", "v_f", "lw_f", "q_b", "k_b", "v_b", "lw_b", "o_f", "o_b"]
    gs = {nm: S.dram("g_" + nm, (128, T), F32, "ExternalOutput" if DEBUG else "Internal") for nm in gnames}
    rnames = ["r_f", "k_f", "v_f", "a_f", "b_f", "lw_f", "r_b", "k_b", "v_b", "a_b", "b_b", "lw_b", "o_f", "o_b", "gout", "bonus"]
    rs = {nm: S.dram("r_" + nm, (128, T), F32, "ExternalOutput" if DEBUG else "Internal") for nm in rnames}

    for _ph in (S.mark(),):
        W = load_w_bf16(S, "W", w_d, KC, NG1 * 128)
        sm = load_const(S, "sm", (128, 40), sm_d[:])
        lr = S.sbuf("lr", (128, 8, 128))
        for i in range(8):
            S.load("scalar", lr, lr[:, i, :], lr_d[i])
        UPF, UPB, W2F, W2B, A2, G20, G21, BD = range(8)
        S.op("vector", lambda e: e.tensor_scalar(out=sm[:, 24:25], in0=sm[:, 6:7], scalar1=-1.0, scalar2=1.0, op0=ALU.mult, op1=ALU.add),
             reads=[sm], writes=[sm])
        S.op("vector", lambda e: e.tensor_tensor(out=sm[:, 25:33], in0=sm[:, 8:16], in1=sm[:, 16:24], op=ALU.add), reads=[sm], writes=[sm])
        S.op("vector", lambda e: e.tensor_scalar(out=sm[:, 25:33], in0=sm[:, 25:33], scalar1=-1.0, scalar2=1.0, op0=ALU.mult, op1=ALU.add),
             reads=[sm], writes=[sm])
        hbs = rot_sbuf(S, "hb", (128, KC, PB + 2), BF16)
        pss = Rot([S.psum(f"pp{i}") for i in range(8)])
        NT_ = 40
        pool = rot_sbuf(S, "tp", (128, PB + 2), F32, n=NT_)
        revp = rot_sbuf(S, "rv", (128, PB), F32, n=8)
        gob = rot_sbuf(S, "gob", (128, PB), BF16, n=2)

        for (t0, n, seg0, seglen) in seq_blocks(cfg, PB):
            rp = rev_pos(t0, n, seg0, seglen)
            lat = seg0 == L
            lo, hi = max(t0 - 1, 0), min(t0 + n + 1, T)
            hb = hbs.get()
            S.load("sync", hb, hb[:, :, lo - (t0 - 1):hi - (t0 - 1)], hT[:, :, lo:hi])
            ne = n + 2

            def fmg(g, c0_, nn):
                p = pss.get()
                for kc in range(KC):
                    S.op("tensor", lambda e: e.matmul(p[:, :nn], W[:, kc, g * 128:(g + 1) * 128], hb[:, kc, c0_:c0_ + nn],
                                                      start=(kc == 0), stop=(kc == KC - 1)), reads=[W, hb], writes=[p])
                return p

            def put(dst_f, dst_b, tile):
                if dst_f is not None:
                    S.store("sync", dst_f, dst_f[:, t0:t0 + n], tile, tile[:, :n])
                if dst_b is not None:
                    r = revp.get()
                    S.op("vector", lambda e: e.tensor_copy(out=r[:, :n], in_=rev_ap(tile[:, :n])), reads=[tile], writes=[r])
                    S.store("scalar", dst_b, dst_b[:, rp:rp + n], r, r[:, :n])

            p = fmg(0, 1, n)
            o = pool.get()
            S.op("vector", lambda e: e.tensor_scalar(out=o[:, :n], in0=p[:, :n], scalar1=128.0 ** -0.5, scalar2=None, op0=ALU.mult), reads=[p], writes=[o])
            put(gs["q_f"], gs["q_b"], o)
            for g, nm in ((1, "k"), (2, "v")):
                p = fmg(g, 1, n)
                o = pool.get()
                S.op("scalar", lambda e: e.activation(out=o[:, :n], in_=p[:, :n], func=AF.Copy), reads=[p], writes=[o])
                put(gs[nm + "_f"], gs[nm + "_b"], o)
            p = fmg(3, 1, n)
            gd = pool.get()
            S.op("vector", lambda e: e.tensor_copy(out=gd[:, :n], in_=p[:, :n]), reads=[p], writes=[gd])
            for (ui_, bcol, fwd) in ((UPF, 0, True), (UPB, 1, False)):
                p = pss.get()
                S.op("tensor", lambda e: e.matmul(p[:, :n], lr[:, ui_, :], gd[:, :n], start=True, stop=True), reads=[lr, gd], writes=[p])
                o = pool.get()
                S.op("scalar", lambda e: e.activation(out=o[:, :n], in_=p[:, :n], func=AF.Sigmoid, bias=sm[:, bcol:bcol + 1]), reads=[p, sm], writes=[o])
                S.op("scalar", lambda e: e.activation(out=o[:, :n], in_=o[:, :n], func=AF.Ln), reads=[o], writes=[o])
                S.op("vector", lambda e: e.tensor_scalar(out=o[:, :n], in0=o[:, :n], scalar1=1.0 / 16.0, scalar2=None, op0=ALU.mult), reads=[o], writes=[o])
                if fwd:
                    put(gs["lw_f"], None, o)
                else:
                    put(None, gs["lw_b"], o)
            if lat:
                p = fmg(4, 1, n)
                ob = gob.get()
                S.op("scalar", lambda e: e.activation(out=ob[:, :n], in_=p[:, :n], func=AF.Silu), reads=[p], writes=[ob])
                S.store("sync", out, out[1, :, t0 - L:t0 - L + n], ob, ob[:, :n])
            sh = {}
            for gi, g in enumerate(range(5, 13)):
                p = fmg(g, 0, ne)
                pe = pool.get()
                S.op("scalar", lambda e: e.activation(out=pe[:, :ne], in_=p[:, :ne], func=AF.Copy), reads=[p], writes=[pe])
                if t0 == seg0:
                    S.op("gpsimd", lambda e: e.memset(pe[:, 0:1], 0.0), reads=[pe], writes=[pe])
                if t0 + n == seg0 + seglen:
                    S.op("gpsimd", lambda e: e.memset(pe[:, n + 1:n + 2], 0.0), reads=[pe], writes=[pe])
                o = pool.get()
                S.op("vector", lambda e: e.tensor_scalar(out=o[:, :n], in0=pe[:, 1:n + 1], scalar1=sm[:, 25 + gi:26 + gi], scalar2=None, op0=ALU.mult),
                     reads=[pe, sm], writes=[o])
                S.op("vector", lambda e: e.scalar_tensor_tensor(out=o[:, :n], in0=pe[:, 0:n], scalar=sm[:, 8 + gi:9 + gi], in1=o[:, :n],
                                                                op0=ALU.mult, op1=ALU.add), reads=[pe, sm, o], writes=[o])
                S.op("vector", lambda e: e.scalar_tensor_tensor(out=o[:, :n], in0=pe[:, 2:n + 2], scalar=sm[:, 16 + gi:17 + gi], in1=o[:, :n],
                                                                op0=ALU.mult, op1=ALU.add), reads=[pe, sm, o], writes=[o])
                sh[g] = o
            rr, rk, rv, wdf, wdb, ad, gd0, gd1 = (sh[g] for g in range(5, 13))
            put(rs["r_f"], rs["r_b"], rr)
            put(rs["v_f"], rs["v_b"], rv)
            for (wd_, wi, bcol, fwd) in ((wdf, W2F, 2, True), (wdb, W2B, 3, False)):
                S.op("scalar", lambda e: e.activation(out=wd_[:, :n], in_=wd_[:, :n], func=AF.Tanh), reads=[wd_], writes=[wd_])
                p = pss.get()
                S.op("tensor", lambda e: e.matmul(p[:, :n], lr[:, wi, :], wd_[:, :n], start=True, stop=True), reads=[lr, wd_], writes=[p])
                o = pool.get()
                S.op("scalar", lambda e: e.activation(out=o[:, :n], in_=p[:, :n], func=AF.Sigmoid, bias=sm[:, bcol:bcol + 1]), reads=[p, sm], writes=[o])
                S.op("vector", lambda e: e.tensor_scalar(out=o[:, :n], in0=o[:, :n], scalar1=-float(np.exp(-0.5)), scalar2=None, op0=ALU.mult),
                     reads=[o], writes=[o])
                if fwd:
                    put(rs["lw_f"], None, o)
                else:
                    put(None, rs["lw_b"], o)
            p = pss.get()
            S.op("tensor", lambda e: e.matmul(p[:, :n], lr[:, A2, :], ad[:, :n], start=True, stop=True), reads=[lr, ad], writes=[p])
            a_ = pool.get()
            S.op("scalar", lambda e: e.activation(out=a_[:, :n], in_=p[:, :n], func=AF.Sigmoid, bias=sm[:, 4:5]), reads=[p, sm], writes=[a_])
            kk = pool.get()
            S.op("vector", lambda e: e.tensor_scalar(out=kk[:, :n], in0=rk[:, :n], scalar1=sm[:, 5:6], scalar2=None, op0=ALU.mult), reads=[rk, sm], writes=[kk])
            k2 = pool.get()
            S.op("scalar", lambda e: e.activation(out=k2[:, :n], in_=kk[:, :n], func=AF.Square), reads=[kk], writes=[k2])
            p = pss.get()
            S.op("tensor", lambda e: e.matmul(p[:, :n], lr[:, BD, :], k2[:, :n], start=True, stop=True), reads=[lr, k2], writes=[p])
            S.op("vector", lambda e: e.tensor_scalar(out=k2[:, :n], in0=p[:, :n], scalar1=1e-12, scalar2=None, op0=ALU.add), reads=[p], writes=[k2])
            S.op("scalar", lambda e: e.activation(out=k2[:, :n], in_=k2[:, :n], func=AF.Sqrt), reads=[k2], writes=[k2])
            S.op("vector", lambda e: e.reciprocal(out=k2[:, :n], in_=k2[:, :n]), reads=[k2], writes=[k2])
            S.op("gpsimd", lambda e: e.tensor_tensor(out=kk[:, :n], in0=kk[:, :n], in1=k2[:, :n], op=ALU.mult), reads=[kk, k2], writes=[kk])
            bv = pool.get()
            S.op("gpsimd", lambda e: e.tensor_tensor(out=bv[:, :n], in0=kk[:, :n], in1=a_[:, :n], op=ALU.mult), reads=[kk, a_], writes=[bv])
            put(rs["b_f"], rs["b_b"], bv)
            av = pool.get()
            S.op("vector", lambda e: e.tensor_scalar(out=av[:, :n], in0=kk[:, :n], scalar1=-1.0, scalar2=None, op0=ALU.mult), reads=[kk], writes=[av])
            put(rs["a_f"], rs["a_b"], av)
            km = pool.get()
            S.op("vector", lambda e: e.tensor_scalar(out=km[:, :n], in0=a_[:, :n], scalar1=sm[:, 6:7], scalar2=sm[:, 24:25], op0=ALU.mult, op1=ALU.add),
                 reads=[a_, sm], writes=[km])
            S.op("gpsimd", lambda e: e.tensor_tensor(out=km[:, :n], in0=km[:, :n], in1=rk[:, :n], op=ALU.mult), reads=[km, rk], writes=[km])
            put(rs["k_f"], rs["k_b"], km)
            if lat:
                t1 = pool.get()
                S.op("vector", lambda e: e.scalar_tensor_tensor(out=t1[:, :n], in0=rr[:, :n], scalar=sm[:, 7:8], in1=km[:, :n], op0=ALU.mult, op1=ALU.mult),
                     reads=[rr, sm, km], writes=[t1])
                p = pss.get()
                S.op("tensor", lambda e: e.matmul(p[:, :n], lr[:, BD, :], t1[:, :n], start=True, stop=True), reads=[lr, t1], writes=[p])
                bo = pool.get()
                S.op("vector", lambda e: e.tensor_tensor(out=bo[:, :n], in0=rv[:, :n], in1=p[:, :n], op=ALU.mult), reads=[rv, p], writes=[bo])
                put(rs["bonus"], None, bo)
                S.op("scalar", lambda e: e.activation(out=gd0[:, :n], in_=gd0[:, :n], func=AF.Sigmoid), reads=[gd0], writes=[gd0])
                S.op("scalar", lambda e: e.activation(out=gd1[:, :n], in_=gd1[:, :n], func=AF.Sigmoid), reads=[gd1], writes=[gd1])
                p = pss.get()
                S.op("tensor", lambda e: e.matmul(p[:, :n], lr[:, G20, :], gd0[:, :n], start=True, stop=False), reads=[lr, gd0], writes=[p])
                S.op("tensor", lambda e: e.matmul(p[:, :n], lr[:, G21, :], gd1[:, :n], start=False, stop=True), reads=[lr, gd1], writes=[p])
                go = pool.get()
                S.op("scalar", lambda e: e.activation(out=go[:, :n], in_=p[:, :n], func=AF.Copy), reads=[p], writes=[go])
                put(rs["gout"], None, go)
        barrier(S)
        S.reset(_ph)
    for _ph in (S.mark(),):
        consts = scan_consts(S, cst_d)
        units = [dict(K=128, V=128, q=gs["q_f"], k=gs["k_f"], lw=gs["lw_f"], v=gs["v_f"], out=gs["o_f"]),
                 dict(K=128, V=128, q=gs["q_b"], k=gs["k_b"], lw=gs["lw_b"], v=gs["v_b"], out=gs["o_b"])]
        if "gla" not in SKIP:
            chunk_scan(S, cfg, units, consts)
        barrier(S)
        S.reset(_ph)
    for _rw in range(0 if "rwkv" not in SKIP else 1, 1):
        for _ph in (S.mark(),):
            consts = scan_consts(S, cst_d)
            units = []
            for hh, d in ((0, "f"), (0, "b"), (1, "f"), (1, "b")):
                U = {}
                for nm, key in (("q", "r"), ("k", "k"), ("v", "v"), ("lw", "lw"), ("a", "a"), ("b", "b")):
                    U[nm] = rs[f"{key}_{d}"][64 * hh:64 * hh + 64, :]
                    U["src_" + nm] = rs[f"{key}_{d}"]
                U["out"] = rs[f"o_{d}"][64 * hh:64 * hh + 64, :]
                U["src_out"] = rs[f"o_{d}"]
                units.append(U)
            rwkv_scan(S, cfg, units, consts)
            barrier(S)
            S.reset(_ph)
    for _ph in (S.mark(),):
        sm = load_const(S, "sm2", (128, 40), sm_d[:])
        bd = load_const(S, "bd", (128, 128), lr_d[7])
        S.op("vector", lambda e: e.tensor_scalar(out=bd[:], in0=bd[:], scalar1=1.0 / 64.0, scalar2=None, op0=ALU.mult), reads=[bd], writes=[bd])
        tp = rot_sbuf(S, "p5", (128, 512), F32, n=12)
        ob_ = rot_sbuf(S, "p5o", (128, 512), BF16, n=4)
        ps = Rot([S.psum(f"p5ps{i}") for i in range(4)])
        for s0 in range(0, SS, 512):
            n = min(512, SS - s0)
            t0 = L + s0
            rp = rev_pos(t0, n, L, SS)
            a, b = tp.get(), tp.get()
            S.load("sync", a, a[:, :n], gs["o_f"][:, t0:t0 + n], src=gs["o_f"])
            S.load("scalar", b, b[:, :n], gs["o_b"][:, rp:rp + n], src=gs["o_b"])
            o = ob_.get()
            S.op("vector", lambda e: e.tensor_tensor(out=o[:, :n], in0=a[:, :n], in1=rev_ap(b[:, :n]), op=ALU.add), reads=[a, b], writes=[o])
            S.store("sync", out, out[0, :, s0:s0 + n], o, o[:, :n])
            a, b, bo, go = tp.get(), tp.get(), tp.get(), tp.get()
            S.load("sync", a, a[:, :n], rs["o_f"][:, t0:t0 + n], src=rs["o_f"])
            S.load("scalar", b, b[:, :n], rs["o_b"][:, rp:rp + n], src=rs["o_b"])
            S.load("sync", bo, bo[:, :n], rs["bonus"][:, t0:t0 + n], src=rs["bonus"])
            S.load("scalar", go, go[:, :n], rs["gout"][:, t0:t0 + n], src=rs["gout"])
            S.op("vector", lambda e: e.tensor_tensor(out=a[:, :n], in0=a[:, :n], in1=rev_ap(b[:, :n]), op=ALU.add), reads=[a, b], writes=[a])
            p1 = ps.get()
            S.op("tensor", lambda e: e.matmul(p1[:, :n], bd[:], a[:, :n], start=True, stop=True), reads=[bd, a], writes=[p1])
            d_ = tp.get()
            S.op("vector", lambda e: e.tensor_tensor(out=d_[:, :n], in0=a[:, :n], in1=p1[:, :n], op=ALU.subtract), reads=[a, p1], writes=[d_])
            d2 = tp.get()
            S.op("scalar", lambda e: e.activation(out=d2[:, :n], in_=d_[:, :n], func=AF.Square), reads=[d_], writes=[d2])
            p2 = ps.get()
            S.op("tensor", lambda e: e.matmul(p2[:, :n], bd[:], d2[:, :n], start=True, stop=True), reads=[bd, d2], writes=[p2])
            S.op("vector", lambda e: e.tensor_scalar(out=d2[:, :n], in0=p2[:, :n], scalar1=RW_LN_EPS, scalar2=None, op0=ALU.add), reads=[p2], writes=[d2])
            S.op("scalar", lambda e: e.activation(out=d2[:, :n], in_=d2[:, :n], func=AF.Sqrt), reads=[d2], writes=[d2])
            S.op("vector", lambda e: e.reciprocal(out=d2[:, :n], in_=d2[:, :n]), reads=[d2], writes=[d2])
            S.op("gpsimd", lambda e: e.tensor_tensor(out=d_[:, :n], in0=d_[:, :n], in1=d2[:, :n], op=ALU.mult), reads=[d_, d2], writes=[d_])
            S.op("vector", lambda e: e.tensor_scalar(out=d_[:, :n], in0=d_[:, :n], scalar1=sm[:, 33:34], scalar2=sm[:, 34:35], op0=ALU.mult, op1=ALU.add),
                 reads=[d_, sm], writes=[d_])
            S.op("gpsimd", lambda e: e.tensor_tensor(out=d_[:, :n], in0=d_[:, :n], in1=bo[:, :n], op=ALU.add), reads=[d_, bo], writes=[d_])
            o = ob_.get()
            S.op("vector", lambda e: e.tensor_tensor(out=o[:, :n], in0=d_[:, :n], in1=go[:, :n], op=ALU.mult), reads=[d_, go], writes=[o])
            S.store("sync", out, out[2, :, s0:s0 + n], o, o[:, :n])
    return nc, stack, S


def run_mix1(cfg, hT_all, inp):
    nc, stack, S = build_mix1(cfg)
    w_in = inp["l1_w_in"]
    hfm = fm(hT_all)
    sc = host_scan_consts(rwkv=True)
    RW0 = 3104
    maps = []
    pidx = np.arange(128)
    bd = (pidx[:, None] // 64 == pidx[None, :] // 64).astype(np.float32)

    def pad_cols(w, n=128):
        return np.concatenate([w, np.zeros((w.shape[0], n - w.shape[1]), np.float32)], axis=1)

    def pad_rows(w, r0=0, n=128):
        o = np.zeros((n, w.shape[1]), np.float32)
        o[r0:r0 + w.shape[0]] = w
        return o

    def pad_vec(v, n=128):
        o = np.zeros((n,), np.float32)
        o[:v.shape[0]] = v
        return o
    for c in range(NCORES):
        gh, vh = c // 2, c % 2
        ch = slice(c * 128, (c + 1) * 128)
        cols = [w_in[:, gh * 128:(gh + 1) * 128], w_in[:, 512 + gh * 128:512 + (gh + 1) * 128],
                w_in[:, 1024 + gh * 256 + vh * 128:1024 + gh * 256 + (vh + 1) * 128],
                pad_cols(w_in[:, 2048:2080]), w_in[:, 2080 + gh * 256 + vh * 128:2080 + gh * 256 + (vh + 1) * 128]]
        rcols = [(0, ch), (1024, ch), (2048, ch)]
        mu_p, mu_n = inp["l1_rwkv_mu_prev"], inp["l1_rwkv_mu_next"]
        mup_g, mun_g = [], []
        for base, sl in rcols:
            cols.append(w_in[:, RW0 + base + sl.start:RW0 + base + sl.stop])
            mup_g.append(mu_p[base + sl.start:base + sl.stop])
            mun_g.append(mu_n[base + sl.start:base + sl.stop])
        for base, width in ((3072, 96), (3168, 96), (3264, 96), (3360, 128), (3488, 128)):
            cols.append(pad_cols(w_in[:, RW0 + base:RW0 + base + width]))
            mup_g.append(pad_vec(mu_p[base:base + width]))
            mun_g.append(pad_vec(mu_n[base:base + width]))
        sm = np.zeros((128, 40), np.float32)
        gk = slice(gh * 128, (gh + 1) * 128)
        sm[:, 0] = inp["l1_gla_gate_bias_f"][gk]
        sm[:, 1] = inp["l1_gla_gate_bias_b"][gk]
        sm[:, 2] = inp["l1_rwkv_w0_f"][ch]
        sm[:, 3] = inp["l1_rwkv_w0_b"][ch]
        sm[:, 4] = inp["l1_rwkv_a0"][ch]
        sm[:, 5] = inp["l1_rwkv_k_k"][ch]
        sm[:, 6] = inp["l1_rwkv_k_a"][ch]
        sm[:, 7] = inp["l1_rwkv_r_k"].reshape(-1)[ch]
        for gi in range(8):
            sm[:, 8 + gi] = mup_g[gi]
            sm[:, 16 + gi] = mun_g[gi]
        sm[:, 33] = inp["l1_rwkv_ln_w"][ch]
        sm[:, 34] = inp["l1_rwkv_ln_b"][ch]
        lrk = np.stack([pad_rows(inp["l1_gla_gate_up_f"][:, gk], 0), pad_rows(inp["l1_gla_gate_up_b"][:, gk], 16),
                        pad_rows(inp["l1_rwkv_w2_f"][:, ch]), pad_rows(inp["l1_rwkv_w2_b"][:, ch]), pad_rows(inp["l1_rwkv_a2"][:, ch]),
                        inp["l1_rwkv_g2"][0:128, ch], inp["l1_rwkv_g2"][128:256, ch], bd], axis=0)
        m = {"hT": hfm, "w": wfm(np.concatenate(cols, axis=1)), "smalls": sm, "lowrank": np.ascontiguousarray(lrk)}
        m.update(sc)
        maps.append(m)
    res = run_prog(nc, stack, S, maps)
    if DEBUG:
        global DBG
        DBG = res
    gla_o = np.concatenate([res[c]["oT"][0] for c in range(NCORES)], axis=0)
    gla_g = np.concatenate([res[c]["oT"][1] for c in range(NCORES)], axis=0)
    rw = np.concatenate([res[c]["oT"][2] for c in range(NCORES)], axis=0)
    return gla_o, gla_g, rw


def build_l5(cfg):
    nc, stack, S = new_prog()
    KC, NB, ntl = cfg.KC, 256, cfg.ntl
    NE = cfg.N_EXP
    xT = S.dram("xT", (128, KC, ntl), F32, "ExternalInput")
    mo = S.dram("mo", (3, 128, 8, ntl), BF16, "ExternalInput")
    mod_d = S.dram("mod", (128, 6 * KC, 2), F32, "ExternalInput")
    gains_d = S.dram("gains", (128, 2, KC), F32, "ExternalInput")
    gn_d = S.dram("gnorm", (128, 2), F32, "ExternalInput")
    wo_d = S.dram("wo", (KC, 128, KC * 128), F32, "ExternalInput")
    rt_d = S.dram("router", (128, KC, NE), F32, "ExternalInput")
    x3T = S.dram("x3T", (128, KC, ntl), F32, "ExternalOutput")
    h2T = S.dram("h2T", (128, KC, ntl), BF16, "ExternalOutput")
    gates = S.dram("gates", (ntl, NE), F32, "ExternalOutput")
    wo_b = S.dram("wo_b", (KC, 128, KC * 128), BF16)
    for _ph in (S.mark(),):
        sf, sb = rot_sbuf(S, "cv_f", (128, 2048), F32, n=3), rot_sbuf(S, "cv_b", (128, 2048), BF16, n=3)
        convert_w(S, wo_d, wo_b, KC, KC * 128, sf, sb, [0])
        barrier(S)
        S.reset(_ph)
    mod = load_const(S, "mod_sb", (128, 6 * KC, 2), mod_d[:])
    gains = load_const(S, "gains_sb", (128, 2, KC), gains_d[:])
    gn = [Buf(f"gain{i}", gains[:, i, :]) for i in range(2)]
    for g in gn:
        g.writers = gains.writers
    gnorm = load_const(S, "gnorm_sb", (128, 2), gn_d[:])
    S.op("vector", lambda e: e.tensor_scalar(out=gnorm[:], in0=gnorm[:], scalar1=16.0, scalar2=None, op0=ALU.mult), reads=[gnorm], writes=[gnorm])
    router = load_const(S, "router_sb", (128, KC, NE), rt_d[:])
    ones = make_ones(S)
    G1 = gate_scalars(S, cfg, mod, gn[0], 2, "g1")
    A2, B2 = mod_scalars(S, cfg, mod, gn[1], 4, 3, "m2")
    xb = S.sbuf("xb", (128, KC, NB))
    go = S.sbuf("go", (128, 8, NB), BF16)
    gg = S.sbuf("gg", (128, 8, NB), BF16)
    ob = S.sbuf("ob", (128, KC, NB), BF16)
    yb = S.sbuf("yb", (128, KC, NB))
    hf = S.sbuf("hf", (128, KC, NB))
    hb = S.sbuf("hb", (128, KC, NB), BF16)
    sq = rot_sbuf(S, "sq", (128, NB))
    tmp = rot_sbuf(S, "tmp", (128, NB), n=4)
    rstd = S.sbuf("rstd", (128, NB))
    wo = rot_sbuf(S, "wo", (128, KC * 128), BF16, n=3)
    ps_s = S.psum("ps_stat")
    ps_y = Rot([S.psum("ps_y0"), S.psum("ps_y1")])
    ps_r = S.psum("ps_r")
    lg = rot_sbuf(S, "lg", (128, 8), n=2)
    m8 = rot_sbuf(S, "m8", (128, 8), n=2)
    ex = rot_sbuf(S, "ex", (128, 8), n=2)
    mk = rot_sbuf(S, "mk", (128, 8), n=2)
    s1 = rot_sbuf(S, "s1", (128, 2), n=2)
    for s0 in range(0, ntl, NB):
        n = min(NB, ntl - s0)
        S.load("sync", xb, xb[:, :, :n], xT[:, :, s0:s0 + n])
        S.load("scalar", go, go[:, :, :n], mo[0, :, :, s0:s0 + n])
        S.load("sync", gg, gg[:, :, :n], mo[1, :, :, s0:s0 + n])
        S.load("scalar", ob, ob[:, 8:16, :n], mo[2, :, :, s0:s0 + n])
        for hh in range(4):
            for c in (2 * hh, 2 * hh + 1):
                q = sq.get()
                S.op("scalar", lambda e: e.activation(out=q[:, :n], in_=go[:, c, :n], func=AF.Square), reads=[go], writes=[q])
                S.op("tensor", lambda e: e.matmul(ps_s[:, :n], ones[:], q[:, :n], start=(c % 2 == 0), stop=(c % 2 == 1)), reads=[ones, q], writes=[ps_s])
            S.op("vector", lambda e: e.tensor_scalar(out=rstd[:, :n], in0=ps_s[:, :n], scalar1=NORM_EPS * 256, scalar2=None, op0=ALU.add),
                 reads=[ps_s], writes=[rstd])
            S.op("scalar", lambda e: e.activation(out=rstd[:, :n], in_=rstd[:, :n], func=AF.Sqrt), reads=[rstd], writes=[rstd])
            S.op("vector", lambda e: e.reciprocal(out=rstd[:, :n], in_=rstd[:, :n]), reads=[rstd], writes=[rstd])
            for c in (2 * hh, 2 * hh + 1):
                t = tmp.get()
                S.op("vector", lambda e: e.scalar_tensor_tensor(out=t[:, :n], in0=go[:, c, :n], scalar=gnorm[:, c % 2:c % 2 + 1], in1=rstd[:, :n],
                                                                op0=ALU.mult, op1=ALU.mult), reads=[go, gnorm, rstd], writes=[t])
                S.op("gpsimd", lambda e: e.tensor_tensor(out=ob[:, c, :n], in0=t[:, :n], in1=gg[:, c, :n], op=ALU.mult), reads=[t, gg], writes=[ob])
        for dc in range(KC):
            w = wo.get()
            S.load("sync", w, w[:], wo_b[dc], src=wo_b)
            p = ps_y.get()
            for kc in range(KC):
                S.op("tensor", lambda e: e.matmul(p[:, :n], w[:, kc * 128:(kc + 1) * 128], ob[:, kc, :n], start=(kc == 0), stop=(kc == KC - 1)),
                     reads=[w, ob], writes=[p])
            S.op("scalar", lambda e: e.activation(out=yb[:, dc, :n], in_=p[:, :n], func=AF.Copy), reads=[p], writes=[yb])
        rms_stats(S, cfg, yb, n, sq, ones, ps_s, rstd)
        resid_norm_add(S, cfg, xb, yb, n, rstd, G1[0], tmp)
        S.store("sync", x3T, x3T[:, :, s0:s0 + n], xb, xb[:, :, :n])
        rms_stats(S, cfg, xb, n, sq, ones, ps_s, rstd)
        norm_mod_apply(S, cfg, xb, n, rstd, A2[0], B2[0], hf, tmp)
        S.op("scalar", lambda e: e.activation(out=hb[:, :, :n], in_=hf[:, :, :n], func=AF.Copy), reads=[hf], writes=[hb])
        S.store("scalar", h2T, h2T[:, :, s0:s0 + n], hb, hb[:, :, :n])
        for tb in range(0, n if ROUTER_ON else 0, 128):
            for kc in range(KC):
                S.op("tensor", lambda e: e.matmul(ps_r[:, 0:NE], hf[:, kc, tb:tb + 128], router[:, kc, :], start=(kc == 0), stop=(kc == KC - 1)),
                     reads=[hf, router], writes=[ps_r])
            l_, m_, e_, k_, s_ = lg.get(), m8.get(), ex.get(), mk.get(), s1.get()
            X = mybir.AxisListType.X
            BIGV = 1.0e30
            S.op("vector", lambda e: e.tensor_copy(out=l_[:], in_=ps_r[:, 0:NE]), reads=[ps_r], writes=[l_])
            S.op("vector", lambda e: e.reduce_max(out=s_[:, 0:1], in_=l_[:], axis=X), reads=[l_], writes=[s_])
            S.op("vector", lambda e: e.tensor_scalar(out=l_[:], in0=l_[:], scalar1=s_[:, 0:1], scalar2=None, op0=ALU.subtract), reads=[l_, s_], writes=[l_])
            S.op("vector", lambda e: e.tensor_scalar(out=m_[:], in0=l_[:], scalar1=BIGV, scalar2=1.0, op0=ALU.mult, op1=ALU.add), reads=[l_], writes=[m_])
            S.op("vector", lambda e: e.tensor_scalar(out=m_[:], in0=m_[:], scalar1=0.0, scalar2=-BIGV, op0=ALU.max, op1=ALU.mult), reads=[m_], writes=[m_])
            S.op("vector", lambda e: e.tensor_tensor(out=m_[:], in0=m_[:], in1=l_[:], op=ALU.add), reads=[m_, l_], writes=[m_])
            S.op("vector", lambda e: e.reduce_max(out=s_[:, 1:2], in_=m_[:], axis=X), reads=[m_], writes=[s_])
            S.op("vector", lambda e: e.tensor_scalar(out=k_[:], in0=l_[:], scalar1=s_[:, 1:2], scalar2=BIGV, op0=ALU.subtract, op1=ALU.mult),
                 reads=[l_, s_], writes=[k_])
            S.op("vector", lambda e: e.tensor_scalar(out=k_[:], in0=k_[:], scalar1=1.0, scalar2=0.0, op0=ALU.add, op1=ALU.max), reads=[k_], writes=[k_])
            S.op("vector", lambda e: e.tensor_scalar(out=k_[:], in0=k_[:], scalar1=1.0, scalar2=None, op0=ALU.min), reads=[k_], writes=[k_])
            S.op("scalar", lambda e: e.activation(out=e_[:], in_=l_[:], func=AF.Exp), reads=[l_], writes=[e_])
            S.op("vector", lambda e: e.tensor_tensor(out=e_[:], in0=e_[:], in1=k_[:], op=ALU.mult), reads=[e_, k_], writes=[e_])
            S.op("vector", lambda e: e.reduce_sum(out=s_[:, 0:1], in_=e_[:], axis=X), reads=[e_], writes=[s_])
            S.op("vector", lambda e: e.reciprocal(out=s_[:, 0:1], in_=s_[:, 0:1]), reads=[s_], writes=[s_])
            S.op("vector", lambda e: e.tensor_scalar(out=e_[:], in0=e_[:], scalar1=s_[:, 0:1], scalar2=None, op0=ALU.mult), reads=[e_, s_], writes=[e_])
            S.store("sync", gates, gates[s0 + tb:s0 + tb + 128, :], e_, e_[:])
    return nc, stack, S


def run_l5(cfg, x2T_lat, gla_o, gla_g, rw, mod1, inp):
    nc, stack, S = build_l5(cfg)
    KC, ntl = cfg.KC, cfg.ntl
    wo = inp["l1_w_out"]
    wo_l = np.ascontiguousarray(wo.reshape(KC, 128, KC, 128).transpose(2, 1, 0, 3).reshape(KC, 128, KC * 128))
    gains = np.ascontiguousarray(np.stack([vec_fm(inp[k]) for k in ("l1_norm_mix_post", "l1_norm_ffn_pre")], axis=1))
    gnorm = np.ascontiguousarray(inp["l1_gla_norm"].reshape(2, 128).T)
    router = wfm(inp["l1_moe_router"])
    maps = []
    for i in range(NCORES):
        sl = slice(i * ntl, (i + 1) * ntl)
        mo = np.stack([fm(np.ascontiguousarray(a[:, sl])) for a in (gla_o, gla_g, rw)], axis=0)
        maps.append({"xT": fm(np.ascontiguousarray(x2T_lat[:, sl])), "mo": mo, "mod": mod1, "gains": gains, "gnorm": gnorm, "wo": wo_l, "router": router})
    res = run_prog(nc, stack, S, maps)
    x3T = np.concatenate([unfm(res[i]["x3T"]) for i in range(NCORES)], axis=1)
    h2T = np.concatenate([unfm(res[i]["h2T"]) for i in range(NCORES)], axis=1)
    gates = np.concatenate([res[i]["gates"] for i in range(NCORES)], axis=0)
    return x3T, h2T, gates


def build_moe(cfg):
    nc, stack, S = new_prog()
    KC, SS = cfg.KC, cfg.S
    NJ = cfg.D_FF_E // 128
    NB = 512
    hT = S.dram("hT", (128, KC, SS), BF16, "ExternalInput")
    gate_d = S.dram("gate", (128, SS), F32, "ExternalInput")
    wg_d = S.dram("wg", (NJ, 128, KC * 128), F32, "ExternalInput")
    wu_d = S.dram("wu", (NJ, 128, KC * 128), F32, "ExternalInput")
    wd_d = S.dram("wd", (KC, 128, NJ * 128), F32, "ExternalInput")
    out = S.dram("part", (128, KC, SS), BF16, "ExternalOutput")
    wg_b = S.dram("wg_b", (NJ, 128, KC * 128), BF16)
    wu_b = S.dram("wu_b", (NJ, 128, KC * 128), BF16)
    wd_b = S.dram("wd_b", (KC, 128, NJ * 128), BF16)
    for _ph in (S.mark(),):
        sf, sb = rot_sbuf(S, "cv_f", (128, 2048), F32, n=4), rot_sbuf(S, "cv_b", (128, 2048), BF16, n=4)
        ctr = [0]
        convert_w(S, wg_d, wg_b, NJ, KC * 128, sf, sb, ctr)
        convert_w(S, wu_d, wu_b, NJ, KC * 128, sf, sb, ctr)
        convert_w(S, wd_d, wd_b, KC, NJ * 128, sf, sb, ctr)
        barrier(S)
        S.reset(_ph)
    fb = ffn_bufs(S, cfg, NJ, NB)
    hbs = rot_sbuf(S, "hb", (128, KC, NB), BF16, n=2)
    gbs = rot_sbuf(S, "gb", (128, NB), F32, n=2)
    obs = rot_sbuf(S, "obs", (128, NB), BF16, n=4)
    for s0 in range(0, SS, NB):
        n = min(NB, SS - s0)
        hb, gb = hbs.get(), gbs.get()
        S.load("sync", hb, hb[:, :, :n], hT[:, :, s0:s0 + n])
        S.load("scalar", gb, gb[:, :n], gate_d[:, s0:s0 + n])

        def evac(dc, po):
            o = obs.get()
            S.op("vector", lambda e: e.tensor_tensor(out=o[:, :n], in0=gb[:, :n], in1=po[:, :n], op=ALU.mult), reads=[gb, po], writes=[o])
            S.store("gpsimd", out, out[:, dc, s0:s0 + n], o, o[:, :n])
        ffn_block(S, cfg, hb, n, NJ, wg_b, wu_b, wd_b, fb, evac)
    return nc, stack, S


def run_moe(cfg, h2T, gates, inp):
    nc, stack, S = build_moe(cfg)
    hfm = fm(h2T)
    maps = []
    for e in range(NCORES):
        wg, wu, wd = host_ffn_w(inp["l1_moe_w_gate"][e], inp["l1_moe_w_up"][e], inp["l1_moe_w_down"][e])
        g = np.ascontiguousarray(np.broadcast_to(gates[:, e][None, :], (128, cfg.S)))
        maps.append({"hT": hfm, "gate": g, "wg": wg, "wu": wu, "wd": wd})
    res = run_prog(nc, stack, S, maps)
    return [res[e]["part"] for e in range(NCORES)]


def build_fin(cfg):
    nc, stack, S = new_prog()
    KC, NB, ntl = cfg.KC, 256, cfg.ntl
    NE = cfg.N_EXP
    xT = S.dram("xT", (128, KC, ntl), F32, "ExternalInput")
    parts = S.dram("parts", (NE, 128, KC, ntl), BF16, "ExternalInput")
    mod_d = S.dram("mod", (128, 6 * KC, 2), F32, "ExternalInput")
    gain_d = S.dram("gain", (128, KC), F32, "ExternalInput")
    outT = S.dram("outT", (128, KC, ntl), F32, "ExternalOutput")
    mod = load_const(S, "mod_sb", (128, 6 * KC, 2), mod_d[:])
    gain = load_const(S, "gain_sb", (128, KC), gain_d[:])
    ones = make_ones(S)
    G2 = gate_scalars(S, cfg, mod, gain, 5, "g2")
    xb = rot_sbuf(S, "xb", (128, KC, NB), n=2)
    pb = rot_sbuf(S, "pb", (128, KC, NB), BF16, n=4)
    fbuf = S.sbuf("fb", (128, KC, NB))
    sq = rot_sbuf(S, "sq", (128, NB))
    tmp = rot_sbuf(S, "tmp", (128, NB), n=4)
    rstd = S.sbuf("rstd", (128, NB))
    ps_s = S.psum("ps_stat")
    for s0 in range(0, ntl, NB):
        n = min(NB, ntl - s0)
        x = xb.get()
        S.load("sync", x, x[:, :, :n], xT[:, :, s0:s0 + n])
        for e_ in range(NE):
            p = pb.get()
            S.load("scalar" if e_ % 2 else "sync", p, p[:, :, :n], parts[e_, :, :, s0:s0 + n])
            eng = "vector" if e_ % 2 == 0 else "gpsimd"
            if e_ == 0:
                S.op(eng, lambda e: e.tensor_copy(out=fbuf[:, :, :n], in_=p[:, :, :n]), reads=[p], writes=[fbuf])
            else:
                S.op(eng, lambda e: e.tensor_tensor(out=fbuf[:, :, :n], in0=fbuf[:, :, :n], in1=p[:, :, :n], op=ALU.add), reads=[fbuf, p], writes=[fbuf])
        rms_stats(S, cfg, fbuf, n, sq, ones, ps_s, rstd)
        resid_norm_add(S, cfg, x, fbuf, n, rstd, G2[0], tmp)
        S.store("sync", outT, outT[:, :, s0:s0 + n], x, x[:, :, :n])
    return nc, stack, S


def run_fin(cfg, x3T, parts, mod1, inp):
    nc, stack, S = build_fin(cfg)
    ntl = cfg.ntl
    maps = []
    for i in range(NCORES):
        sl = slice(i * ntl, (i + 1) * ntl)
        maps.append({"xT": fm(np.ascontiguousarray(x3T[:, sl])), "parts": np.ascontiguousarray(np.stack([p[:, :, sl] for p in parts], axis=0)),
                     "mod": mod1, "gain": vec_fm(inp["l1_norm_ffn_post"])})
    res = run_prog(nc, stack, S, maps)
    return np.concatenate([unfm(res[i]["outT"]) for i in range(NCORES)], axis=1)


def kernel(**inp):
    inp = {k: np.asarray(v) for k, v in inp.items()}
    S_len, L_len = inp["x"].shape[1], inp["ctx"].shape[1]
    cfg = Cfg(S=S_len, L=L_len, D=inp["x"].shape[2], D_FF=inp["l0_ffn_w_gate"].shape[1], N_EXP=inp["l1_moe_w_gate"].shape[0],
              D_FF_E=inp["l1_moe_w_gate"].shape[2])
    xT_lat = np.ascontiguousarray(inp["x"][0].T)
    xT_ctx = np.ascontiguousarray(inp["ctx"][0].T)
    mods = run_ada(cfg, inp)
    hT0 = run_pre(cfg, xT_lat, xT_ctx, mods[0], inp["l0_norm_mix_pre"])
    oT0 = run_mix0(cfg, hT0, inp)
    x2c, x2l, hT1 = run_l3(cfg, xT_lat, xT_ctx, oT0, mods, inp)
    gla_o, gla_g, rw = run_mix1(cfg, hT1, inp)
    x3T, h2T, gates = run_l5(cfg, x2l, gla_o, gla_g, rw, mods[1], inp)
    parts = run_moe(cfg, h2T, gates, inp)
    outT = run_fin(cfg, x3T, parts, mods[1], inp)
    return np.ascontiguousarray(outT.T)[None].astype(np.float32)
```

```python
import contextlib
import numpy as np
import ml_dtypes
import concourse.bass as bass
import concourse.mybir as mybir
from concourse.bass_utils import run_bass_kernel_spmd

F32 = mybir.dt.float32
BF16 = mybir.dt.bfloat16
U8 = mybir.dt.uint8
AF = mybir.ActivationFunctionType
ALU = mybir.AluOpType
NCORES = 8
NORM_EPS = 1e-6
DEBUG = False
DEBUG_BI = 0
RECYCLE_SEMS = True
ROUTER_ON = True
SKIP = set()


class Cfg:
    def __init__(self, S=16384, L=256, D=2048, D_FF=5632, N_EXP=8, D_FF_E=7168):
        self.S, self.L, self.D, self.D_FF, self.N_EXP, self.D_FF_E = S, L, D, D_FF, N_EXP, D_FF_E
        self.T = S + L
        self.KC = D // 128
        self.ntl = S // NCORES
        self.ntc = L // NCORES
        self.ntok = self.ntl + self.ntc


class Tok:
    __slots__ = ("sem", "count", "closed", "deps")

    def __init__(self, sem, count):
        self.sem, self.count, self.closed, self.deps = sem, count, False, []


class Buf:
    def __init__(self, name, t=None, excl=False, accum=False):
        self.name, self.t, self.excl, self.accum = name, t, excl, accum
        self.writers, self.readers = {}, {}
        self.dma_sem, self.dma_tok = None, None

    def __getitem__(self, idx):
        return self.t[idx]


class _Rec:
    def __getattr__(self, name):
        def f(*a, **k):
            self.call = (name, a, k)
        return f


class Eng:
    def __init__(self, name, sem):
        self.name, self.sem, self.count, self.items, self.seen = name, sem, 0, [], {}


class Sched:
    ENGS = ("sync", "scalar", "vector", "gpsimd", "tensor")

    def __init__(self, nc, stack):
        self.nc, self.stack, self.root = nc, stack, stack
        self.nsem = 0
        self.free_sems = []
        self._init_mem()
        self.eng = {n: Eng(n, self.new_sem("e_" + n)) for n in self.ENGS}
        self.dma_keys = []
        self.nbuf = 0

    def new_sem(self, name=None):
        self.nsem += 1
        return self.root.enter_context(self.nc.semaphore(name or f"s{self.nsem}"))

    ARENA_WORDS = 45056

    def _init_mem(self):
        self.arena = self.root.enter_context(self.nc.sbuf_tensor("arena", [128, self.ARENA_WORDS], F32))
        self.banks = [self.root.enter_context(self.nc.psum_tensor(f"bank{i}", [128, 512], F32)) for i in range(8)]
        self.aoff, self.pidx = 0, 0

    def sbuf(self, name, shape, dt=F32):
        esz = {F32: 4, BF16: 2, U8: 1}[dt]
        nel = int(np.prod(shape[1:]))
        words = (nel * esz + 3) // 4
        assert self.aoff + words <= self.ARENA_WORDS, f"SBUF arena overflow at {name}: {self.aoff}+{words}"
        ap = self.arena[:, self.aoff:self.aoff + words]
        self.aoff += words
        if dt != F32:
            ap = ap.bitcast(dt)[:, 0:nel]
        if len(shape) == 3:
            ap = ap.rearrange("p (a b) -> p a b", b=shape[2])
        if shape[0] < 128:
            ap = ap[0:shape[0]]
        return Buf(name, ap)

    def psum(self, name, shape=(128, 512), dt=F32):
        assert self.pidx < 8, "out of PSUM banks"
        b = Buf(name, self.banks[self.pidx][:], excl=True)
        self.pidx += 1
        return b

    def mark(self):
        return (self.aoff, self.pidx, len(self.dma_keys))

    def reset(self, m):
        self.aoff, self.pidx = m[0], m[1]
        for key in self.dma_keys[m[2]:]:
            if RECYCLE_SEMS:
                self.free_sems.append((key.dma_sem, key.dma_tok.count))
            key.dma_sem, key.dma_tok = None, None
        del self.dma_keys[m[2]:]

    def dram(self, name, shape, dt, kind="Internal"):
        t = self.nc.dram_tensor(name, list(shape), dt, kind=kind)
        return Buf(name, t.ap(), accum=True)

    def _wait(self, E, tok, raw=False):
        if tok.sem is E.sem and (not raw or E.name == "tensor"):
            return
        tok.closed = True
        k = id(tok.sem)
        if E.seen.get(k, 0) >= tok.count:
            return
        E.seen[k] = tok.count
        E.items.append(("wait", tok.sem, tok.count))

    def _hazards(self, E, reads, writes, skip=None):
        deps = []
        for b in reads:
            if b.excl:
                continue
            for t in b.writers.values():
                if t is not skip:
                    self._wait(E, t, raw=True)
                    deps.append(t)
        for b in list(writes) + [b for b in reads if b.excl]:
            if not b.accum:
                for t in b.writers.values():
                    if t is not skip:
                        self._wait(E, t, raw=(b.excl and b in reads))
                        deps.append(t)
            for t in b.readers.values():
                if t is not skip:
                    self._wait(E, t)
                    deps.append(t)
        return deps

    def _record(self, tok, reads, writes):
        k = id(tok.sem)
        for b in reads:
            if b.excl:
                b.writers, b.readers = {k: tok}, {}
            else:
                b.readers[k] = tok
        for b in writes:
            if b.accum:
                b.writers[k] = tok
                b.readers = {}
            else:
                b.writers, b.readers = {k: tok}, {}

    def op(self, eng, fn, reads=(), writes=()):
        E = self.eng[eng]
        self._hazards(E, reads, writes)
        E.count += 1
        tok = Tok(E.sem, E.count)
        rec = _Rec()
        fn(rec)
        E.items.append(("op", rec.call))
        self._record(tok, reads, writes)

    def dma(self, q, out, in_, key, reads=(), writes=(), **kw):
        E = self.eng[q]
        cur = key.dma_tok if (key.dma_tok is not None and not key.dma_tok.closed) else None
        deps = self._hazards(E, reads, writes, skip=cur)
        if cur is not None:
            for t in cur.deps:
                self._wait(E, t)
            cur.deps.extend(deps)
        if key.dma_sem is None:
            if self.free_sems:
                key.dma_sem, cnt = self.free_sems.pop()
                key.dma_tok = Tok(key.dma_sem, cnt)
                key.dma_tok.closed = True
            else:
                key.dma_sem = self.new_sem()
            self.dma_keys.append(key)
        t = key.dma_tok
        if t is None or t.closed:
            if t is not None:
                self._wait(E, t)
            t = Tok(key.dma_sem, t.count if t is not None else 0)
            t.deps = list(deps)
            key.dma_tok = t
        t.count += 16
        E.items.append(("dma", out, in_, key.dma_sem, kw))
        self._record(t, reads, writes)

    def load(self, q, dst, dst_ap, src_ap, src=None, **kw):
        self.dma(q, dst_ap, src_ap, key=dst, reads=([src] if src is not None else []), writes=[dst], **kw)

    def store(self, q, dst, dst_ap, src, src_ap, **kw):
        self.dma(q, dst_ap, src_ap, key=src, reads=[src], writes=[dst], **kw)

    def finish(self):
        E = self.eng["sync"]
        for key in self.dma_keys:
            if key.dma_tok is not None:
                self._wait(E, key.dma_tok)
        for n in self.ENGS:
            if n != "sync" and self.eng[n].count:
                self._wait(E, Tok(self.eng[n].sem, self.eng[n].count))

    def emit(self):
        self.finish()
        with self.nc.Block() as block:
            for n in self.ENGS:
                E = self.eng[n]

                def body(e, E=E):
                    for it in E.items:
                        if it[0] == "wait":
                            e.wait_ge(it[1], it[2])
                        elif it[0] == "op":
                            nm, a, k = it[1]
                            getattr(e, nm)(*a, **k).then_inc(E.sem, 1)
                        else:
                            e.dma_start(out=it[1], in_=it[2], **it[4]).then_inc(it[3], 16)

                getattr(block, n)(body)


def new_prog():
    nc = bass.Bass("TRN2", target_bir_lowering=False)
    stack = contextlib.ExitStack()
    return nc, stack, Sched(nc, stack)


def run_prog(nc, stack, S, in_maps):
    S.emit()
    stack.close()
    res = run_bass_kernel_spmd(nc, in_maps, core_ids=list(range(NCORES)))
    return res.results


def make_ones(S, name="ones", dt=F32):
    ones = S.sbuf(name, (128, 128), dt)
    S.op("gpsimd", lambda e: e.memset(ones[:], 1.0), writes=[ones])
    return ones


def load_const(S, name, shape, dram_ap, dt=F32, q="sync"):
    b = S.sbuf(name, shape, dt)
    S.load(q, b, b[:], dram_ap)
    return b


def mod_scalars(S, cfg, mod, gain, v_scale, v_shift, tag):
    KC = cfg.KC
    A, B = [], []
    for r in range(2):
        a = S.sbuf(f"A_{tag}{r}", (128, KC))
        bb = S.sbuf(f"B_{tag}{r}", (128, KC))
        S.op("vector", lambda e, a=a, r=r: e.tensor_scalar(
            out=a[:], in0=mod[:, v_scale * KC:(v_scale + 1) * KC, r], scalar1=1.0, scalar2=float(cfg.D) ** 0.5,
            op0=ALU.add, op1=ALU.mult), reads=[mod], writes=[a])
        S.op("vector", lambda e, a=a: e.tensor_tensor(out=a[:], in0=a[:], in1=gain[:], op=ALU.mult), reads=[a, gain], writes=[a])
        S.op("vector", lambda e, bb=bb, r=r: e.tensor_copy(out=bb[:], in_=mod[:, v_shift * KC:(v_shift + 1) * KC, r]),
             reads=[mod], writes=[bb])
        A.append(a)
        B.append(bb)
    return A, B


def rms_stats(S, cfg, x, n, sq, ones, ps, rstd, nchunks=None, c0=0):
    nch = nchunks or cfg.KC
    Dn = nch * 128
    for c in range(nch):
        q = sq.get()
        S.op("scalar", lambda e: e.activation(out=q[:, :n], in_=x[:, c0 + c, :n], func=AF.Square), reads=[x], writes=[q])
        S.op("tensor", lambda e: e.matmul(ps[:, :n], ones[:], q[:, :n], start=(c == 0), stop=(c == nch - 1)),
             reads=[ones, q], writes=[ps])
    S.op("vector", lambda e: e.tensor_scalar(out=rstd[:, :n], in0=ps[:, :n], scalar1=NORM_EPS * Dn, scalar2=None,
                                             op0=ALU.add), reads=[ps], writes=[rstd])
    S.op("scalar", lambda e: e.activation(out=rstd[:, :n], in_=rstd[:, :n], func=AF.Sqrt), reads=[rstd], writes=[rstd])
    S.op("vector", lambda e: e.reciprocal(out=rstd[:, :n], in_=rstd[:, :n]), reads=[rstd], writes=[rstd])


def norm_mod_apply(S, cfg, x, n, rstd, A, B, out, tmp):
    for c in range(cfg.KC):
        t = tmp.get()
        S.op("vector", lambda e: e.scalar_tensor_tensor(out=t[:, :n], in0=x[:, c, :n], scalar=A[:, c:c + 1],
                                                        in1=rstd[:, :n], op0=ALU.mult, op1=ALU.mult),
             reads=[x, A, rstd], writes=[t])
        S.op("gpsimd", lambda e: e.tensor_scalar(out=out[:, c, :n], in0=t[:, :n], scalar1=B[:, c:c + 1],
                                                 scalar2=None, op0=ALU.add), reads=[t, B], writes=[out])


def resid_norm_add(S, cfg, x, y, n, rstd, G, tmp):
    for c in range(cfg.KC):
        t = tmp.get()
        S.op("gpsimd", lambda e: e.tensor_tensor(out=t[:, :n], in0=y[:, c, :n], in1=rstd[:, :n], op=ALU.mult),
             reads=[y, rstd], writes=[t])
        S.op("vector", lambda e: e.scalar_tensor_tensor(out=x[:, c, :n], in0=t[:, :n], scalar=G[:, c:c + 1], in1=x[:, c, :n],
                                                        op0=ALU.mult, op1=ALU.add), reads=[t, G, x], writes=[x])


def gate_scalars(S, cfg, mod, gain, v_gate, tag):
    KC = cfg.KC
    G = []
    for r in range(2):
        g = S.sbuf(f"G_{tag}{r}", (128, KC))
        S.op("vector", lambda e: e.tensor_scalar(out=g[:], in0=mod[:, v_gate * KC:(v_gate + 1) * KC, r], scalar1=float(cfg.D) ** 0.5,
                                                 scalar2=None, op0=ALU.mult), reads=[mod], writes=[g])
        S.op("vector", lambda e: e.tensor_tensor(out=g[:], in0=g[:], in1=gain[:], op=ALU.mult), reads=[g, gain], writes=[g])
        G.append(g)
    return G


def token_blocks(cfg, bs=512):
    blocks = []
    for s in range(0, cfg.ntl, bs):
        blocks.append((s, min(bs, cfg.ntl - s), 0))
    blocks.append((cfg.ntl, cfg.ntc, 1))
    return blocks


def build_ada(cfg):
    nc, stack, S = new_prog()
    KC = cfg.KC
    NJ = 6 * KC // NCORES
    c2 = S.dram("c2", (128, KC, 2), F32, "ExternalInput")
    outs = []
    ones = None
    sT = S.sbuf("sT", (128, KC, 2))
    S.load("sync", sT, sT[:], c2[:])
    S.op("scalar", lambda e: e.activation(out=sT[:], in_=sT[:], func=AF.Silu), reads=[sT], writes=[sT])
    ps = [S.psum(f"ps{i}") for i in range(2)]
    for l in range(2):
        w = S.dram(f"w{l}", (128, KC, NJ * 128), F32, "ExternalInput")
        b = S.dram(f"b{l}", (128, NJ), F32, "ExternalInput")
        o = S.dram(f"mod{l}", (128, NJ, 2), F32, "ExternalOutput")
        if l == 0:
            wt_shared = S.sbuf("wt", (128, KC, NJ * 128))
        wt = wt_shared
        bt = S.sbuf(f"bt{l}", (128, NJ))
        ot = S.sbuf(f"ot{l}", (128, NJ, 2))
        for kc in range(KC):
            S.load("sync" if kc % 2 == 0 else "scalar", wt, wt[:, kc, :], w[:, kc, :])
        S.load("sync", bt, bt[:], b[:])
        for j in range(NJ):
            p = ps[j % 2]
            for kc in range(KC):
                S.op("tensor", lambda e, p=p, j=j, kc=kc, wt=wt: e.matmul(p[:, 0:2], wt[:, kc, j * 128:(j + 1) * 128], sT[:, kc, :],
                                                                      start=(kc == 0), stop=(kc == KC - 1)),
                     reads=[wt, sT], writes=[p])
            S.op("vector", lambda e, p=p, j=j, ot=ot, bt=bt: e.tensor_scalar(out=ot[:, j, :], in0=p[:, 0:2], scalar1=bt[:, j:j + 1],
                                                                           scalar2=None, op0=ALU.add),
                 reads=[p, bt], writes=[ot])
        S.store("sync", o, o[:], ot, ot[:])
    return nc, stack, S


def host_ada(cfg, inp):
    KC = cfg.KC
    NJ = 6 * KC // NCORES
    c2 = np.stack([inp["c"][0], inp["c_ctx"]], axis=-1)
    c2 = np.ascontiguousarray(c2.reshape(KC, 128, 2).transpose(1, 0, 2))
    maps = []
    for i in range(NCORES):
        m = {"c2": c2}
        for l in range(2):
            w = inp[f"l{l}_ada_w"][:, i * NJ * 128:(i + 1) * NJ * 128]
            m[f"w{l}"] = np.ascontiguousarray(w.reshape(KC, 128, NJ * 128).transpose(1, 0, 2))
            b = inp[f"l{l}_ada_b"][i * NJ * 128:(i + 1) * NJ * 128]
            m[f"b{l}"] = np.ascontiguousarray(b.reshape(NJ, 128).T)
        maps.append(m)
    return maps


def run_ada(cfg, inp):
    nc, stack, S = build_ada(cfg)
    res = run_prog(nc, stack, S, host_ada(cfg, inp))
    mods = []
    for l in range(2):
        mods.append(np.ascontiguousarray(np.concatenate([res[i][f"mod{l}"] for i in range(NCORES)], axis=1)))
    return mods


def build_pre(cfg):
    nc, stack, S = new_prog()
    KC = cfg.KC
    xT = S.dram("xT", (128, KC, cfg.ntok), F32, "ExternalInput")
    mod_d = S.dram("mod", (128, 6 * KC, 2), F32, "ExternalInput")
    gain_d = S.dram("gain", (128, KC), F32, "ExternalInput")
    hT = S.dram("hT", (128, KC, cfg.ntok), BF16, "ExternalOutput")
    mod = load_const(S, "mod_sb", (128, 6 * KC, 2), mod_d[:])
    gain = load_const(S, "gain_sb", (128, KC), gain_d[:])
    ones = make_ones(S)
    A, B = mod_scalars(S, cfg, mod, gain, 1, 0, "m")
    xb = [S.sbuf(f"xb{i}", (128, KC, 512)) for i in range(2)]
    sq = rot_sbuf(S, "sq", (128, 512))
    tmp = rot_sbuf(S, "tmp", (128, 512))
    hb = [S.sbuf(f"hb{i}", (128, KC, 512), BF16) for i in range(2)]
    rstd = S.sbuf("rstd", (128, 512))
    ps = S.psum("ps")
    for bi, (s0, n, kind) in enumerate(token_blocks(cfg)):
        x = xb[bi % 2]
        h = hb[bi % 2]
        S.load("sync", x, x[:, :, :n], xT[:, :, s0:s0 + n])
        rms_stats(S, cfg, x, n, sq, ones, ps, rstd)
        norm_mod_apply(S, cfg, x, n, rstd, A[kind], B[kind], h, tmp)
        S.store("sync", hT, hT[:, :, s0:s0 + n], h, h[:, :, :n])
    return nc, stack, S


def fm(a):
    D, n = a.shape
    return np.ascontiguousarray(a.reshape(D // 128, 128, n).transpose(1, 0, 2))


def unfm(a):
    p, C, n = a.shape
    return np.ascontiguousarray(a.transpose(1, 0, 2).reshape(C * 128, n))


def vec_fm(v):
    return np.ascontiguousarray(v.reshape(-1, 128).T)


def own_tokens_T(cfg, xT_lat, xT_ctx, i):
    return np.concatenate([xT_lat[:, i * cfg.ntl:(i + 1) * cfg.ntl], xT_ctx[:, i * cfg.ntc:(i + 1) * cfg.ntc]], axis=1)


def gather_tokens_T(cfg, per_core):
    lat = np.concatenate([a[:, :cfg.ntl] for a in per_core], axis=1)
    ctx = np.concatenate([a[:, cfg.ntl:] for a in per_core], axis=1)
    return ctx, lat


def run_pre(cfg, xT_lat, xT_ctx, mod, gain):
    nc, stack, S = build_pre(cfg)
    maps = [{"xT": fm(own_tokens_T(cfg, xT_lat, xT_ctx, i)), "mod": mod, "gain": vec_fm(gain)} for i in range(NCORES)]
    res = run_prog(nc, stack, S, maps)
    if DEBUG:
        global DBG
        DBG = res[0]
    ctx, lat = gather_tokens_T(cfg, [unfm(res[i]["hT"]) for i in range(NCORES)])
    return np.concatenate([ctx, lat], axis=1)


class Rot:
    def __init__(self, bufs):
        self.bufs, self.i = bufs, 0

    def get(self):
        b = self.bufs[self.i % len(self.bufs)]
        self.i += 1
        return b


def rot_sbuf(S, name, shape, dt=F32, n=2):
    return Rot([S.sbuf(f"{name}{i}", shape, dt) for i in range(n)])


def seq_blocks(cfg, bs=512):
    blocks = []
    for seg0, seglen in ((0, cfg.L), (cfg.L, cfg.S)):
        for s in range(0, seglen, bs):
            blocks.append((seg0 + s, min(bs, seglen - s), seg0, seglen))
    return blocks


def rev_pos(t0, n, seg0, seglen):
    return seg0 + seglen - (t0 - seg0) - n


def barrier(S):
    toks = [Tok(S.eng[n].sem, S.eng[n].count) for n in S.ENGS if S.eng[n].count]
    dtoks = [k.dma_tok for k in S.dma_keys if k.dma_tok is not None]
    for n in S.ENGS:
        E = S.eng[n]
        for t in toks + dtoks:
            if t.sem is not E.sem:
                S._wait(E, t)


def make_identity(S, ident_d, name="ident", dt=F32):
    return load_const(S, name, (128, 128), ident_d[:], dt)


def fm_group(S, ps, W, hb, g, n, KC):
    for kc in range(KC):
        S.op("tensor", lambda e, kc=kc: e.matmul(ps[:, :n], W[:, kc, g * 128:(g + 1) * 128], hb[:, kc, :n],
                                               start=(kc == 0), stop=(kc == KC - 1)), reads=[W, hb], writes=[ps])


def load_w_bf16(S, name, w_d, KC, ncols):
    W = S.sbuf(name, (128, KC, ncols), BF16)
    for kc in range(KC):
        S.load("gpsimd", W, W[:, kc, :], w_d[:, kc, :])
    return W


def chunk_scan(S, cfg, units, consts):
    T = cfg.T
    ident, zero1, cmask, mask_ui, m96 = consts["ident"], consts["zero1"], consts["cmask"], consts["mask_ui"], consts["m96"]
    nu = len(units)
    SB = 512
    for u, U in enumerate(units):
        K, V = U["K"], U["V"]
        U["in"] = {nm: rot_sbuf(S, f"u{u}_{nm}", (128, SB)) for nm in ("q", "k", "lw", "v")}
        U["L"] = S.sbuf(f"u{u}_L", (128, SB))
        U["Ep"] = S.sbuf(f"u{u}_Ep", (128, SB))
        U["En"] = S.sbuf(f"u{u}_En", (128, SB))
        U["qh"] = S.sbuf(f"u{u}_qh", (128, SB))
        U["kh"] = S.sbuf(f"u{u}_kh", (128, SB))
        U["AT"] = S.sbuf(f"u{u}_AT", (128, 128))
        U["ktok"] = S.sbuf(f"u{u}_ktok", (128, 128))
        U["vtok"] = S.sbuf(f"u{u}_vtok", (128, 128))
        U["ktokz"] = S.sbuf(f"u{u}_ktokz", (128, 128))
        U["kvd"] = S.sbuf(f"u{u}_kvd", (128, 128))
        U["Z"] = [S.sbuf(f"u{u}_Z{i}", (128, 128)) for i in range(2)]
        U["zi"] = 0
        U["osb"] = rot_sbuf(S, f"u{u}_osb", (128, SB))
        U["ps_g"] = S.psum(f"u{u}_psg")
        U["ps_t"] = S.psum(f"u{u}_pst")
        U["ps_o"] = S.psum(f"u{u}_pso")
        U["ps_kv"] = S.psum(f"u{u}_pskv")
        S.op("gpsimd", lambda e, U=U: e.memset(U["AT"][:], 0.0), writes=[U["AT"]])
        S.op("gpsimd", lambda e, U=U: e.memset(U["Z"][0][:], 0.0), writes=[U["Z"][0]])
    for t0 in range(0, T, SB):
        n = min(SB, T - t0)
        for U in units:
            K, V = U["K"], U["V"]
            cur = {}
            for nm in ("q", "k", "lw", "v"):
                b = U["in"][nm].get()
                P = V if nm == "v" else K
                S.load("sync", b, b[:P, :n], U[nm][:P, t0:t0 + n], src=U[nm])
                cur[nm] = b
            L, Ep, En, qh, kh = U["L"], U["Ep"], U["En"], U["qh"], U["kh"]
            S.op("vector", lambda e, L=L, cur=cur, K=K: e.tensor_tensor_scan(
                out=L[:K, :n], data0=cmask[:K, :n], data1=cur["lw"][:K, :n], initial=zero1[:K, 0:1],
                op0=ALU.mult, op1=ALU.add), reads=[cmask, cur["lw"], zero1], writes=[L])
            S.op("scalar", lambda e, L=L, Ep=Ep, K=K: e.activation(out=Ep[:K, :n], in_=L[:K, :n], func=AF.Exp),
                 reads=[L], writes=[Ep])
            S.op("vector", lambda e, Ep=Ep, En=En, K=K: e.reciprocal(out=En[:K, :n], in_=Ep[:K, :n]), reads=[Ep], writes=[En])
            S.op("gpsimd", lambda e, qh=qh, cur=cur, Ep=Ep, K=K: e.tensor_tensor(out=qh[:K, :n], in0=cur["q"][:K, :n], in1=Ep[:K, :n],
                                                                              op=ALU.mult), reads=[cur["q"], Ep], writes=[qh])
            S.op("gpsimd", lambda e, kh=kh, cur=cur, En=En, K=K: e.tensor_tensor(out=kh[:K, :n], in0=cur["k"][:K, :n], in1=En[:K, :n],
                                                                              op=ALU.mult), reads=[cur["k"], En], writes=[kh])
            U["cur"] = cur
            U["o"] = U["osb"].get()
            if DEBUG and t0 == 0 and U is units[0]:
                for nm, bb in (("L", L), ("Ep", Ep), ("qh", qh), ("kh", kh), ("lwin", cur["lw"]), ("qin", cur["q"])):
                    dd = S.dram("dbg_" + nm, (128, 512), F32, "ExternalOutput")
                    S.store("sync", dd, dd[:, :n], bb, bb[:, :n])
        for j in range(0, n, 128):
            def unit_block(U):
                K, V = U["K"], U["V"]
                qh, kh, Ep, cur = U["qh"], U["kh"], U["Ep"], U["cur"]
                AT, ktok, vtok, kvd, o = U["AT"], U["ktok"], U["vtok"], U["kvd"], U["o"]
                ps_g, ps_t, ps_o, ps_kv = U["ps_g"], U["ps_t"], U["ps_o"], U["ps_kv"]
                js = slice(j, j + 128)
                S.op("tensor", lambda e, K=K, kh=kh, qh=qh, ps_g=ps_g, js=js: e.matmul(ps_g[:, 0:128], kh[:K, js], qh[:K, js], start=True, stop=True),
                     reads=[kh, qh], writes=[ps_g])
                yield
                S.op("vector", lambda e, AT=AT, ps_g=ps_g: e.copy_predicated(out=AT[:], mask=mask_ui[:], data=ps_g[:, 0:128]),
                     reads=[ps_g, mask_ui], writes=[AT])
                S.op("tensor", lambda e, K=K, kh=kh, ps_t=ps_t, js=js: e.matmul(ps_t[:, 0:K], kh[:K, js], ident[:K, :K], is_transpose=True, start=True, stop=True),
                     reads=[kh, ident], writes=[ps_t])
                S.op("tensor", lambda e, V=V, cur=cur, ps_t=ps_t, js=js: e.matmul(ps_t[:, 128:128 + V], cur["v"][:V, js], ident[:V, :V], is_transpose=True, start=True, stop=True),
                     reads=[cur["v"], ident], writes=[ps_t])
                yield
                S.op("scalar", lambda e, K=K, ktok=ktok, ps_t=ps_t: e.activation(out=ktok[:, :K], in_=ps_t[:, 0:K], func=AF.Copy),
                     reads=[ps_t], writes=[ktok])
                S.op("vector", lambda e, V=V, vtok=vtok, ps_t=ps_t: e.tensor_copy(out=vtok[:, :V], in_=ps_t[:, 128:128 + V]),
                     reads=[ps_t], writes=[vtok])
                ktz = U["ktokz"]
                S.op("vector", lambda e, K=K, ktz=ktz, ps_t=ps_t: e.tensor_scalar(out=ktz[64:128, :K], in0=ps_t[64:128, 0:K], scalar1=m96[64:128, 0:1],
                                                                              scalar2=None, op0=ALU.mult), reads=[ps_t, m96], writes=[ktz])
                yield
                S.op("tensor", lambda e, V=V, vtok=vtok, AT=AT, ps_o=ps_o: e.matmul(ps_o[:V, 0:128], vtok[:, :V], AT[:], start=True, stop=False),
                     reads=[vtok, AT], writes=[ps_o])
                yield
            gens = [unit_block(U) for U in units]
            while gens:
                alive = []
                for g in gens:
                    try:
                        next(g)
                        alive.append(g)
                    except StopIteration:
                        pass
                gens = alive
            for c in range(4):
                for U in units:
                    K, V = U["K"], U["V"]
                    qh, Ep = U["qh"], U["Ep"]
                    ktok, vtok, kvd, ps_o, ps_kv = U["ktok"], U["vtok"], U["kvd"], U["ps_o"], U["ps_kv"]
                    Zc, Zn = U["Z"][U["zi"]], U["Z"][1 - U["zi"]]
                    U["zi"] = 1 - U["zi"]
                    cs = slice(j + 32 * c, j + 32 * c + 32)
                    wc = j + 32 * c + 31
                    S.op("tensor", lambda e, K=K, V=V, Zc=Zc, qh=qh, ps_o=ps_o, cs=cs, c=c: e.matmul(
                        ps_o[:V, 32 * c:32 * c + 32], Zc[:K, :V], qh[:K, cs], start=False, stop=(c == 3)),
                        reads=[Zc, qh], writes=[ps_o])
                    if c < 3:
                        S.op("tensor", lambda e, K=K, V=V, ktok=ktok, vtok=vtok, ps_kv=ps_kv, c=c: e.matmul(
                            ps_kv[:K, :V], ktok[32 * c:32 * c + 32, :K], vtok[32 * c:32 * c + 32, :V], start=True, stop=True),
                            reads=[ktok, vtok], writes=[ps_kv])
                    else:
                        ktz = U["ktokz"]
                        S.op("tensor", lambda e, K=K, V=V, ktz=ktz, vtok=vtok, ps_kv=ps_kv: e.matmul(
                            ps_kv[:K, :V], ktz[64:128, :K], vtok[64:128, :V], start=True, stop=True),
                            reads=[ktz, vtok], writes=[ps_kv])
                    S.op("vector", lambda e, K=K, V=V, kvd=kvd, ps_kv=ps_kv, Ep=Ep, wc=wc: e.tensor_scalar(
                        out=kvd[:K, :V], in0=ps_kv[:K, :V], scalar1=Ep[:K, wc:wc + 1], scalar2=None, op0=ALU.mult),
                        reads=[ps_kv, Ep], writes=[kvd])
                    S.op("vector", lambda e, K=K, V=V, Zn=Zn, Zc=Zc, kvd=kvd, Ep=Ep, wc=wc: e.scalar_tensor_tensor(
                        out=Zn[:K, :V], in0=Zc[:K, :V], scalar=Ep[:K, wc:wc + 1], in1=kvd[:K, :V], op0=ALU.mult, op1=ALU.add),
                        reads=[Zc, Ep, kvd], writes=[Zn])
            for U in units:
                V = U["V"]
                o, ps_o = U["o"], U["ps_o"]
                S.op("scalar", lambda e, V=V, o=o, ps_o=ps_o, j=j: e.activation(out=o[:V, j:j + 128], in_=ps_o[:V, 0:128], func=AF.Copy),
                     reads=[ps_o], writes=[o])
        for U in units:
            V = U["V"]
            S.store("sync", U["out"], U["out"][:V, t0:t0 + n], U["o"], U["o"][:V, :n])


def scan_consts(S, cst_d):
    c = {}
    c["ident"] = load_const(S, "ident", (128, 128), cst_d["ident"][:])
    c["cmask"] = load_const(S, "cmask", (128, 512), cst_d["cmask"][:])
    c["mask_ui"] = load_const(S, "mask_ui", (128, 128), cst_d["mask_ui"][:], U8)
    c["m96"] = load_const(S, "m96", (128, 1), cst_d["m96"][:])
    if "mask_su" in cst_d:
        c["mask_su"] = load_const(S, "mask_su", (128, 128), cst_d["mask_su"][:], U8)
        c["mask_sl"] = load_const(S, "mask_sl", (128, 128), cst_d["mask_sl"][:], U8)
    z = S.sbuf("zero1", (128, 1))
    S.op("gpsimd", lambda e: e.memset(z[:], 0.0), writes=[z])
    c["zero1"] = z
    return c


def host_scan_consts(rwkv=False):
    ident = np.eye(128, dtype=np.float32)
    cmask = np.ones((128, 512), np.float32)
    cmask[:, ::32] = 0.0
    s = np.arange(128)[:, None]
    t = np.arange(128)[None, :]
    mask_ui = ((s // 32 == t // 32) & (t >= s)).astype(np.uint8)
    m96 = (np.arange(128) >= 96).astype(np.float32).reshape(128, 1)
    mask_su = ((s // 32 == t // 32) & (t > s)).astype(np.uint8)
    mask_sl = ((s // 32 == t // 32) & (t < s)).astype(np.uint8)
    d = {"ident": ident, "cmask": cmask, "mask_ui": mask_ui, "m96": m96}
    if rwkv:
        d.update({"mask_su": mask_su, "mask_sl": mask_sl})
    return d


def declare_scan_consts(S, rwkv=False):
    d = {"ident": S.dram("ident", (128, 128), F32, "ExternalInput"),
         "cmask": S.dram("cmask", (128, 512), F32, "ExternalInput"),
         "mask_ui": S.dram("mask_ui", (128, 128), U8, "ExternalInput"),
         "m96": S.dram("m96", (128, 1), F32, "ExternalInput")}
    if rwkv:
        d["mask_su"] = S.dram("mask_su", (128, 128), U8, "ExternalInput")
        d["mask_sl"] = S.dram("mask_sl", (128, 128), U8, "ExternalInput")
    return d


def rev_ap(ap):
    return ap[:, ::-1]


NG0 = 10


def build_mix0(cfg):
    nc, stack, S = new_prog()
    KC, T, L, SS = cfg.KC, cfg.T, cfg.L, cfg.S
    hT = S.dram("hT", (128, KC, T), BF16, "ExternalInput")
    w_d = S.dram("w", (128, KC, NG0 * 128), F32, "ExternalInput")
    cosT = S.dram("cosT", (128, SS), F32, "ExternalInput")
    sinT = S.dram("sinT", (128, SS), F32, "ExternalInput")
    lbl_d = S.dram("lbl", (128, 2), F32, "ExternalInput")
    hn_d = S.dram("hnorm", (128, 1), F32, "ExternalInput")
    sink_d = S.dram("sink", (128, 1), F32, "ExternalInput")
    mlo_d = S.dram("mask_lo", (128, 128), BF16, "ExternalInput")
    mup_d = S.dram("mask_up", (128, 128), BF16, "ExternalInput")
    cst_d = declare_scan_consts(S)
    out = S.dram("oT", (2, 128, T), BF16, "ExternalOutput")
    qr = S.dram("qr", (128, T), BF16)
    kr = S.dram("kr", (128, T), BF16)
    vt = S.dram("vt", (T, 128), BF16)
    names = ["q_f", "v_f", "k_f", "lw_f", "q_b", "v_b", "k_b", "lw_b", "gate", "o_f", "o_b"]
    scr = {nm: S.dram("scr_" + nm, (128, T), F32, "ExternalOutput" if DEBUG else "Internal") for nm in names}

    for _ph in (S.mark(),):
        W = load_w_bf16(S, "W", w_d, KC, NG0 * 128)
        lbl = load_const(S, "lbl", (128, 2), lbl_d[:])
        lb = S.sbuf("lb", (128, 1))
        oml = S.sbuf("oml", (128, 1))
        S.op("vector", lambda e: e.tensor_tensor(out=lb[:], in0=lbl[:, 0:1], in1=lbl[:, 1:2], op=ALU.subtract), reads=[lbl], writes=[lb])
        S.op("scalar", lambda e: e.activation(out=lb[:], in_=lb[:], func=AF.Sigmoid), reads=[lb], writes=[lb])
        S.op("vector", lambda e: e.tensor_scalar(out=oml[:], in0=lb[:], scalar1=-1.0, scalar2=1.0, op0=ALU.mult, op1=ALU.add),
             reads=[lb], writes=[oml])
        hbs = rot_sbuf(S, "hb", (128, KC, 512), BF16)
        pss = Rot([S.psum(f"pp{i}") for i in range(6)])
        st = {nm: rot_sbuf(S, "st_" + nm, (128, 512)) for nm in ["q", "v", "kf", "lwf", "kb", "lwb", "g", "qr_", "vr_", "kbr", "lwbr", "t1", "t2", "f"]}
        stb = {nm: rot_sbuf(S, "stb_" + nm, (128, 512), BF16) for nm in ["aq", "ak"]}
        stv = rot_sbuf(S, "stv", (128, 128), BF16, n=4)
        cosb = rot_sbuf(S, "cosb", (128, 512))
        sinb = rot_sbuf(S, "sinb", (128, 512))
        for (t0, n, seg0, seglen) in seq_blocks(cfg):
            rp = rev_pos(t0, n, seg0, seglen)
            lat = seg0 == L
            hb = hbs.get()
            S.load("sync", hb, hb[:, :, :n], hT[:, :, t0:t0 + n])
            if lat:
                cb, sb = cosb.get(), sinb.get()
                S.load("scalar", cb, cb[:, :n], cosT[:, t0 - L:t0 - L + n])
                S.load("scalar", sb, sb[:, :n], sinT[:, t0 - L:t0 - L + n])
            for (g, dst, key) in ((0, qr, "aq"), (2, kr, "ak")):
                p1 = pss.get()
                fm_group(S, p1, W, hb, g, n, KC)
                o = stb[key].get()
                if lat:
                    p2 = pss.get()
                    fm_group(S, p2, W, hb, g + 1, n, KC)
                    t1, t2 = st["t1"].get(), st["t2"].get()
                    S.op("vector", lambda e, t1=t1, p1=p1, cb=cb: e.tensor_tensor(out=t1[:, :n], in0=p1[:, :n], in1=cb[:, :n], op=ALU.mult),
                         reads=[p1, cb], writes=[t1])
                    S.op("vector", lambda e, t2=t2, p2=p2, sb=sb: e.tensor_tensor(out=t2[:, :n], in0=p2[:, :n], in1=sb[:, :n], op=ALU.mult),
                         reads=[p2, sb], writes=[t2])
                    S.op("gpsimd", lambda e, o=o, t1=t1, t2=t2: e.tensor_tensor(out=o[:, :n], in0=t1[:, :n], in1=t2[:, :n], op=ALU.add),
                         reads=[t1, t2], writes=[o])
                else:
                    S.op("scalar", lambda e, o=o, p1=p1: e.activation(out=o[:, :n], in_=p1[:, :n], func=AF.Copy), reads=[p1], writes=[o])
                S.store("sync", dst, dst[:, t0:t0 + n], o, o[:, :n])
            for sb_ in range(0, n, 128):
                p = pss.get()
                for kc in range(KC):
                    S.op("tensor", lambda e, p=p, kc=kc, sb_=sb_, hb=hb: e.matmul(p[:, 0:128], hb[:, kc, sb_:sb_ + 128], W[:, kc, 4 * 128:5 * 128],
                                                                           start=(kc == 0), stop=(kc == KC - 1)), reads=[hb, W], writes=[p])
                o = stv.get()
                S.op("vector", lambda e, o=o, p=p: e.tensor_copy(out=o[:], in_=p[:, 0:128]), reads=[p], writes=[o])
                S.store("sync", vt, vt[t0 + sb_:t0 + sb_ + 128, :], o, o[:])

            def put(nm_f, nm_b, tile, rkey):
                if nm_f is not None:
                    S.store("sync", scr[nm_f], scr[nm_f][:, t0:t0 + n], tile, tile[:, :n])
                if nm_b is not None:
                    r = st[rkey].get()
                    S.op("vector", lambda e, r=r, tile=tile: e.tensor_copy(out=r[:, :n], in_=rev_ap(tile[:, :n])), reads=[tile], writes=[r])
                    S.store("sync", scr[nm_b], scr[nm_b][:, rp:rp + n], r, r[:, :n])

            p = pss.get()
            fm_group(S, p, W, hb, 5, n, KC)
            o = st["q"].get()
            S.op("scalar", lambda e, o=o, p=p: e.activation(out=o[:, :n], in_=p[:, :n], func=AF.Silu), reads=[p], writes=[o])
            put("q_f", "q_b", o, "qr_")
            p = pss.get()
            fm_group(S, p, W, hb, 6, n, KC)
            o = st["v"].get()
            S.op("vector", lambda e, o=o, p=p: e.tensor_copy(out=o[:, :n], in_=p[:, :n]), reads=[p], writes=[o])
            put("v_f", "v_b", o, "vr_")
            for (g, kkey, lkey, fwd) in ((7, "kf", "lwf", True), (8, "kb", "lwb", False)):
                p = pss.get()
                fm_group(S, p, W, hb, g, n, KC)
                f = st["f"].get()
                S.op("scalar", lambda e, f=f, p=p: e.activation(out=f[:, :n], in_=p[:, :n], func=AF.Sigmoid), reads=[p], writes=[f])
                S.op("vector", lambda e, f=f: e.tensor_scalar(out=f[:, :n], in0=f[:, :n], scalar1=oml[:, 0:1], scalar2=lb[:, 0:1],
                                                             op0=ALU.mult, op1=ALU.add), reads=[f, oml, lb], writes=[f])
                lw = st[lkey].get()
                kk = st[kkey].get()
                S.op("scalar", lambda e, lw=lw, f=f: e.activation(out=lw[:, :n], in_=f[:, :n], func=AF.Ln), reads=[f], writes=[lw])
                S.op("vector", lambda e, kk=kk, f=f: e.tensor_scalar(out=kk[:, :n], in0=f[:, :n], scalar1=-1.0, scalar2=1.0,
                                                               op0=ALU.mult, op1=ALU.add), reads=[f], writes=[kk])
                if fwd:
                    put("k_f", None, kk, None)
                    put("lw_f", None, lw, None)
                else:
                    put(None, "k_b", kk, "kbr")
                    put(None, "lw_b", lw, "lwbr")
            p = pss.get()
            fm_group(S, p, W, hb, 9, n, KC)
            o = st["g"].get()
            S.op("scalar", lambda e, o=o, p=p: e.activation(out=o[:, :n], in_=p[:, :n], func=AF.Silu), reads=[p], writes=[o])
            put("gate", None, o, None)
        barrier(S)
        S.reset(_ph)
    for _ph in (S.mark(),):
        consts = scan_consts(S, cst_d)
        units = [dict(K=128, V=128, q=scr["q_f"], k=scr["k_f"], lw=scr["lw_f"], v=scr["v_f"], out=scr["o_f"]),
                 dict(K=128, V=128, q=scr["q_b"], k=scr["k_b"], lw=scr["lw_b"], v=scr["v_b"], out=scr["o_b"])]
        chunk_scan(S, cfg, units, consts)
        barrier(S)
        S.reset(_ph)
    for _ph in (S.mark(),):
        ones = make_ones(S)
        hn = load_const(S, "hn", (128, 1), hn_d[:])
        S.op("vector", lambda e: e.tensor_scalar(out=hn[:], in0=hn[:], scalar1=float(128) ** 0.5, scalar2=None, op0=ALU.mult), reads=[hn], writes=[hn])
        ofb, obb, gb = rot_sbuf(S, "ofb", (128, 512)), rot_sbuf(S, "obb", (128, 512)), rot_sbuf(S, "gb", (128, 512))
        sq = S.sbuf("hsq", (128, 1, 512))
        rstd = S.sbuf("hrstd", (128, 512))
        ps = S.psum("hps")
        res = rot_sbuf(S, "hres", (128, 512), BF16)
        for (t0, n, seg0, seglen) in seq_blocks(cfg):
            rp = rev_pos(t0, n, seg0, seglen)
            of, ob, g = ofb.get(), obb.get(), gb.get()
            S.load("sync", of, of[:, :n], scr["o_f"][:, t0:t0 + n], src=scr["o_f"])
            S.load("scalar", ob, ob[:, :n], scr["o_b"][:, rp:rp + n], src=scr["o_b"])
            S.load("sync", g, g[:, :n], scr["gate"][:, t0:t0 + n], src=scr["gate"])
            S.op("vector", lambda e, of=of, ob=ob: e.tensor_tensor(out=of[:, :n], in0=of[:, :n], in1=rev_ap(ob[:, :n]), op=ALU.add),
                 reads=[of, ob], writes=[of])
            S.op("scalar", lambda e, of=of: e.activation(out=sq[:, 0, :n], in_=of[:, :n], func=AF.Square), reads=[of], writes=[sq])
            S.op("tensor", lambda e: e.matmul(ps[:, :n], ones[:], sq[:, 0, :n], start=True, stop=True), reads=[ones, sq], writes=[ps])
            S.op("vector", lambda e: e.tensor_scalar(out=rstd[:, :n], in0=ps[:, :n], scalar1=NORM_EPS * 128, scalar2=None, op0=ALU.add),
                 reads=[ps], writes=[rstd])
            S.op("scalar", lambda e: e.activation(out=rstd[:, :n], in_=rstd[:, :n], func=AF.Sqrt), reads=[rstd], writes=[rstd])
            S.op("vector", lambda e: e.reciprocal(out=rstd[:, :n], in_=rstd[:, :n]), reads=[rstd], writes=[rstd])
            S.op("vector", lambda e, of=of: e.scalar_tensor_tensor(out=of[:, :n], in0=of[:, :n], scalar=hn[:, 0:1], in1=rstd[:, :n],
                                                                 op0=ALU.mult, op1=ALU.mult), reads=[of, hn, rstd], writes=[of])
            r = res.get()
            S.op("gpsimd", lambda e, r=r, of=of, g=g: e.tensor_tensor(out=r[:, :n], in0=of[:, :n], in1=g[:, :n], op=ALU.mult),
                 reads=[of, g], writes=[r])
            S.store("sync", out, out[1, :, t0:t0 + n], r, r[:, :n])
        attention(S, cfg, qr, kr, vt, sink_d, mlo_d, mup_d, out)
    return nc, stack, S


def attention(S, cfg, qr, kr, vt, sink_d, mlo_d, mup_d, out):
    L, SS, T = cfg.L, cfg.S, cfg.T
    NCB = L // 128
    scale = 128.0 ** -0.5
    ones_b = S.sbuf("ones_b", (128, 128), BF16)
    S.op("gpsimd", lambda e: e.memset(ones_b[:], 1.0), writes=[ones_b])
    mlo = load_const(S, "mlo", (128, 128), mlo_d[:], BF16)
    mup = load_const(S, "mup", (128, 128), mup_d[:], BF16)
    es = load_const(S, "es", (128, 1), sink_d[:])
    S.op("scalar", lambda e: e.activation(out=es[:], in_=es[:], func=AF.Exp), reads=[es], writes=[es])
    kc_sb = S.sbuf("kc_sb", (128, L), BF16)
    S.load("sync", kc_sb, kc_sb[:], kr[:, 0:L], src=kr)
    vc_sb = S.sbuf("vc_sb", (128, NCB, 128), BF16)
    for j in range(NCB):
        S.load("sync", vc_sb, vc_sb[:, j, :], vt[j * 128:(j + 1) * 128, :], src=vt)
    qb = rot_sbuf(S, "aqb", (128, 128), BF16, n=3)
    kb = rot_sbuf(S, "akb", (128, 384), BF16, n=3)
    vb = rot_sbuf(S, "avb", (128, 3, 128), BF16, n=3)
    pT = rot_sbuf(S, "apT", (128, 5, 128), BF16, n=2)
    den = rot_sbuf(S, "aden", (128, 128), F32, n=2)
    ob = rot_sbuf(S, "aob", (128, 128), BF16, n=3)
    ps_s = Rot([S.psum(f"aps_s{i}", (128, 512)) for i in range(2)])
    ps_s2 = Rot([S.psum(f"aps_t{i}", (128, 512)) for i in range(2)])
    ps_o = Rot([S.psum(f"aps_o{i}", (128, 512)) for i in range(2)])
    nqb = SS // 128

    def one_block(q0, ktiles):
        q = qb.get()
        S.load("sync", q, q[:], qr[:, q0:q0 + 128], src=qr)
        p1, p2, po = ps_s.get(), ps_s2.get(), ps_o.get()
        P = pT.get()
        nt = len(ktiles)
        for i, (kbuf, kap, vbuf, vap, mask) in enumerate(ktiles):
            pp = p1 if i < 4 else p2
            col = (i % 4) * 128
            S.op("tensor", lambda e, pp=pp, col=col, kap=kap, q=q: e.matmul(pp[:, col:col + 128], kap, q[:], start=True, stop=True),
                 reads=[kbuf, q], writes=[pp])
        n1 = min(nt, 4)
        S.op("scalar", lambda e, P=P, p1=p1, n1=n1: e.activation(out=P[:, 0:n1, :], in_=p1[:, 0:n1 * 128].rearrange("p (a b) -> p a b", b=128),
                                                              func=AF.Exp, scale=scale),
             reads=[p1], writes=[P])
        if nt > 4:
            S.op("scalar", lambda e, P=P, p2=p2: e.activation(out=P[:, 4, :], in_=p2[:, 0:128], func=AF.Exp, scale=scale),
                 reads=[p2], writes=[P])
        for i, (kbuf, kap, vbuf, vap, mask) in enumerate(ktiles):
            if mask is not None:
                S.op("gpsimd", lambda e, P=P, i=i, mask=mask: e.tensor_tensor(out=P[:, i, :], in0=P[:, i, :], in1=mask[:], op=ALU.mult),
                     reads=[P, mask], writes=[P])
        for i, (kbuf, kap, vbuf, vap, mask) in enumerate(ktiles):
            S.op("tensor", lambda e, po=po, vap=vap, P=P, i=i: e.matmul(po[:, 0:128], vap, P[:, i, :], start=(i == 0), stop=(i == nt - 1)),
                 reads=[vbuf, P], writes=[po])
        for i in range(nt):
            S.op("tensor", lambda e, po=po, P=P, i=i: e.matmul(po[:, 128:256], ones_b[:], P[:, i, :], start=(i == 0), stop=(i == nt - 1),
                                                              skip_group_check=True),
                 reads=[ones_b, P], writes=[po])
        d = den.get()
        S.op("vector", lambda e, d=d, po=po: e.tensor_scalar(out=d[:], in0=po[:, 128:256], scalar1=es[:, 0:1], scalar2=None, op0=ALU.add),
             reads=[po, es], writes=[d])
        S.op("vector", lambda e, d=d: e.reciprocal(out=d[:], in_=d[:]), reads=[d], writes=[d])
        o = ob.get()
        S.op("vector", lambda e, o=o, d=d, po=po: e.tensor_tensor(out=o[:], in0=d[:], in1=po[:, 0:128], op=ALU.mult),
             reads=[d, po], writes=[o])
        S.store("sync", out, out[0, :, q0:q0 + 128], o, o[:])

    ctx_tiles = [(kc_sb, kc_sb[:, j * 128:(j + 1) * 128], vc_sb, vc_sb[:, j, :], None) for j in range(NCB)]
    for b in range(NCB):
        one_block(b * 128, ctx_tiles)
    for b in range(nqb):
        lo = max(b - 1, 0)
        hi = min(b + 1, nqb - 1)
        nk = hi - lo + 1
        k = kb.get()
        v = vb.get()
        S.load("scalar", k, k[:, :nk * 128], kr[:, L + lo * 128:L + (hi + 1) * 128], src=kr)
        for i in range(nk):
            S.load("scalar", v, v[:, i, :], vt[L + (lo + i) * 128:L + (lo + i + 1) * 128, :], src=vt)
        tiles = []
        for i in range(nk):
            kbk = lo + i
            mask = mlo if kbk == b - 1 else (mup if kbk == b + 1 else None)
            tiles.append((k, k[:, i * 128:(i + 1) * 128], v, v[:, i, :], mask))
        one_block(L + b * 128, tiles + ctx_tiles)


def rope_tables(S_len):
    n_freq = 32
    t = np.arange(S_len)
    row = (t // 64).astype(np.float32)
    col = (t % 64).astype(np.float32)
    inv = (np.float32(10000.0) ** (-np.arange(n_freq, dtype=np.float32) / np.float32(n_freq))).astype(np.float32)
    d = np.arange(128)
    axis, half, f = d // 64, (d % 64) // 32, d % 32
    pos = np.where(axis[:, None] == 0, row[None, :], col[None, :]).astype(np.float32)
    ang = pos * inv[f][:, None]
    cosT = np.cos(ang).astype(np.float32)
    sinT = (np.sin(ang) * np.where(half[:, None] == 0, -1.0, 1.0)).astype(np.float32)
    partner = np.where(half == 0, d + 32, d - 32)
    return cosT, sinT, partner


def wfm(w):
    D, n = w.shape
    return np.ascontiguousarray(w.reshape(D // 128, 128, n).transpose(1, 0, 2))


def run_mix0(cfg, hT_all, inp):
    nc, stack, S = build_mix0(cfg)
    cosT, sinT, partner = rope_tables(cfg.S)
    w_in = inp["l0_w_in"]
    hfm = fm(hT_all)
    jj = np.arange(128)[:, None]
    ii = np.arange(128)[None, :]
    mlo = (ii <= jj).astype(ml_dtypes.bfloat16)
    mup = (jj <= ii).astype(ml_dtypes.bfloat16)
    sc = host_scan_consts()
    maps = []
    for c in range(NCORES):
        g = c // 4
        q = w_in[:, c * 128:(c + 1) * 128]
        k = w_in[:, 1024 + g * 128:1024 + (g + 1) * 128]
        v = w_in[:, 1280 + g * 128:1280 + (g + 1) * 128]
        cols = [q, q[:, partner], k, k[:, partner], v]
        for base in (1536, 2560, 3584, 4608, 5632):
            cols.append(w_in[:, base + c * 128:base + (c + 1) * 128])
        m = {"hT": hfm, "w": wfm(np.concatenate(cols, axis=1)), "cosT": cosT, "sinT": sinT,
             "lbl": np.ascontiguousarray(inp["hgrn_lb_logits"][:, c * 128:(c + 1) * 128].T),
             "hnorm": np.ascontiguousarray(inp["l0_hgrn_norm"].reshape(128, 1)),
             "sink": np.full((128, 1), inp["l0_attn_sink"][c], np.float32),
             "mask_lo": mlo, "mask_up": mup}
        m.update(sc)
        maps.append(m)
    res = run_prog(nc, stack, S, maps)
    if DEBUG:
        global DBG
        DBG = res
    att = np.concatenate([res[c]["oT"][0] for c in range(NCORES)], axis=0)
    hg = np.concatenate([res[c]["oT"][1] for c in range(NCORES)], axis=0)
    return np.concatenate([att, hg], axis=0)


CAST_ENGS = ("scalar", "gpsimd", "vector")


def convert_w(S, src, dst, NT, F, stage_f, stage_b, ctr):
    step = 2048
    for t in range(NT):
        for f0 in range(0, F, step):
            fn = min(step, F - f0)
            a, b = stage_f.get(), stage_b.get()
            S.load("sync" if ctr[0] % 2 == 0 else "scalar", a, a[:, :fn], src[t, :, f0:f0 + fn])
            eng = CAST_ENGS[ctr[0] % 3]
            if eng == "scalar":
                S.op("scalar", lambda e: e.activation(out=b[:, :fn], in_=a[:, :fn], func=AF.Copy), reads=[a], writes=[b])
            else:
                S.op(eng, lambda e: e.tensor_copy(out=b[:, :fn], in_=a[:, :fn]), reads=[a], writes=[b])
            S.store("sync" if ctr[0] % 2 == 1 else "scalar", dst, dst[t, :, f0:f0 + fn], b, b[:, :fn])
            ctr[0] += 1


def ffn_block(S, cfg, hb, n, NJ, wg_b, wu_b, wd_b, bufs, evac):
    KC = cfg.KC
    hid = bufs["hid"]
    for j in range(NJ):
        wg, wu = bufs["wg"].get(), bufs["wu"].get()
        S.load("sync", wg, wg[:], wg_b[j], src=wg_b)
        S.load("sync", wu, wu[:], wu_b[j], src=wu_b)
        pg, pu = bufs["pg"].get(), bufs["pu"].get()
        for kc in range(KC):
            S.op("tensor", lambda e: e.matmul(pg[:, :n], wg[:, kc * 128:(kc + 1) * 128], hb[:, kc, :n], start=(kc == 0), stop=(kc == KC - 1)),
                 reads=[wg, hb], writes=[pg])
        for kc in range(KC):
            S.op("tensor", lambda e: e.matmul(pu[:, :n], wu[:, kc * 128:(kc + 1) * 128], hb[:, kc, :n], start=(kc == 0), stop=(kc == KC - 1)),
                 reads=[wu, hb], writes=[pu])
        sg = bufs["sg"].get()
        S.op("scalar", lambda e: e.activation(out=sg[:, :n], in_=pg[:, :n], func=AF.Silu), reads=[pg], writes=[sg])
        S.op("vector", lambda e: e.tensor_tensor(out=hid[:, j, :n], in0=sg[:, :n], in1=pu[:, :n], op=ALU.mult), reads=[sg, pu], writes=[hid])
    for dc in range(KC):
        wd = bufs["wd"].get()
        S.load("sync", wd, wd[:, :NJ * 128], wd_b[dc], src=wd_b)
        po = bufs["po"].get()
        for j in range(NJ):
            S.op("tensor", lambda e: e.matmul(po[:, :n], wd[:, j * 128:(j + 1) * 128], hid[:, j, :n], start=(j == 0), stop=(j == NJ - 1)),
                 reads=[wd, hid], writes=[po])
        evac(dc, po)


def ffn_bufs(S, cfg, NJ, nmax):
    KC = cfg.KC
    return {"hid": S.sbuf("hid", (128, NJ, nmax), BF16),
            "wg": rot_sbuf(S, "wg", (128, KC * 128), BF16, n=3), "wu": rot_sbuf(S, "wu", (128, KC * 128), BF16, n=3),
            "wd": rot_sbuf(S, "wd", (128, NJ * 128), BF16, n=2),
            "sg": rot_sbuf(S, "sg", (128, nmax), F32, n=2),
            "pg": Rot([S.psum("pg0"), S.psum("pg1")]), "pu": Rot([S.psum("pu0"), S.psum("pu1")]),
            "po": Rot([S.psum("po0"), S.psum("po1")])}


def host_ffn_w(wg, wu, wd):
    D, FF = wg.shape
    KC, NJ = D // 128, FF // 128

    def gu(w):
        return np.ascontiguousarray(w.reshape(KC, 128, NJ, 128).transpose(2, 1, 0, 3).reshape(NJ, 128, KC * 128))
    wdl = np.ascontiguousarray(wd.reshape(NJ, 128, KC, 128).transpose(2, 1, 0, 3).reshape(KC, 128, NJ * 128))
    return gu(wg), gu(wu), wdl


def build_l3(cfg):
    nc, stack, S = new_prog()
    KC, NB = cfg.KC, 256
    NJ = cfg.D_FF // 128
    xT = S.dram("xT", (128, KC, cfg.ntok), F32, "ExternalInput")
    oT = S.dram("oT", (128, KC, cfg.ntok), BF16, "ExternalInput")
    mod_d = [S.dram(f"mod{l}", (128, 6 * KC, 2), F32, "ExternalInput") for l in range(2)]
    gains_d = S.dram("gains", (128, 4, KC), F32, "ExternalInput")
    wo_d = S.dram("wo", (KC, 128, KC * 128), F32, "ExternalInput")
    wg_d = S.dram("wg", (NJ, 128, KC * 128), F32, "ExternalInput")
    wu_d = S.dram("wu", (NJ, 128, KC * 128), F32, "ExternalInput")
    wd_d = S.dram("wd", (KC, 128, NJ * 128), F32, "ExternalInput")
    x2T = S.dram("x2T", (128, KC, cfg.ntok), F32, "ExternalOutput")
    hT1 = S.dram("hT1", (128, KC, cfg.ntok), BF16, "ExternalOutput")
    wo_b = S.dram("wo_b", (KC, 128, KC * 128), BF16)
    wg_b = S.dram("wg_b", (NJ, 128, KC * 128), BF16)
    wu_b = S.dram("wu_b", (NJ, 128, KC * 128), BF16)
    wd_b = S.dram("wd_b", (KC, 128, NJ * 128), BF16)
    for _ph in (S.mark(),):
        sf, sb = rot_sbuf(S, "cv_f", (128, 2048), F32, n=3), rot_sbuf(S, "cv_b", (128, 2048), BF16, n=3)
        ctr = [0]
        convert_w(S, wo_d, wo_b, KC, KC * 128, sf, sb, ctr)
        convert_w(S, wg_d, wg_b, NJ, KC * 128, sf, sb, ctr)
        convert_w(S, wu_d, wu_b, NJ, KC * 128, sf, sb, ctr)
        convert_w(S, wd_d, wd_b, KC, NJ * 128, sf, sb, ctr)
        barrier(S)
        S.reset(_ph)
    mod = [load_const(S, f"mod_sb{l}", (128, 6 * KC, 2), mod_d[l][:]) for l in range(2)]
    gains = load_const(S, "gains_sb", (128, 4, KC), gains_d[:])
    gn = [Buf(f"gain{i}", gains[:, i, :]) for i in range(4)]
    for g in gn:
        g.writers = gains.writers
    ones = make_ones(S)
    G1 = gate_scalars(S, cfg, mod[0], gn[0], 2, "g1")
    A2, B2 = mod_scalars(S, cfg, mod[0], gn[1], 4, 3, "m2")
    G2 = gate_scalars(S, cfg, mod[0], gn[2], 5, "g2")
    A3, B3 = mod_scalars(S, cfg, mod[1], gn[3], 1, 0, "m3")
    xb = S.sbuf("xb", (128, KC, NB))
    ob = S.sbuf("ob", (128, KC, NB), BF16)
    yb = S.sbuf("yb", (128, KC, NB))
    hb = S.sbuf("hb", (128, KC, NB), BF16)
    sq = rot_sbuf(S, "sq", (128, NB))
    tmp = rot_sbuf(S, "tmp", (128, NB))
    rstd = S.sbuf("rstd", (128, NB))
    wo = rot_sbuf(S, "wo", (128, KC * 128), BF16, n=3)
    ps_s = S.psum("ps_stat")
    ps_y = Rot([S.psum("ps_y0")])
    fb = ffn_bufs(S, cfg, NJ, NB)
    ps_y = fb["po"]
    for (s0, n, kind) in token_blocks(cfg, NB):
        S.load("sync", xb, xb[:, :, :n], xT[:, :, s0:s0 + n])
        S.load("scalar", ob, ob[:, :, :n], oT[:, :, s0:s0 + n])
        for dc in range(KC):
            w = wo.get()
            S.load("sync", w, w[:], wo_b[dc], src=wo_b)
            p = ps_y.get()
            for kc in range(KC):
                S.op("tensor", lambda e: e.matmul(p[:, :n], w[:, kc * 128:(kc + 1) * 128], ob[:, kc, :n], start=(kc == 0), stop=(kc == KC - 1)),
                     reads=[w, ob], writes=[p])
            S.op("scalar", lambda e: e.activation(out=yb[:, dc, :n], in_=p[:, :n], func=AF.Copy), reads=[p], writes=[yb])
        rms_stats(S, cfg, yb, n, sq, ones, ps_s, rstd)
        resid_norm_add(S, cfg, xb, yb, n, rstd, G1[kind], tmp)
        rms_stats(S, cfg, xb, n, sq, ones, ps_s, rstd)
        norm_mod_apply(S, cfg, xb, n, rstd, A2[kind], B2[kind], hb, tmp)

        def evac(dc, po):
            S.op("scalar", lambda e: e.activation(out=yb[:, dc, :n], in_=po[:, :n], func=AF.Copy), reads=[po], writes=[yb])
        ffn_block(S, cfg, hb, n, NJ, wg_b, wu_b, wd_b, fb, evac)
        rms_stats(S, cfg, yb, n, sq, ones, ps_s, rstd)
        resid_norm_add(S, cfg, xb, yb, n, rstd, G2[kind], tmp)
        S.store("gpsimd", x2T, x2T[:, :, s0:s0 + n], xb, xb[:, :, :n])
        rms_stats(S, cfg, xb, n, sq, ones, ps_s, rstd)
        norm_mod_apply(S, cfg, xb, n, rstd, A3[kind], B3[kind], hb, tmp)
        S.store("gpsimd", hT1, hT1[:, :, s0:s0 + n], hb, hb[:, :, :n])
    return nc, stack, S


def run_l3(cfg, xT_lat, xT_ctx, oT_all, mods, inp):
    nc, stack, S = build_l3(cfg)
    L = cfg.L
    wo = inp["l0_w_out"]
    KC = cfg.KC
    wo_l = np.ascontiguousarray(wo.reshape(KC, 128, KC, 128).transpose(2, 1, 0, 3).reshape(KC, 128, KC * 128))
    wg, wu, wd = host_ffn_w(inp["l0_ffn_w_gate"], inp["l0_ffn_w_up"], inp["l0_ffn_w_down"])
    gains = np.ascontiguousarray(np.stack([vec_fm(inp[k]) for k in ("l0_norm_mix_post", "l0_norm_ffn_pre", "l0_norm_ffn_post", "l1_norm_mix_pre")], axis=1))
    maps = []
    for i in range(NCORES):
        maps.append({"xT": fm(own_tokens_T(cfg, xT_lat, xT_ctx, i)), "oT": fm(own_tokens_T(cfg, oT_all[:, L:], oT_all[:, :L], i)),
                     "mod0": mods[0], "mod1": mods[1], "gains": gains, "wo": wo_l, "wg": wg, "wu": wu, "wd": wd})
    res = run_prog(nc, stack, S, maps)
    x2c, x2l = gather_tokens_T(cfg, [unfm(res[i]["x2T"]) for i in range(NCORES)])
    hc, hl = gather_tokens_T(cfg, [unfm(res[i]["hT1"]) for i in range(NCORES)])
    return x2c, x2l, np.concatenate([hc, hl], axis=1)


def rwkv_scan(S, cfg, units, consts, SB=256):
    T = cfg.T
    ident, zero1, cmask, m96 = consts["ident"], consts["zero1"], consts["cmask"], consts["m96"]
    m_ui, m_su, m_sl = consts["mask_ui"], consts["mask_su"], consts["mask_sl"]
    K = V = 64
    names = ("q", "k", "v", "lw", "a", "b")
    for u, U in enumerate(units):
        U["in"] = {nm: rot_sbuf(S, f"r{u}_{nm}", (128, SB)) for nm in names}
        for nm in ("L", "Ep", "En", "Eex", "qh", "kh", "ah", "bh"):
            U[nm] = S.sbuf(f"r{u}_{nm}", (128, SB))
        for nm in ("NT", "Nn", "MrbT", "MakT", "MrkT", "Xa", "Xb", "PTa", "Pa", "PTb", "Pb"):
            U[nm] = S.sbuf(f"r{u}_{nm}", (128, 128))
        for nm in ("btok", "ktok", "vtok", "Apz", "bz", "kz", "Rp"):
            U[nm] = S.sbuf(f"r{u}_{nm}", (128, 128 if nm == "Rp" else 64))
        for nm in ("PTc", "Qd"):
            U[nm] = S.sbuf(f"r{u}_{nm}", (128, 64))
        U["Z"] = [S.sbuf(f"r{u}_Z{i}", (128, 64)) for i in range(2)]
        U["zi"] = 0
        U["osb"] = rot_sbuf(S, f"r{u}_osb", (128, SB))
        bA, bB = S.psum(f"r{u}_psA"), S.psum(f"r{u}_psB")
        U["ps"] = [bA, bB, bA, bB]
        for nm in ("NT", "Nn", "MrbT", "MakT", "MrkT", "Apz", "bz", "kz"):
            S.op("gpsimd", lambda e: e.memset(U[nm][:], 0.0), writes=[U[nm]])
        S.op("gpsimd", lambda e: e.memset(U["Z"][0][:], 0.0), writes=[U["Z"][0]])
    for t0 in range(0, T, SB):
        n = min(SB, T - t0)
        for U in units:
            cur = {}
            for i, nm in enumerate(names):
                b = U["in"][nm].get()
                S.load("sync" if i % 2 == 0 else "scalar", b, b[:K, :n], U[nm][0:K, t0:t0 + n], src=U["src_" + nm])
                cur[nm] = b
            L, Ep, En, Eex, qh, kh, ah, bh = (U[x] for x in ("L", "Ep", "En", "Eex", "qh", "kh", "ah", "bh"))
            S.op("vector", lambda e: e.tensor_tensor_scan(out=L[:K, :n], data0=cmask[:K, :n], data1=cur["lw"][:K, :n], initial=zero1[:K, 0:1],
                                                          op0=ALU.mult, op1=ALU.add), reads=[cmask, cur["lw"], zero1], writes=[L])
            S.op("scalar", lambda e: e.activation(out=Ep[:K, :n], in_=L[:K, :n], func=AF.Exp), reads=[L], writes=[Ep])
            S.op("vector", lambda e: e.reciprocal(out=En[:K, :n], in_=Ep[:K, :n]), reads=[Ep], writes=[En])
            S.op("gpsimd", lambda e: e.tensor_tensor(out=Eex[:K, :n], in0=L[:K, :n], in1=cur["lw"][:K, :n], op=ALU.subtract),
                 reads=[L, cur["lw"]], writes=[Eex])
            S.op("scalar", lambda e: e.activation(out=Eex[:K, :n], in_=Eex[:K, :n], func=AF.Exp), reads=[Eex], writes=[Eex])
            S.op("gpsimd", lambda e: e.tensor_tensor(out=qh[:K, :n], in0=cur["q"][:K, :n], in1=Ep[:K, :n], op=ALU.mult), reads=[cur["q"], Ep], writes=[qh])
            S.op("gpsimd", lambda e: e.tensor_tensor(out=kh[:K, :n], in0=cur["k"][:K, :n], in1=En[:K, :n], op=ALU.mult), reads=[cur["k"], En], writes=[kh])
            S.op("vector", lambda e: e.tensor_tensor(out=ah[:K, :n], in0=cur["a"][:K, :n], in1=Eex[:K, :n], op=ALU.mult), reads=[cur["a"], Eex], writes=[ah])
            S.op("gpsimd", lambda e: e.tensor_tensor(out=bh[:K, :n], in0=cur["b"][:K, :n], in1=En[:K, :n], op=ALU.mult), reads=[cur["b"], En], writes=[bh])
            U["cur"] = cur
            U["o"] = U["osb"].get()
        for j in range(0, n, 128):
            js = slice(j, j + 128)

            def unit_block(U):
                cur = U["cur"]
                qh, kh, ah, bh = U["qh"], U["kh"], U["ah"], U["bh"]
                b0, b1, b2, b3 = U["ps"]
                NT, Nn, MrbT, MakT, MrkT = U["NT"], U["Nn"], U["MrbT"], U["MakT"], U["MrkT"]
                btok, ktok, vtok, Apz, bz, kz, Rp = U["btok"], U["ktok"], U["vtok"], U["Apz"], U["bz"], U["kz"], U["Rp"]

                def mm(out, lhsT, rhs, rd, wr, start=True, stop=True, tr=False):
                    if tr:
                        S.op("tensor", lambda e: e.matmul(out, lhsT, rhs, is_transpose=True, start=True, stop=True), reads=rd, writes=[wr])
                    else:
                        S.op("tensor", lambda e: e.matmul(out, lhsT, rhs, start=start, stop=stop), reads=rd, writes=[wr])
                mm(b0[:, 0:128], bh[:K, js], ah[:K, js], [bh, ah], b0)
                mm(b0[:, 128:256], bh[:K, js], qh[:K, js], [bh, qh], b0)
                mm(b0[:, 256:384], kh[:K, js], ah[:K, js], [kh, ah], b0)
                mm(b0[:, 384:512], kh[:K, js], qh[:K, js], [kh, qh], b0)
                mm(b1[:, 0:128], ah[:K, js], bh[:K, js], [ah, bh], b1)
                yield
                for (dst, src, msk) in ((NT, b0[:, 0:128], m_su), (MrbT, b0[:, 128:256], m_ui), (MakT, b0[:, 256:384], m_su),
                                        (MrkT, b0[:, 384:512], m_ui)):
                    S.op("vector", lambda e: e.copy_predicated(out=dst[:], mask=msk[:], data=src), reads=[b0, msk], writes=[dst])
                S.op("vector", lambda e: e.copy_predicated(out=Nn[:], mask=m_sl[:], data=b1[:, 0:128]), reads=[b1, m_sl], writes=[Nn])
                yield
                mm(b1[:, 128:192], ah[:K, js], ident[:K, :K], [ah, ident], b1, tr=True)
                mm(b1[:, 192:256], bh[:K, js], ident[:K, :K], [bh, ident], b1, tr=True)
                mm(b1[:, 256:320], kh[:K, js], ident[:K, :K], [kh, ident], b1, tr=True)
                mm(b1[:, 320:384], cur["v"][:V, js], ident[:V, :V], [cur["v"], ident], b1, tr=True)
                yield
                X = U["Xa"]
                S.op("scalar", lambda e: e.activation(out=X[:, 0:K], in_=b1[:, 128:192], func=AF.Copy), reads=[b1], writes=[X])
                S.op("vector", lambda e: e.tensor_copy(out=btok[:, :K], in_=b1[:, 192:256]), reads=[b1], writes=[btok])
                S.op("scalar", lambda e: e.activation(out=ktok[:, :K], in_=b1[:, 256:320], func=AF.Copy), reads=[b1], writes=[ktok])
                S.op("vector", lambda e: e.tensor_copy(out=vtok[:, :V], in_=b1[:, 320:384]), reads=[b1], writes=[vtok])
                S.op("vector", lambda e: e.tensor_scalar(out=bz[64:128, :K], in0=b1[64:128, 192:256], scalar1=m96[64:128, 0:1], scalar2=None,
                                                         op0=ALU.mult), reads=[b1, m96], writes=[bz])
                S.op("vector", lambda e: e.tensor_scalar(out=kz[64:128, :K], in0=b1[64:128, 256:320], scalar1=m96[64:128, 0:1], scalar2=None,
                                                         op0=ALU.mult), reads=[b1, m96], writes=[kz])
                yield
                mm(b1[:, 384:448], MakT[:], vtok[:, :V], [MakT, vtok], b1)
                yield
                S.op("scalar", lambda e: e.activation(out=X[:, K:K + V], in_=b1[:, 384:448], func=AF.Copy), reads=[b1], writes=[X])
                yield
                PT, P = NT, Nn
                spare = [(U["PTa"], U["Pa"]), (U["PTb"], U["Pb"])]
                for i in range(5):
                    Xn = U["Xb"] if X is U["Xa"] else U["Xa"]
                    mm(b2[:, 0:128], PT[:], X[:], [PT, X], b2)
                    if i < 4:
                        PTn, Pn = spare[i % 2]
                        mm(b2[:, 128:256], P[:], PT[:], [P, PT], b2)
                        if i < 3:
                            mm(b2[:, 256:384], PT[:], P[:], [PT, P], b2)
                    yield
                    S.op("vector", lambda e: e.tensor_tensor(out=Xn[:], in0=X[:], in1=b2[:, 0:128], op=ALU.add), reads=[X, b2], writes=[Xn])
                    if i < 4:
                        S.op("scalar", lambda e: e.activation(out=PTn[:], in_=b2[:, 128:256], func=AF.Copy), reads=[b2], writes=[PTn])
                        if i < 3:
                            S.op("scalar", lambda e: e.activation(out=Pn[:], in_=b2[:, 256:384], func=AF.Copy), reads=[b2], writes=[Pn])
                        PT, P = PTn, Pn
                    X = Xn
                    yield
                yield
                U["X5"] = X
                S.op("gpsimd", lambda e: e.tensor_scalar(out=Apz[64:128, :K], in0=X[64:128, 0:K], scalar1=m96[64:128, 0:1], scalar2=None,
                                                         op0=ALU.mult), reads=[X, m96], writes=[Apz])
                yield
                mm(b3[:K, 0:128], X[:, 0:K], MrbT[:], [X, MrbT], b3)
                yield
                S.op("vector", lambda e: e.tensor_tensor(out=Rp[:K, :], in0=qh[:K, js], in1=b3[:K, 0:128], op=ALU.add), reads=[qh, b3], writes=[Rp])
                mm(b3[:V, 128:256], X[:, K:K + V], MrbT[:], [X, MrbT], b3, start=True, stop=False)
                mm(b3[:V, 128:256], vtok[:, :V], MrkT[:], [vtok, MrkT], b3, start=False, stop=False)
                yield
            gens = [unit_block(U) for U in units]
            while gens:
                alive = []
                for g in gens:
                    try:
                        next(g)
                        alive.append(g)
                    except StopIteration:
                        pass
                gens = alive
            for c in range(4):
                for U in units:
                    b0, b1, b2, b3 = U["ps"]
                    X, Rp, btok, ktok, vtok, Apz, bz, kz = U["X5"], U["Rp"], U["btok"], U["ktok"], U["vtok"], U["Apz"], U["bz"], U["kz"]
                    PTc, Qd, Ep = U["PTc"], U["Qd"], U["Ep"]
                    Zc, Zn = U["Z"][U["zi"]], U["Z"][1 - U["zi"]]
                    U["zi"] = 1 - U["zi"]
                    wc = j + 32 * c + 31
                    rs = slice(32 * c, 32 * c + 32)
                    hi = slice(64, 128)
                    S.op("tensor", lambda e: e.matmul(b3[:V, 128 + 32 * c:128 + 32 * c + 32], Zc[:K, :V], Rp[:K, 32 * c:32 * c + 32],
                                                      start=False, stop=(c == 3)), reads=[Zc, Rp], writes=[b3])
                    if c < 3:
                        S.op("tensor", lambda e: e.matmul(b2[:K, 256:256 + K], X[rs, 0:K], btok[rs, :K], start=True, stop=True),
                             reads=[X, btok], writes=[b2])
                    else:
                        S.op("tensor", lambda e: e.matmul(b2[:K, 256:256 + K], Apz[hi, :K], btok[hi, :K], start=True, stop=True),
                             reads=[Apz, btok], writes=[b2])
                    S.op("vector", lambda e: e.tensor_tensor(out=PTc[:K, :K], in0=b2[:K, 256:256 + K], in1=ident[:K, :K], op=ALU.add),
                         reads=[b2, ident], writes=[PTc])
                    if c < 3:
                        S.op("tensor", lambda e: e.matmul(b2[:K, 320:320 + V], btok[rs, :K], X[rs, K:K + V], start=True, stop=False),
                             reads=[btok, X], writes=[b2])
                        S.op("tensor", lambda e: e.matmul(b2[:K, 320:320 + V], ktok[rs, :K], vtok[rs, :V], start=False, stop=True),
                             reads=[ktok, vtok], writes=[b2])
                    else:
                        S.op("tensor", lambda e: e.matmul(b2[:K, 320:320 + V], bz[hi, :K], X[hi, K:K + V], start=True, stop=False),
                             reads=[bz, X], writes=[b2])
                        S.op("tensor", lambda e: e.matmul(b2[:K, 320:320 + V], kz[hi, :K], vtok[hi, :V], start=False, stop=True),
                             reads=[kz, vtok], writes=[b2])
                    S.op("vector", lambda e: e.tensor_scalar(out=Qd[:K, :V], in0=b2[:K, 320:320 + V], scalar1=Ep[:K, wc:wc + 1], scalar2=None,
                                                             op0=ALU.mult), reads=[b2, Ep], writes=[Qd])
                    S.op("tensor", lambda e: e.matmul(b2[:K, 384:384 + V], PTc[:K, :K], Zc[:K, :V], start=True, stop=True),
                         reads=[PTc, Zc], writes=[b2])
                    S.op("vector", lambda e: e.scalar_tensor_tensor(out=Zn[:K, :V], in0=b2[:K, 384:384 + V], scalar=Ep[:K, wc:wc + 1], in1=Qd[:K, :V],
                                                                    op0=ALU.mult, op1=ALU.add), reads=[b2, Ep, Qd], writes=[Zn])
            for U in units:
                o, b3 = U["o"], U["ps"][3]
                S.op("scalar", lambda e: e.activation(out=o[:V, j:j + 128], in_=b3[:V, 128:256], func=AF.Copy), reads=[b3], writes=[o])
        for U in units:
            S.store("sync", U["src_out"], U["out"][0:V, t0:t0 + n], U["o"], U["o"][:V, :n])


NG1 = 13
RW_LN_EPS = 64e-5


def build_mix1(cfg):
    nc, stack, S = new_prog()
    KC, T, L, SS = cfg.KC, cfg.T, cfg.L, cfg.S
    PB = 256
    hT = S.dram("hT", (128, KC, T), BF16, "ExternalInput")
    w_d = S.dram("w", (128, KC, NG1 * 128), F32, "ExternalInput")
    sm_d = S.dram("smalls", (128, 40), F32, "ExternalInput")
    lr_d = S.dram("lowrank", (8, 128, 128), F32, "ExternalInput")
    cst_d = declare_scan_consts(S, rwkv=True)
    out = S.dram("oT", (3, 128, SS), BF16, "ExternalOutput")
    gnames = ["q_f", "k_f", "v_f", "lw_f", "q_b", "k_b", "v_b", "lw_b", "o_f", "o_b"]
    gs = {nm: S.dram("g_" + nm, (128, T), F32, "ExternalOutput" if DEBUG else "Internal") for nm in gnames}
    rnames = ["r_f", "k_f", "v_f", "a_f", "b_f", "lw_f", "r_b", "k_b", "v_b", "a_b", "b_b", "lw_b", "o_f", "o_b", "gout", "bonus"]
    rs = {nm: S.dram("r_" + nm, (128, T), F32, "ExternalOutput" if DEBUG else "Internal") for nm in rnames}

    for _ph in (S.mark(),):
        W = load_w_bf16(S, "W", w_d, KC, NG1 * 128)
        sm = load_const(S, "sm", (128, 40), sm_d[:])
        lr = S.sbuf("lr", (128, 8, 128))
        for i in range(8):
            S.load("scalar", lr, lr[:, i, :], lr_d[i])
        UPF, UPB, W2F, W2B, A2, G20, G21, BD = range(8)
        S.op("vector", lambda e: e.tensor_scalar(out=sm[:, 24:25], in0=sm[:, 6:7], scalar1=-1.0, scalar2=1.0, op0=ALU.mult, op1=ALU.add),
             reads=[sm], writes=[sm])
        S.op("vector", lambda e: e.tensor_tensor(out=sm[:, 25:33], in0=sm[:, 8:16], in1=sm[:, 16:24], op=ALU.add), reads=[sm], writes=[sm])
        S.op("vector", lambda e: e.tensor_scalar(out=sm[:, 25:33], in0=sm[:, 25:33], scalar1=-1.0, scalar2=1.0, op0=ALU.mult, op1=ALU.add),
             reads=[sm], writes=[sm])
        hbs = rot_sbuf(S, "hb", (128, KC, PB + 2), BF16)
        pss = Rot([S.psum(f"pp{i}") for i in range(8)])
        NT_ = 40
        pool = rot_sbuf(S, "tp", (128, PB + 2), F32, n=NT_)
        revp = rot_sbuf(S, "rv", (128, PB), F32, n=8)
        gob = rot_sbuf(S, "gob", (128, PB), BF16, n=2)

        for (t0, n, seg0, seglen) in seq_blocks(cfg, PB):
            rp = rev_pos(t0, n, seg0, seglen)
            lat = seg0 == L
            lo, hi = max(t0 - 1, 0), min(t0 + n + 1, T)
            hb = hbs.get()
            S.load("sync", hb, hb[:, :, lo - (t0 - 1):hi - (t0 - 1)], hT[:, :, lo:hi])
            ne = n + 2

            def fmg(g, c0_, nn):
                p = pss.get()
                for kc in range(KC):
                    S.op("tensor", lambda e: e.matmul(p[:, :nn], W[:, kc, g * 128:(g + 1) * 128], hb[:, kc, c0_:c0_ + nn],
                                                      start=(kc == 0), stop=(kc == KC - 1)), reads=[W, hb], writes=[p])
                return p

            def put(dst_f, dst_b, tile):
                if dst_f is not None:
                    S.store("sync", dst_f, dst_f[:, t0:t0 + n], tile, tile[:, :n])
                if dst_b is not None:
                    r = revp.get()
                    S.op("vector", lambda e: e.tensor_copy(out=r[:, :n], in_=rev_ap(tile[:, :n])), reads=[tile], writes=[r])
                    S.store("scalar", dst_b, dst_b[:, rp:rp + n], r, r[:, :n])

            p = fmg(0, 1, n)
            o = pool.get()
            S.op("vector", lambda e: e.tensor_scalar(out=o[:, :n], in0=p[:, :n], scalar1=128.0 ** -0.5, scalar2=None, op0=ALU.mult), reads=[p], writes=[o])
            put(gs["q_f"], gs["q_b"], o)
            for g, nm in ((1, "k"), (2, "v")):
                p = fmg(g, 1, n)
                o = pool.get()
                S.op("scalar", lambda e: e.activation(out=o[:, :n], in_=p[:, :n], func=AF.Copy), reads=[p], writes=[o])
                put(gs[nm + "_f"], gs[nm + "_b"], o)
            p = fmg(3, 1, n)
            gd = pool.get()
            S.op("vector", lambda e: e.tensor_copy(out=gd[:, :n], in_=p[:, :n]), reads=[p], writes=[gd])
            for (ui_, bcol, fwd) in ((UPF, 0, True), (UPB, 1, False)):
                p = pss.get()
                S.op("tensor", lambda e: e.matmul(p[:, :n], lr[:, ui_, :], gd[:, :n], start=True, stop=True), reads=[lr, gd], writes=[p])
                o = pool.get()
                S.op("scalar", lambda e: e.activation(out=o[:, :n], in_=p[:, :n], func=AF.Sigmoid, bias=sm[:, bcol:bcol + 1]), reads=[p, sm], writes=[o])
                S.op("scalar", lambda e: e.activation(out=o[:, :n], in_=o[:, :n], func=AF.Ln), reads=[o], writes=[o])
                S.op("vector", lambda e: e.tensor_scalar(out=o[:, :n], in0=o[:, :n], scalar1=1.0 / 16.0, scalar2=None, op0=ALU.mult), reads=[o], writes=[o])
                if fwd:
                    put(gs["lw_f"], None, o)
                else:
                    put(None, gs["lw_b"], o)
            if lat:
                p = fmg(4, 1, n)
                ob = gob.get()
                S.op("scalar", lambda e: e.activation(out=ob[:, :n], in_=p[:, :n], func=AF.Silu), reads=[p], writes=[ob])
                S.store("sync", out, out[1, :, t0 - L:t0 - L + n], ob, ob[:, :n])
            sh = {}
            for gi, g in enumerate(range(5, 13)):
                p = fmg(g, 0, ne)
                pe = pool.get()
                S.op("scalar", lambda e: e.activation(out=pe[:, :ne], in_=p[:, :ne], func=AF.Copy), reads=[p], writes=[pe])
                if t0 == seg0:
                    S.op("gpsimd", lambda e: e.memset(pe[:, 0:1], 0.0), reads=[pe], writes=[pe])
                if t0 + n == seg0 + seglen:
                    S.op("gpsimd", lambda e: e.memset(pe[:, n + 1:n + 2], 0.0), reads=[pe], writes=[pe])
                o = pool.get()
                S.op("vector", lambda e: e.tensor_scalar(out=o[:, :n], in0=pe[:, 1:n + 1], scalar1=sm[:, 25 + gi:26 + gi], scalar2=None, op0=ALU.mult),
                     reads=[pe, sm], writes=[o])
                S.op("vector", lambda e: e.scalar_tensor_tensor(out=o[:, :n], in0=pe[:, 0:n], scalar=sm[:, 8 + gi:9 + gi], in1=o[:, :n],
                                                                op0=ALU.mult, op1=ALU.add), reads=[pe, sm, o], writes=[o])
                S.op("vector", lambda e: e.scalar_tensor_tensor(out=o[:, :n], in0=pe[:, 2:n + 2], scalar=sm[:, 16 + gi:17 + gi], in1=o[:, :n],
                                                                op0=ALU.mult, op1=ALU.add), reads=[pe, sm, o], writes=[o])
                sh[g] = o
            rr, rk, rv, wdf, wdb, ad, gd0, gd1 = (sh[g] for g in range(5, 13))
            put(rs["r_f"], rs["r_b"], rr)
            put(rs["v_f"], rs["v_b"], rv)
            for (wd_, wi, bcol, fwd) in ((wdf, W2F, 2, True), (wdb, W2B, 3, False)):
                S.op("scalar", lambda e: e.activation(out=wd_[:, :n], in_=wd_[:, :n], func=AF.Tanh), reads=[wd_], writes=[wd_])
                p = pss.get()
                S.op("tensor", lambda e: e.matmul(p[:, :n], lr[:, wi, :], wd_[:, :n], start=True, stop=True), reads=[lr, wd_], writes=[p])
                o = pool.get()
                S.op("scalar", lambda e: e.activation(out=o[:, :n], in_=p[:, :n], func=AF.Sigmoid, bias=sm[:, bcol:bcol + 1]), reads=[p, sm], writes=[o])
                S.op("vector", lambda e: e.tensor_scalar(out=o[:, :n], in0=o[:, :n], scalar1=-float(np.exp(-0.5)), scalar2=None, op0=ALU.mult),
                     reads=[o], writes=[o])
                if fwd:
                    put(rs["lw_f"], None, o)
                else:
                    put(None, rs["lw_b"], o)
            p = pss.get()
            S.op("tensor", lambda e: e.matmul(p[:, :n], lr[:, A2, :], ad[:, :n], start=True, stop=True), reads=[lr, ad], writes=[p])
            a_ = pool.get()
            S.op("scalar", lambda e: e.activation(out=a_[:, :n], in_=p[:, :n], func=AF.Sigmoid, bias=sm[:, 4:5]), reads=[p, sm], writes=[a_])
            kk = pool.get()
            S.op("vector", lambda e: e.tensor_scalar(out=kk[:, :n], in0=rk[:, :n], scalar1=sm[:, 5:6], scalar2=None, op0=ALU.mult), reads=[rk, sm], writes=[kk])
            k2 = pool.get()
            S.op("scalar", lambda e: e.activation(out=k2[:, :n], in_=kk[:, :n], func=AF.Square), reads=[kk], writes=[k2])
            p = pss.get()
            S.op("tensor", lambda e: e.matmul(p[:, :n], lr[:, BD, :], k2[:, :n], start=True, stop=True), reads=[lr, k2], writes=[p])
            S.op("vector", lambda e: e.tensor_scalar(out=k2[:, :n], in0=p[:, :n], scalar1=1e-12, scalar2=None, op0=ALU.add), reads=[p], writes=[k2])
            S.op("scalar", lambda e: e.activation(out=k2[:, :n], in_=k2[:, :n], func=AF.Sqrt), reads=[k2], writes=[k2])
            S.op("vector", lambda e: e.reciprocal(out=k2[:, :n], in_=k2[:, :n]), reads=[k2], writes=[k2])
            S.op("gpsimd", lambda e: e.tensor_tensor(out=kk[:, :n], in0=kk[:, :n], in1=k2[:, :n], op=ALU.mult), reads=[kk, k2], writes=[kk])
            bv = pool.get()
            S.op("gpsimd", lambda e: e.tensor_tensor(out=bv[:, :n], in0=kk[:, :n], in1=a_[:, :n], op=ALU.mult), reads=[kk, a_], writes=[bv])
            put(rs["b_f"], rs["b_b"], bv)
            av = pool.get()
            S.op("vector", lambda e: e.tensor_scalar(out=av[:, :n], in0=kk[:, :n], scalar1=-1.0, scalar2=None, op0=ALU.mult), reads=[kk], writes=[av])
            put(rs["a_f"], rs["a_b"], av)
            km = pool.get()
            S.op("vector", lambda e: e.tensor_scalar(out=km[:, :n], in0=a_[:, :n], scalar1=sm[:, 6:7], scalar2=sm[:, 24:25], op0=ALU.mult, op1=ALU.add),
                 reads=[a_, sm], writes=[km])
            S.op("gpsimd", lambda e: e.tensor_tensor(out=km[:, :n], in0=km[:, :n], in1=rk[:, :n], op=ALU.mult), reads=[km, rk], writes=[km])
            put(rs["k_f"], rs["k_b"], km)
            if lat:
                t1 = pool.get()
                S.op("vector", lambda e: e.scalar_tensor_tensor(out=t1[:, :n], in0=rr[:, :n], scalar=sm[:, 7:8], in1=km[:, :n], op0=ALU.mult, op1=ALU.mult),
                     reads=[rr, sm, km], writes=[t1])
                p = pss.get()
                S.op("tensor", lambda e: e.matmul(p[:, :n], lr[:, BD, :], t1[:, :n], start=True, stop=True), reads=[lr, t1], writes=[p])
                bo = pool.get()
                S.op("vector", lambda e: e.tensor_tensor(out=bo[:, :n], in0=rv[:, :n], in1=p[:, :n], op=ALU.mult), reads=[rv, p], writes=[bo])
                put(rs["bonus"], None, bo)
                S.op("scalar", lambda e: e.activation(out=gd0[:, :n], in_=gd0[:, :n], func=AF.Sigmoid), reads=[gd0], writes=[gd0])
                S.op("scalar", lambda e: e.activation(out=gd1[:, :n], in_=gd1[:, :n], func=AF.Sigmoid), reads=[gd1], writes=[gd1])
                p = pss.get()
                S.op("tensor", lambda e: e.matmul(p[:, :n], lr[:, G20, :], gd0[:, :n], start=True, stop=False), reads=[lr, gd0], writes=[p])
                S.op("tensor", lambda e: e.matmul(p[:, :n], lr[:, G21, :], gd1[:, :n], start=False, stop=True), reads=[lr, gd1], writes=[p])
                go = pool.get()
                S.op("scalar", lambda e: e.activation(out=go[:, :n], in_=p[:, :n], func=AF.Copy), reads=[p], writes=[go])
                put(rs["gout"], None, go)
        barrier(S)
        S.reset(_ph)
    for _ph in (S.mark(),):
        consts = scan_consts(S, cst_d)
        units = [dict(K=128, V=128, q=gs["q_f"], k=gs["k_f"], lw=gs["lw_f"], v=gs["v_f"], out=gs["o_f"]),
                 dict(K=128, V=128, q=gs["q_b"], k=gs["k_b"], lw=gs["lw_b"], v=gs["v_b"], out=gs["o_b"])]
        if "gla" not in SKIP:
            chunk_scan(S, cfg, units, consts)
        barrier(S)
        S.reset(_ph)
    for _rw in range(0 if "rwkv" not in SKIP else 1, 1):
        for _ph in (S.mark(),):
            consts = scan_consts(S, cst_d)
            units = []
            for hh, d in ((0, "f"), (0, "b"), (1, "f"), (1, "b")):
                U = {}
                for nm, key in (("q", "r"), ("k", "k"), ("v", "v"), ("lw", "lw"), ("a", "a"), ("b", "b")):
                    U[nm] = rs[f"{key}_{d}"][64 * hh:64 * hh + 64, :]
                    U["src_" + nm] = rs[f"{key}_{d}"]
                U["out"] = rs[f"o_{d}"][64 * hh:64 * hh + 64, :]
                U["src_out"] = rs[f"o_{d}"]
                units.append(U)
            rwkv_scan(S, cfg, units, consts)
            barrier(S)
            S.reset(_ph)
    for _ph in (S.mark(),):
        sm = load_const(S, "sm2", (128, 40), sm_d[:])
        bd = load_const(S, "bd", (128, 128), lr_d[7])
        S.op("vector", lambda e: e.tensor_scalar(out=bd[:], in0=bd[:], scalar1=1.0 / 64.0, scalar2=None, op0=ALU.mult), reads=[bd], writes=[bd])
        tp = rot_sbuf(S, "p5", (128, 512), F32, n=12)
        ob_ = rot_sbuf(S, "p5o", (128, 512), BF16, n=4)
        ps = Rot([S.psum(f"p5ps{i}") for i in range(4)])
        for s0 in range(0, SS, 512):
            n = min(512, SS - s0)
            t0 = L + s0
            rp = rev_pos(t0, n, L, SS)
            a, b = tp.get(), tp.get()
            S.load("sync", a, a[:, :n], gs["o_f"][:, t0:t0 + n], src=gs["o_f"])
            S.load("scalar", b, b[:, :n], gs["o_b"][:, rp:rp + n], src=gs["o_b"])
            o = ob_.get()
            S.op("vector", lambda e: e.tensor_tensor(out=o[:, :n], in0=a[:, :n], in1=rev_ap(b[:, :n]), op=ALU.add), reads=[a, b], writes=[o])
            S.store("sync", out, out[0, :, s0:s0 + n], o, o[:, :n])
            a, b, bo, go = tp.get(), tp.get(), tp.get(), tp.get()
            S.load("sync", a, a[:, :n], rs["o_f"][:, t0:t0 + n], src=rs["o_f"])
            S.load("scalar", b, b[:, :n], rs["o_b"][:, rp:rp + n], src=rs["o_b"])
            S.load("sync", bo, bo[:, :n], rs["bonus"][:, t0:t0 + n], src=rs["bonus"])
            S.load("scalar", go, go[:, :n], rs["gout"][:, t0:t0 + n], src=rs["gout"])
            S.op("vector", lambda e: e.tensor_tensor(out=a[:, :n], in0=a[:, :n], in1=rev_ap(b[:, :n]), op=ALU.add), reads=[a, b], writes=[a])
            p1 = ps.get()
            S.op("tensor", lambda e: e.matmul(p1[:, :n], bd[:], a[:, :n], start=True, stop=True), reads=[bd, a], writes=[p1])
            d_ = tp.get()
            S.op("vector", lambda e: e.tensor_tensor(out=d_[:, :n], in0=a[:, :n], in1=p1[:, :n], op=ALU.subtract), reads=[a, p1], writes=[d_])
            d2 = tp.get()
            S.op("scalar", lambda e: e.activation(out=d2[:, :n], in_=d_[:, :n], func=AF.Square), reads=[d_], writes=[d2])
            p2 = ps.get()
            S.op("tensor", lambda e: e.matmul(p2[:, :n], bd[:], d2[:, :n], start=True, stop=True), reads=[bd, d2], writes=[p2])
            S.op("vector", lambda e: e.tensor_scalar(out=d2[:, :n], in0=p2[:, :n], scalar1=RW_LN_EPS, scalar2=None, op0=ALU.add), reads=[p2], writes=[d2])
            S.op("scalar", lambda e: e.activation(out=d2[:, :n], in_=d2[:, :n], func=AF.Sqrt), reads=[d2], writes=[d2])
            S.op("vector", lambda e: e.reciprocal(out=d2[:, :n], in_=d2[:, :n]), reads=[d2], writes=[d2])
            S.op("gpsimd", lambda e: e.tensor_tensor(out=d_[:, :n], in0=d_[:, :n], in1=d2[:, :n], op=ALU.mult), reads=[d_, d2], writes=[d_])
            S.op("vector", lambda e: e.tensor_scalar(out=d_[:, :n], in0=d_[:, :n], scalar1=sm[:, 33:34], scalar2=sm[:, 34:35], op0=ALU.mult, op1=ALU.add),
                 reads=[d_, sm], writes=[d_])
            S.op("gpsimd", lambda e: e.tensor_tensor(out=d_[:, :n], in0=d_[:, :n], in1=bo[:, :n], op=ALU.add), reads=[d_, bo], writes=[d_])
            o = ob_.get()
            S.op("vector", lambda e: e.tensor_tensor(out=o[:, :n], in0=d_[:, :n], in1=go[:, :n], op=ALU.mult), reads=[d_, go], writes=[o])
            S.store("sync", out, out[2, :, s0:s0 + n], o, o[:, :n])
    return nc, stack, S


def run_mix1(cfg, hT_all, inp):
    nc, stack, S = build_mix1(cfg)
    w_in = inp["l1_w_in"]
    hfm = fm(hT_all)
    sc = host_scan_consts(rwkv=True)
    RW0 = 3104
    maps = []
    pidx = np.arange(128)
    bd = (pidx[:, None] // 64 == pidx[None, :] // 64).astype(np.float32)

    def pad_cols(w, n=128):
        return np.concatenate([w, np.zeros((w.shape[0], n - w.shape[1]), np.float32)], axis=1)

    def pad_rows(w, r0=0, n=128):
        o = np.zeros((n, w.shape[1]), np.float32)
        o[r0:r0 + w.shape[0]] = w
        return o

    def pad_vec(v, n=128):
        o = np.zeros((n,), np.float32)
        o[:v.shape[0]] = v
        return o
    for c in range(NCORES):
        gh, vh = c // 2, c % 2
        ch = slice(c * 128, (c + 1) * 128)
        cols = [w_in[:, gh * 128:(gh + 1) * 128], w_in[:, 512 + gh * 128:512 + (gh + 1) * 128],
                w_in[:, 1024 + gh * 256 + vh * 128:1024 + gh * 256 + (vh + 1) * 128],
                pad_cols(w_in[:, 2048:2080]), w_in[:, 2080 + gh * 256 + vh * 128:2080 + gh * 256 + (vh + 1) * 128]]
        rcols = [(0, ch), (1024, ch), (2048, ch)]
        mu_p, mu_n = inp["l1_rwkv_mu_prev"], inp["l1_rwkv_mu_next"]
        mup_g, mun_g = [], []
        for base, sl in rcols:
            cols.append(w_in[:, RW0 + base + sl.start:RW0 + base + sl.stop])
            mup_g.append(mu_p[base + sl.start:base + sl.stop])
            mun_g.append(mu_n[base + sl.start:base + sl.stop])
        for base, width in ((3072, 96), (3168, 96), (3264, 96), (3360, 128), (3488, 128)):
            cols.append(pad_cols(w_in[:, RW0 + base:RW0 + base + width]))
            mup_g.append(pad_vec(mu_p[base:base + width]))
            mun_g.append(pad_vec(mu_n[base:base + width]))
        sm = np.zeros((128, 40), np.float32)
        gk = slice(gh * 128, (gh + 1) * 128)
        sm[:, 0] = inp["l1_gla_gate_bias_f"][gk]
        sm[:, 1] = inp["l1_gla_gate_bias_b"][gk]
        sm[:, 2] = inp["l1_rwkv_w0_f"][ch]
        sm[:, 3] = inp["l1_rwkv_w0_b"][ch]
        sm[:, 4] = inp["l1_rwkv_a0"][ch]
        sm[:, 5] = inp["l1_rwkv_k_k"][ch]
        sm[:, 6] = inp["l1_rwkv_k_a"][ch]
        sm[:, 7] = inp["l1_rwkv_r_k"].reshape(-1)[ch]
        for gi in range(8):
            sm[:, 8 + gi] = mup_g[gi]
            sm[:, 16 + gi] = mun_g[gi]
        sm[:, 33] = inp["l1_rwkv_ln_w"][ch]
        sm[:, 34] = inp["l1_rwkv_ln_b"][ch]
        lrk = np.stack([pad_rows(inp["l1_gla_gate_up_f"][:, gk], 0), pad_rows(inp["l1_gla_gate_up_b"][:, gk], 16),
                        pad_rows(inp["l1_rwkv_w2_f"][:, ch]), pad_rows(inp["l1_rwkv_w2_b"][:, ch]), pad_rows(inp["l1_rwkv_a2"][:, ch]),
                        inp["l1_rwkv_g2"][0:128, ch], inp["l1_rwkv_g2"][128:256, ch], bd], axis=0)
        m = {"hT": hfm, "w": wfm(np.concatenate(cols, axis=1)), "smalls": sm, "lowrank": np.ascontiguousarray(lrk)}
        m.update(sc)
        maps.append(m)
    res = run_prog(nc, stack, S, maps)
    if DEBUG:
        global DBG
        DBG = res
    gla_o = np.concatenate([res[c]["oT"][0] for c in range(NCORES)], axis=0)
    gla_g = np.concatenate([res[c]["oT"][1] for c in range(NCORES)], axis=0)
    rw = np.concatenate([res[c]["oT"][2] for c in range(NCORES)], axis=0)
    return gla_o, gla_g, rw


def build_l5(cfg):
    nc, stack, S = new_prog()
    KC, NB, ntl = cfg.KC, 256, cfg.ntl
    NE = cfg.N_EXP
    xT = S.dram("xT", (128, KC, ntl), F32, "ExternalInput")
    mo = S.dram("mo", (3, 128, 8, ntl), BF16, "ExternalInput")
    mod_d = S.dram("mod", (128, 6 * KC, 2), F32, "ExternalInput")
    gains_d = S.dram("gains", (128, 2, KC), F32, "ExternalInput")
    gn_d = S.dram("gnorm", (128, 2), F32, "ExternalInput")
    wo_d = S.dram("wo", (KC, 128, KC * 128), F32, "ExternalInput")
    rt_d = S.dram("router", (128, KC, NE), F32, "ExternalInput")
    x3T = S.dram("x3T", (128, KC, ntl), F32, "ExternalOutput")
    h2T = S.dram("h2T", (128, KC, ntl), BF16, "ExternalOutput")
    gates = S.dram("gates", (ntl, NE), F32, "ExternalOutput")
    wo_b = S.dram("wo_b", (KC, 128, KC * 128), BF16)
    for _ph in (S.mark(),):
        sf, sb = rot_sbuf(S, "cv_f", (128, 2048), F32, n=3), rot_sbuf(S, "cv_b", (128, 2048), BF16, n=3)
        convert_w(S, wo_d, wo_b, KC, KC * 128, sf, sb, [0])
        barrier(S)
        S.reset(_ph)
    mod = load_const(S, "mod_sb", (128, 6 * KC, 2), mod_d[:])
    gains = load_const(S, "gains_sb", (128, 2, KC), gains_d[:])
    gn = [Buf(f"gain{i}", gains[:, i, :]) for i in range(2)]
    for g in gn:
        g.writers = gains.writers
    gnorm = load_const(S, "gnorm_sb", (128, 2), gn_d[:])
    S.op("vector", lambda e: e.tensor_scalar(out=gnorm[:], in0=gnorm[:], scalar1=16.0, scalar2=None, op0=ALU.mult), reads=[gnorm], writes=[gnorm])
    router = load_const(S, "router_sb", (128, KC, NE), rt_d[:])
    ones = make_ones(S)
    G1 = gate_scalars(S, cfg, mod, gn[0], 2, "g1")
    A2, B2 = mod_scalars(S, cfg, mod, gn[1], 4, 3, "m2")
    xb = S.sbuf("xb", (128, KC, NB))
    go = S.sbuf("go", (128, 8, NB), BF16)
    gg = S.sbuf("gg", (128, 8, NB), BF16)
    ob = S.sbuf("ob", (128, KC, NB), BF16)
    yb = S.sbuf("yb", (128, KC, NB))
    hf = S.sbuf("hf", (128, KC, NB))
    hb = S.sbuf("hb", (128, KC, NB), BF16)
    sq = rot_sbuf(S, "sq", (128, NB))
    tmp = rot_sbuf(S, "tmp", (128, NB), n=4)
    rstd = S.sbuf("rstd", (128, NB))
    wo = rot_sbuf(S, "wo", (128, KC * 128), BF16, n=3)
    ps_s = S.psum("ps_stat")
    ps_y = Rot([S.psum("ps_y0"), S.psum("ps_y1")])
    ps_r = S.psum("ps_r")
    lg = rot_sbuf(S, "lg", (128, 8), n=2)
    m8 = rot_sbuf(S, "m8", (128, 8), n=2)
    ex = rot_sbuf(S, "ex", (128, 8), n=2)
    mk = rot_sbuf(S, "mk", (128, 8), n=2)
    s1 = rot_sbuf(S, "s1", (128, 2), n=2)
    for s0 in range(0, ntl, NB):
        n = min(NB, ntl - s0)
        S.load("sync", xb, xb[:, :, :n], xT[:, :, s0:s0 + n])
        S.load("scalar", go, go[:, :, :n], mo[0, :, :, s0:s0 + n])
        S.load("sync", gg, gg[:, :, :n], mo[1, :, :, s0:s0 + n])
        S.load("scalar", ob, ob[:, 8:16, :n], mo[2, :, :, s0:s0 + n])
        for hh in range(4):
            for c in (2 * hh, 2 * hh + 1):
                q = sq.get()
                S.op("scalar", lambda e: e.activation(out=q[:, :n], in_=go[:, c, :n], func=AF.Square), reads=[go], writes=[q])
                S.op("tensor", lambda e: e.matmul(ps_s[:, :n], ones[:], q[:, :n], start=(c % 2 == 0), stop=(c % 2 == 1)), reads=[ones, q], writes=[ps_s])
            S.op("vector", lambda e: e.tensor_scalar(out=rstd[:, :n], in0=ps_s[:, :n], scalar1=NORM_EPS * 256, scalar2=None, op0=ALU.add),
                 reads=[ps_s], writes=[rstd])
            S.op("scalar", lambda e: e.activation(out=rstd[:, :n], in_=rstd[:, :n], func=AF.Sqrt), reads=[rstd], writes=[rstd])
            S.op("vector", lambda e: e.reciprocal(out=rstd[:, :n], in_=rstd[:, :n]), reads=[rstd], writes=[rstd])
            for c in (2 * hh, 2 * hh + 1):
                t = tmp.get()
                S.op("vector", lambda e: e.scalar_tensor_tensor(out=t[:, :n], in0=go[:, c, :n], scalar=gnorm[:, c % 2:c % 2 + 1], in1=rstd[:, :n],
                                                                op0=ALU.mult, op1=ALU.mult), reads=[go, gnorm, rstd], writes=[t])
                S.op("gpsimd", lambda e: e.tensor_tensor(out=ob[:, c, :n], in0=t[:, :n], in1=gg[:, c, :n], op=ALU.mult), reads=[t, gg], writes=[ob])
        for dc in range(KC):
            w = wo.get()
            S.load("sync", w, w[:], wo_b[dc], src=wo_b)
            p = ps_y.get()
            for kc in range(KC):
                S.op("tensor", lambda e: e.matmul(p[:, :n], w[:, kc * 128:(kc + 1) * 128], ob[:, kc, :n], start=(kc == 0), stop=(kc == KC - 1)),
                     reads=[w, ob], writes=[p])
            S.op("scalar", lambda e: e.activation(out=yb[:, dc, :n], in_=p[:, :n], func=AF.Copy), reads=[p], writes=[yb])
        rms_stats(S, cfg, yb, n, sq, ones, ps_s, rstd)
        resid_norm_add(S, cfg, xb, yb, n, rstd, G1[0], tmp)
        S.store("sync", x3T, x3T[:, :, s0:s0 + n], xb, xb[:, :, :n])
        rms_stats(S, cfg, xb, n, sq, ones, ps_s, rstd)
        norm_mod_apply(S, cfg, xb, n, rstd, A2[0], B2[0], hf, tmp)
        S.op("scalar", lambda e: e.activation(out=hb[:, :, :n], in_=hf[:, :, :n], func=AF.Copy), reads=[hf], writes=[hb])
        S.store("scalar", h2T, h2T[:, :, s0:s0 + n], hb, hb[:, :, :n])
        for tb in range(0, n if ROUTER_ON else 0, 128):
            for kc in range(KC):
                S.op("tensor", lambda e: e.matmul(ps_r[:, 0:NE], hf[:, kc, tb:tb + 128], router[:, kc, :], start=(kc == 0), stop=(kc == KC - 1)),
                     reads=[hf, router], writes=[ps_r])
            l_, m_, e_, k_, s_ = lg.get(), m8.get(), ex.get(), mk.get(), s1.get()
            X = mybir.AxisListType.X
            BIGV = 1.0e30
            S.op("vector", lambda e: e.tensor_copy(out=l_[:], in_=ps_r[:, 0:NE]), reads=[ps_r], writes=[l_])
            S.op("vector", lambda e: e.reduce_max(out=s_[:, 0:1], in_=l_[:], axis=X), reads=[l_], writes=[s_])
            S.op("vector", lambda e: e.tensor_scalar(out=l_[:], in0=l_[:], scalar1=s_[:, 0:1], scalar2=None, op0=ALU.subtract), reads=[l_, s_], writes=[l_])
            S.op("vector", lambda e: e.tensor_scalar(out=m_[:], in0=l_[:], scalar1=BIGV, scalar2=1.0, op0=ALU.mult, op1=ALU.add), reads=[l_], writes=[m_])
            S.op("vector", lambda e: e.tensor_scalar(out=m_[:], in0=m_[:], scalar1=0.0, scalar2=-BIGV, op0=ALU.max, op1=ALU.mult), reads=[m_], writes=[m_])
            S.op("vector", lambda e: e.tensor_tensor(out=m_[:], in0=m_[:], in1=l_[:], op=ALU.add), reads=[m_, l_], writes=[m_])
            S.op("vector", lambda e: e.reduce_max(out=s_[:, 1:2], in_=m_[:], axis=X), reads=[m_], writes=[s_])
            S.op("vector", lambda e: e.tensor_scalar(out=k_[:], in0=l_[:], scalar1=s_[:, 1:2], scalar2=BIGV, op0=ALU.subtract, op1=ALU.mult),
                 reads=[l_, s_], writes=[k_])
            S.op("vector", lambda e: e.tensor_scalar(out=k_[:], in0=k_[:], scalar1=1.0, scalar2=0.0, op0=ALU.add, op1=ALU.max), reads=[k_], writes=[k_])
            S.op("vector", lambda e: e.tensor_scalar(out=k_[:], in0=k_[:], scalar1=1.0, scalar2=None, op0=ALU.min), reads=[k_], writes=[k_])
            S.op("scalar", lambda e: e.activation(out=e_[:], in_=l_[:], func=AF.Exp), reads=[l_], writes=[e_])
            S.op("vector", lambda e: e.tensor_tensor(out=e_[:], in0=e_[:], in1=k_[:], op=ALU.mult), reads=[e_, k_], writes=[e_])
            S.op("vector", lambda e: e.reduce_sum(out=s_[:, 0:1], in_=e_[:], axis=X), reads=[e_], writes=[s_])
            S.op("vector", lambda e: e.reciprocal(out=s_[:, 0:1], in_=s_[:, 0:1]), reads=[s_], writes=[s_])
            S.op("vector", lambda e: e.tensor_scalar(out=e_[:], in0=e_[:], scalar1=s_[:, 0:1], scalar2=None, op0=ALU.mult), reads=[e_, s_], writes=[e_])
            S.store("sync", gates, gates[s0 + tb:s0 + tb + 128, :], e_, e_[:])
    return nc, stack, S


def run_l5(cfg, x2T_lat, gla_o, gla_g, rw, mod1, inp):
    nc, stack, S = build_l5(cfg)
    KC, ntl = cfg.KC, cfg.ntl
    wo = inp["l1_w_out"]
    wo_l = np.ascontiguousarray(wo.reshape(KC, 128, KC, 128).transpose(2, 1, 0, 3).reshape(KC, 128, KC * 128))
    gains = np.ascontiguousarray(np.stack([vec_fm(inp[k]) for k in ("l1_norm_mix_post", "l1_norm_ffn_pre")], axis=1))
    gnorm = np.ascontiguousarray(inp["l1_gla_norm"].reshape(2, 128).T)
    router = wfm(inp["l1_moe_router"])
    maps = []
    for i in range(NCORES):
        sl = slice(i * ntl, (i + 1) * ntl)
        mo = np.stack([fm(np.ascontiguousarray(a[:, sl])) for a in (gla_o, gla_g, rw)], axis=0)
        maps.append({"xT": fm(np.ascontiguousarray(x2T_lat[:, sl])), "mo": mo, "mod": mod1, "gains": gains, "gnorm": gnorm, "wo": wo_l, "router": router})
    res = run_prog(nc, stack, S, maps)
    x3T = np.concatenate([unfm(res[i]["x3T"]) for i in range(NCORES)], axis=1)
    h2T = np.concatenate([unfm(res[i]["h2T"]) for i in range(NCORES)], axis=1)
    gates = np.concatenate([res[i]["gates"] for i in range(NCORES)], axis=0)
    return x3T, h2T, gates


def build_moe(cfg):
    nc, stack, S = new_prog()
    KC, SS = cfg.KC, cfg.S
    NJ = cfg.D_FF_E // 128
    NB = 512
    hT = S.dram("hT", (128, KC, SS), BF16, "ExternalInput")
    gate_d = S.dram("gate", (128, SS), F32, "ExternalInput")
    wg_d = S.dram("wg", (NJ, 128, KC * 128), F32, "ExternalInput")
    wu_d = S.dram("wu", (NJ, 128, KC * 128), F32, "ExternalInput")
    wd_d = S.dram("wd", (KC, 128, NJ * 128), F32, "ExternalInput")
    out = S.dram("part", (128, KC, SS), BF16, "ExternalOutput")
    wg_b = S.dram("wg_b", (NJ, 128, KC * 128), BF16)
    wu_b = S.dram("wu_b", (NJ, 128, KC * 128), BF16)
    wd_b = S.dram("wd_b", (KC, 128, NJ * 128), BF16)
    for _ph in (S.mark(),):
        sf, sb = rot_sbuf(S, "cv_f", (128, 2048), F32, n=4), rot_sbuf(S, "cv_b", (128, 2048), BF16, n=4)
        ctr = [0]
        convert_w(S, wg_d, wg_b, NJ, KC * 128, sf, sb, ctr)
        convert_w(S, wu_d, wu_b, NJ, KC * 128, sf, sb, ctr)
        convert_w(S, wd_d, wd_b, KC, NJ * 128, sf, sb, ctr)
        barrier(S)
        S.reset(_ph)
    fb = ffn_bufs(S, cfg, NJ, NB)
    hbs = rot_sbuf(S, "hb", (128, KC, NB), BF16, n=2)
    gbs = rot_sbuf(S, "gb", (128, NB), F32, n=2)
    obs = rot_sbuf(S, "obs", (128, NB), BF16, n=4)
    for s0 in range(0, SS, NB):
        n = min(NB, SS - s0)
        hb, gb = hbs.get(), gbs.get()
        S.load("sync", hb, hb[:, :, :n], hT[:, :, s0:s0 + n])
        S.load("scalar", gb, gb[:, :n], gate_d[:, s0:s0 + n])

        def evac(dc, po):
            o = obs.get()
            S.op("vector", lambda e: e.tensor_tensor(out=o[:, :n], in0=gb[:, :n], in1=po[:, :n], op=ALU.mult), reads=[gb, po], writes=[o])
            S.store("gpsimd", out, out[:, dc, s0:s0 + n], o, o[:, :n])
        ffn_block(S, cfg, hb, n, NJ, wg_b, wu_b, wd_b, fb, evac)
    return nc, stack, S


def run_moe(cfg, h2T, gates, inp):
    nc, stack, S = build_moe(cfg)
    hfm = fm(h2T)
    maps = []
    for e in range(NCORES):
        wg, wu, wd = host_ffn_w(inp["l1_moe_w_gate"][e], inp["l1_moe_w_up"][e], inp["l1_moe_w_down"][e])
        g = np.ascontiguousarray(np.broadcast_to(gates[:, e][None, :], (128, cfg.S)))
        maps.append({"hT": hfm, "gate": g, "wg": wg, "wu": wu, "wd": wd})
    res = run_prog(nc, stack, S, maps)
    return [res[e]["part"] for e in range(NCORES)]


def build_fin(cfg):
    nc, stack, S = new_prog()
    KC, NB, ntl = cfg.KC, 256, cfg.ntl
    NE = cfg.N_EXP
    xT = S.dram("xT", (128, KC, ntl), F32, "ExternalInput")
    parts = S.dram("parts", (NE, 128, KC, ntl), BF16, "ExternalInput")
    mod_d = S.dram("mod", (128, 6 * KC, 2), F32, "ExternalInput")
    gain_d = S.dram("gain", (128, KC), F32, "ExternalInput")
    outT = S.dram("outT", (128, KC, ntl), F32, "ExternalOutput")
    mod = load_const(S, "mod_sb", (128, 6 * KC, 2), mod_d[:])
    gain = load_const(S, "gain_sb", (128, KC), gain_d[:])
    ones = make_ones(S)
    G2 = gate_scalars(S, cfg, mod, gain, 5, "g2")
    xb = rot_sbuf(S, "xb", (128, KC, NB), n=2)
    pb = rot_sbuf(S, "pb", (128, KC, NB), BF16, n=4)
    fbuf = S.sbuf("fb", (128, KC, NB))
    sq = rot_sbuf(S, "sq", (128, NB))
    tmp = rot_sbuf(S, "tmp", (128, NB), n=4)
    rstd = S.sbuf("rstd", (128, NB))
    ps_s = S.psum("ps_stat")
    for s0 in range(0, ntl, NB):
        n = min(NB, ntl - s0)
        x = xb.get()
        S.load("sync", x, x[:, :, :n], xT[:, :, s0:s0 + n])
        for e_ in range(NE):
            p = pb.get()
            S.load("scalar" if e_ % 2 else "sync", p, p[:, :, :n], parts[e_, :, :, s0:s0 + n])
            eng = "vector" if e_ % 2 == 0 else "gpsimd"
            if e_ == 0:
                S.op(eng, lambda e: e.tensor_copy(out=fbuf[:, :, :n], in_=p[:, :, :n]), reads=[p], writes=[fbuf])
            else:
                S.op(eng, lambda e: e.tensor_tensor(out=fbuf[:, :, :n], in0=fbuf[:, :, :n], in1=p[:, :, :n], op=ALU.add), reads=[fbuf, p], writes=[fbuf])
        rms_stats(S, cfg, fbuf, n, sq, ones, ps_s, rstd)
        resid_norm_add(S, cfg, x, fbuf, n, rstd, G2[0], tmp)
        S.store("sync", outT, outT[:, :, s0:s0 + n], x, x[:, :, :n])
    return nc, stack, S


def run_fin(cfg, x3T, parts, mod1, inp):
    nc, stack, S = build_fin(cfg)
    ntl = cfg.ntl
    maps = []
    for i in range(NCORES):
        sl = slice(i * ntl, (i + 1) * ntl)
        maps.append({"xT": fm(np.ascontiguousarray(x3T[:, sl])), "parts": np.ascontiguousarray(np.stack([p[:, :, sl] for p in parts], axis=0)),
                     "mod": mod1, "gain": vec_fm(inp["l1_norm_ffn_post"])})
    res = run_prog(nc, stack, S, maps)
    return np.concatenate([unfm(res[i]["outT"]) for i in range(NCORES)], axis=1)


def kernel(**inp):
    inp = {k: np.asarray(v) for k, v in inp.items()}
    S_len, L_len = inp["x"].shape[1], inp["ctx"].shape[1]
    cfg = Cfg(S=S_len, L=L_len, D=inp["x"].shape[2], D_FF=inp["l0_ffn_w_gate"].shape[1], N_EXP=inp["l1_moe_w_gate"].shape[0],
              D_FF_E=inp["l1_moe_w_gate"].shape[2])
    xT_lat = np.ascontiguousarray(inp["x"][0].T)
    xT_ctx = np.ascontiguousarray(inp["ctx"][0].T)
    mods = run_ada(cfg, inp)
    hT0 = run_pre(cfg, xT_lat, xT_ctx, mods[0], inp["l0_norm_mix_pre"])
    oT0 = run_mix0(cfg, hT0, inp)
    x2c, x2l, hT1 = run_l3(cfg, xT_lat, xT_ctx, oT0, mods, inp)
    gla_o, gla_g, rw = run_mix1(cfg, hT1, inp)
    x3T, h2T, gates = run_l5(cfg, x2l, gla_o, gla_g, rw, mods[1], inp)
    parts = run_moe(cfg, h2T, gates, inp)
    outT = run_fin(cfg, x3T, parts, mods[1], inp)
    return np.ascontiguousarray(outT.T)[None].astype(np.float32)
```
